# Optimizing a Trainium2 kernel written in Bass

```python
import math
import jax, jax.numpy as jnp
from jax import lax
import numpy as np

D_MODEL = 1024
BATCH = 2
SEQ = 8192
DEPTH = 1

N_MEM = 256
D_LRU = 512
LRU_BLOCKS = 8
CONV_W = 4
LRU_C = 8.0
N_HEADS = 8
N_KV = 2
HEAD_DIM = 64
D_NSA = N_HEADS * HEAD_DIM
D_MIX = D_LRU + D_NSA
CMP_BLOCK = 32
CMP_STRIDE = 16
SEL_BLOCK = 64
N_SELECT = 16
WINDOW = 512
Q_BLOCK = 128
MEM_HEADS = 4
MEM_HEAD_DIM = D_MODEL // MEM_HEADS
PEER_HEADS = 8
PEER_NKEYS = 128
PEER_N = PEER_NKEYS * PEER_NKEYS
PEER_TOPK = 16
PEER_QDIM = 256
PEER_TOK_BLOCK = 128
EPS = 1e-6
KV_W = N_KV * HEAD_DIM
IN_SIZES = [D_LRU, D_LRU, D_NSA, KV_W, KV_W, KV_W, KV_W, KV_W, KV_W, 3 * N_HEADS]
N_IN = sum(IN_SIZES)

kernel_name = 'hymba_style_rglru_nsa_peer_block'


def rmsnorm(x, g):
    xf = x.astype(jnp.float32)
    y = xf * lax.rsqrt(jnp.mean(xf * xf, axis=-1, keepdims=True) + EPS)
    return (y * g.astype(jnp.float32)).astype(x.dtype)


def masked_softmax(s, mask):
    s = jnp.where(mask, s.astype(jnp.float32), -1e30)
    p = jax.nn.softmax(s, axis=-1)
    return jnp.where(mask, p, 0.0)


def rg_lru(xb, gate, conv_w, conv_b, w_a, b_a, w_i, b_i, lam):
    B, S, _ = xb.shape
    xp = jnp.pad(xb, ((0, 0), (CONV_W - 1, 0), (0, 0)))
    xc = conv_b
    for k in range(CONV_W):
        xc = xc + xp[:, k:k + S] * conv_w[k]
    xh = xc.reshape(B, S, LRU_BLOCKS, D_LRU // LRU_BLOCKS)
    r = jax.nn.sigmoid(jnp.einsum('bshi,hij->bshj', xh, w_a) + b_a).reshape(B, S, D_LRU)
    i = jax.nn.sigmoid(jnp.einsum('bshi,hij->bshj', xh, w_i) + b_i).reshape(B, S, D_LRU)
    log_a = -LRU_C * r.astype(jnp.float32) * jax.nn.softplus(-lam.astype(jnp.float32))
    a = jnp.exp(log_a)
    b = jnp.sqrt(-jnp.expm1(2.0 * log_a)) * (i * xc).astype(jnp.float32)

    def comb(left, right):
        a1, b1 = left
        a2, b2 = right
        return a1 * a2, a2 * b1 + b2

    _, h = lax.associative_scan(comb, (a, b), axis=1)
    return h.astype(xb.dtype) * jax.nn.gelu(gate)


def compress(k_raw, pos, w1, w2):
    B, S = k_raw.shape[:2]
    nc = (S - CMP_BLOCK) // CMP_STRIDE + 1
    idx = jnp.arange(nc)[:, None] * CMP_STRIDE + jnp.arange(CMP_BLOCK)[None, :]
    blk = k_raw[:, idx] + pos[None, None, :, None, :]
    blk = blk.transpose(0, 1, 3, 2, 4).reshape(B, nc, N_KV, CMP_BLOCK * HEAD_DIM)
    return jax.nn.gelu(blk @ w1) @ w2


def nsa(q, kc, vc, ks, vs, kw, vw, gates):
    B, S = q.shape[:2]
    G = N_HEADS // N_KV
    nc = kc.shape[1]
    nsb = S // SEL_BLOCK
    nsel = min(N_SELECT, nsb)
    ci = jnp.arange(nc)[:, None] * CMP_STRIDE
    bj = jnp.arange(nsb)[None, :] * SEL_BLOCK
    overlap = ((ci < bj + SEL_BLOCK) & (ci + CMP_BLOCK > bj)).astype(jnp.float32)
    cmp_end = jnp.arange(nc) * CMP_STRIDE + CMP_BLOCK - 1
    ks_b = ks.reshape(B, nsb, SEL_BLOCK, N_KV, HEAD_DIM).transpose(0, 3, 1, 2, 4)
    vs_b = vs.reshape(B, nsb, SEL_BLOCK, N_KV, HEAD_DIM).transpose(0, 3, 1, 2, 4)
    kw_p = jnp.pad(kw, ((0, 0), (WINDOW, 0), (0, 0), (0, 0)))
    vw_p = jnp.pad(vw, ((0, 0), (WINDOW, 0), (0, 0), (0, 0)))
    q5 = q.reshape(B, S, N_KV, G, HEAD_DIM) * (HEAD_DIM ** -0.5)
    bi = jnp.arange(B)[:, None, None, None]
    hi = jnp.arange(N_KV)[None, :, None, None]
    jb = jnp.arange(nsb)

    def block(c):
        s0 = c * Q_BLOCK
        t = s0 + jnp.arange(Q_BLOCK)
        qc = lax.dynamic_slice_in_dim(q5, s0, Q_BLOCK, axis=1)
        gc = lax.dynamic_slice_in_dim(gates, s0, Q_BLOCK, axis=1)
        sc = jnp.einsum('bqkgd,bnkd->bkgqn', qc, kc)
        mc = cmp_end[None, :] <= t[:, None]
        pc = masked_softmax(sc, mc)
        o_cmp = jnp.einsum('bkgqn,bnkd->bqkgd', pc.astype(vc.dtype), vc)
        imp = jnp.einsum('bkgqn,nj->bkqj', pc, overlap)
        cur = t // SEL_BLOCK
        valid = jb[None, :] <= cur[:, None]
        forced = (jb[None, :] == 0) | (jb[None, :] == cur[:, None]) | (jb[None, :] == cur[:, None] - 1)
        imp = jnp.where(valid, jnp.where(forced, jnp.inf, imp), -jnp.inf)
        top, idx = lax.top_k(imp, nsel)
        kg = ks_b[bi, hi, idx]
        vg = vs_b[bi, hi, idx]
        tok = idx[..., None] * SEL_BLOCK + jnp.arange(SEL_BLOCK)
        ms = (top > -1.0)[..., None] & (tok <= t[None, None, :, None, None])
        ss = jnp.einsum('bqkgd,bkqsld->bkgqsl', qc, kg)
        ps = masked_softmax(ss.reshape(B, N_KV, G, Q_BLOCK, nsel * SEL_BLOCK),
                            ms.reshape(B, N_KV, 1, Q_BLOCK, nsel * SEL_BLOCK)).reshape(ss.shape)
        o_slc = jnp.einsum('bkgqsl,bkqsld->bqkgd', ps.astype(vg.dtype), vg)
        kwc = lax.dynamic_slice_in_dim(kw_p, s0, Q_BLOCK + WINDOW, axis=1)
        vwc = lax.dynamic_slice_in_dim(vw_p, s0, Q_BLOCK + WINDOW, axis=1)
        kpos = s0 - WINDOW + jnp.arange(Q_BLOCK + WINDOW)
        mw = (kpos[None, :] <= t[:, None]) & (kpos[None, :] > t[:, None] - WINDOW) & (kpos[None, :] >= 0)
        sw = jnp.einsum('bqkgd,bnkd->bkgqn', qc, kwc)
        pw = masked_softmax(sw, mw)
        o_win = jnp.einsum('bkgqn,bnkd->bqkgd', pw.astype(vwc.dtype), vwc)
        out = gc[..., 0:1] * o_cmp + gc[..., 1:2] * o_slc + gc[..., 2:3] * o_win
        return out.reshape(B, Q_BLOCK, D_NSA)

    outs = lax.map(block, jnp.arange(S // Q_BLOCK))
    return outs.transpose(1, 0, 2, 3).reshape(B, S, D_NSA)


def mem_attn(xn, memn, wq, wk, wv, wo):
    B, S, _ = xn.shape
    M = memn.shape[1]
    q = (xn @ wq).reshape(B, S, MEM_HEADS, MEM_HEAD_DIM) * (MEM_HEAD_DIM ** -0.5)
    k = (memn @ wk).reshape(B, M, MEM_HEADS, MEM_HEAD_DIM)
    v = (memn @ wv).reshape(B, M, MEM_HEADS, MEM_HEAD_DIM)
    s = jnp.einsum('bshd,bmhd->bhsm', q, k).astype(jnp.float32)
    p = jax.nn.softmax(s, axis=-1).astype(v.dtype)
    o = jnp.einsum('bhsm,bmhd->bshd', p, v).reshape(B, S, D_MODEL)
    return o @ wo


def peer(xn, w_pq, sub_keys, U, V):
    B, S, D = xn.shape
    xt = xn.reshape(-1, PEER_TOK_BLOCK, D)

    def block(xc):
        T = xc.shape[0]
        q = (xc @ w_pq).reshape(T, PEER_HEADS, 2, PEER_QDIM // 2)
        s = jnp.einsum('thcd,hcnd->thcn', q, sub_keys).astype(jnp.float32)
        v1, i1 = lax.top_k(s[:, :, 0], PEER_TOPK)
        v2, i2 = lax.top_k(s[:, :, 1], PEER_TOPK)
        cand = (v1[..., :, None] + v2[..., None, :]).reshape(T, PEER_HEADS, PEER_TOPK * PEER_TOPK)
        cid = (i1[..., :, None] * PEER_NKEYS + i2[..., None, :]).reshape(T, PEER_HEADS, PEER_TOPK * PEER_TOPK)
        top, pos = lax.top_k(cand, PEER_TOPK)
        eid = jnp.take_along_axis(cid, pos, axis=-1)
        g = jax.nn.softmax(top, axis=-1).astype(xc.dtype)
        u = U[eid]
        v = V[eid]
        h = jax.nn.gelu(jnp.einsum('td,thkd->thk', xc, u))
        return jnp.einsum('thk,thkd->td', g * h, v)

    return lax.map(block, xt).reshape(B, S, D)


def setup_inputs(seed: int = 0) -> dict:
    key = jax.random.key(seed)
    ks = iter(jax.random.split(key, 40))
    f32 = jnp.float32

    def nrm(shape, scale):
        return jax.random.normal(next(ks), shape, f32) * scale

    def gain(shape):
        return 1.0 + 0.02 * jax.random.normal(next(ks), shape, f32)

    L = DEPTH
    bd = D_LRU // LRU_BLOCKS
    u = jax.random.uniform(next(ks), (L, D_LRU), f32, minval=0.9, maxval=0.999)
    a0 = u ** (1.0 / LRU_C)
    lam = jnp.log(a0) - jnp.log1p(-a0)
    return {
        'x': nrm((BATCH, SEQ, D_MODEL), 1.0),
        'mem': nrm((BATCH, N_MEM, D_MODEL), 1.0),
        'g_mix': gain((L, D_MODEL)),
        'w_in': nrm((L, D_MODEL, N_IN), D_MODEL ** -0.5),
        'b_gate': nrm((L, 3 * N_HEADS), 0.1),
        'conv_w': nrm((L, CONV_W, D_LRU), CONV_W ** -0.5),
        'conv_b': nrm((L, D_LRU), 0.02),
        'w_rg_a': nrm((L, LRU_BLOCKS, bd, bd), bd ** -0.5),
        'b_rg_a': nrm((L, LRU_BLOCKS, bd), 0.1),
        'w_rg_i': nrm((L, LRU_BLOCKS, bd, bd), bd ** -0.5),
        'b_rg_i': nrm((L, LRU_BLOCKS, bd), 0.1),
        'lam': lam,
        'cmp_pos_k': nrm((L, CMP_BLOCK, HEAD_DIM), 0.02),
        'cmp_w1_k': nrm((L, CMP_BLOCK * HEAD_DIM, HEAD_DIM), (CMP_BLOCK * HEAD_DIM) ** -0.5),
        'cmp_w2_k': nrm((L, HEAD_DIM, HEAD_DIM), HEAD_DIM ** -0.5),
        'cmp_pos_v': nrm((L, CMP_BLOCK, HEAD_DIM), 0.02),
        'cmp_w1_v': nrm((L, CMP_BLOCK * HEAD_DIM, HEAD_DIM), (CMP_BLOCK * HEAD_DIM) ** -0.5),
        'cmp_w2_v': nrm((L, HEAD_DIM, HEAD_DIM), HEAD_DIM ** -0.5),
        'g_out_lru': gain((L, D_LRU)),
        'g_out_nsa': gain((L, D_NSA)),
        'w_out': nrm((L, D_MIX, D_MODEL), D_MIX ** -0.5),
        'g_mem_q': gain((L, D_MODEL)),
        'g_mem_kv': gain((L, D_MODEL)),
        'w_mq': nrm((L, D_MODEL, D_MODEL), D_MODEL ** -0.5),
        'w_mk': nrm((L, D_MODEL, D_MODEL), D_MODEL ** -0.5),
        'w_mv': nrm((L, D_MODEL, D_MODEL), D_MODEL ** -0.5),
        'w_mo': nrm((L, D_MODEL, D_MODEL), D_MODEL ** -0.5),
        'g_ffn': gain((L, D_MODEL)),
        'w_pq': nrm((L, D_MODEL, PEER_HEADS * PEER_QDIM), D_MODEL ** -0.5),
        'sub_keys': nrm((L, PEER_HEADS, 2, PEER_NKEYS, PEER_QDIM // 2), (PEER_QDIM // 2) ** -0.5),
        'peer_u': nrm((L, PEER_N, D_MODEL), D_MODEL ** -0.5),
        'peer_v': nrm((L, PEER_N, D_MODEL), (PEER_HEADS * PEER_TOPK) ** -0.5),
        'g_final': gain((D_MODEL,)),
    }


def reference(x, mem, g_mix, w_in, b_gate, conv_w, conv_b, w_rg_a, b_rg_a, w_rg_i, b_rg_i, lam,
              cmp_pos_k, cmp_w1_k, cmp_w2_k, cmp_pos_v, cmp_w1_v, cmp_w2_v, g_out_lru, g_out_nsa,
              w_out, g_mem_q, g_mem_kv, w_mq, w_mk, w_mv, w_mo, g_ffn, w_pq, sub_keys, peer_u,
              peer_v, g_final):
    B, S, _ = x.shape
    G = N_HEADS // N_KV
    offs = [int(o) for o in np.cumsum(IN_SIZES)[:-1]]

    def kv(t):
        return t.reshape(B, S, N_KV, HEAD_DIM)

    for l in range(DEPTH):
        h = rmsnorm(x, g_mix[l])
        z = h @ w_in[l]
        x_lru, gate_lru, q, k_c, v_c, k_s, v_s, k_w, v_w, g_raw = jnp.split(z, offs, axis=-1)
        y_lru = rg_lru(x_lru, gate_lru, conv_w[l], conv_b[l], w_rg_a[l], b_rg_a[l],
                       w_rg_i[l], b_rg_i[l], lam[l])
        kc = compress(kv(k_c), cmp_pos_k[l], cmp_w1_k[l], cmp_w2_k[l])
        vc = compress(kv(v_c), cmp_pos_v[l], cmp_w1_v[l], cmp_w2_v[l])
        gates = jax.nn.sigmoid(g_raw + b_gate[l]).reshape(B, S, N_KV, G, 3)
        y_nsa = nsa(q.reshape(B, S, N_HEADS, HEAD_DIM), kc, vc, kv(k_s), kv(v_s), kv(k_w), kv(v_w), gates)
        y = jnp.concatenate([rmsnorm(y_lru, g_out_lru[l]), rmsnorm(y_nsa, g_out_nsa[l])], axis=-1)
        x = x + y @ w_out[l]
        x = x + mem_attn(rmsnorm(x, g_mem_q[l]), rmsnorm(mem, g_mem_kv[l]),
                         w_mq[l], w_mk[l], w_mv[l], w_mo[l])
        x = x + peer(rmsnorm(x, g_ffn[l]), w_pq[l], sub_keys[l], peer_u[l], peer_v[l])
    return rmsnorm(x, g_final)
```

```python
import numpy as np
import concourse.bass as bass
import concourse.mybir as mybir
from concourse.bass_utils import run_bass_kernel_spmd
from contextlib import ExitStack

F32 = mybir.dt.float32
BF16 = mybir.dt.bfloat16
U32 = mybir.dt.uint32
I32 = mybir.dt.int32
AF = mybir.ActivationFunctionType
ALU = mybir.AluOpType
AX = mybir.AxisListType

ENGS = ['pe', 'dve', 'act', 'pool', 'sp']
NDMA = 24
NSW = 8
SAME_ENG_SYNC = {'pe': False, 'dve': True, 'act': True, 'pool': True, 'sp': True}
NEG = -30000.0
_CUT = 99
EPS = 1e-6


class Prog:
    def __init__(self, nc, ctx):
        self.nc = nc
        self.ops = {e: [] for e in ENGS}
        self.cnt = {e: 0 for e in ENGS}
        self.sems = {e: ctx.enter_context(nc.semaphore("s_" + e)) for e in ENGS}
        self.dsem = [ctx.enter_context(nc.semaphore("d%d" % i)) for i in range(NDMA + NSW)]
        self.dcnt = [0] * (NDMA + NSW)
        self.drr = 0
        self.srr = 0
        self.last_w = {}
        self.readers = {}
        self.seen = {e: {} for e in ENGS}
        self.defer = None

    def capture_begin(self):
        self.defer = []

    def capture_end(self):
        q, self.defer = self.defer, None
        return q

    def drain(self, q, n):
        for _ in range(min(n, len(q))):
            self.op(*q.pop(0))

    def op(self, eng, fn, reads=(), writes=(), dma=False):
        if self.defer is not None:
            self.defer.append((eng, fn, tuple(reads), tuple(writes), dma))
            return None
        deps = {}

        def add(tok):
            if tok is None:
                return
            sem, val, teng, sid = tok
            if teng == eng and not SAME_ENG_SYNC[eng]:
                return
            if sid not in deps or deps[sid][1] < val:
                deps[sid] = tok

        for k in reads:
            add(self.last_w.get(k))
        for k in writes:
            add(self.last_w.get(k))
            for t in self.readers.get(k, {}).values():
                add(t)
        slot = None
        if dma:
            if eng == 'pool':
                slot = NDMA + self.srr
                self.srr = (self.srr + 1) % NSW
            else:
                slot = self.drr
                self.drr = (self.drr + 1) % NDMA
            if self.dcnt[slot] > 0:
                add((self.dsem[slot], self.dcnt[slot], 'dma', ('d', slot)))
        waits = []
        seen = self.seen[eng]
        for sid, tok in deps.items():
            if seen.get(sid, 0) >= tok[1]:
                continue
            seen[sid] = tok[1]
            waits.append((tok[0], tok[1]))
        if dma:
            self.dcnt[slot] += 16
            tok = (self.dsem[slot], self.dcnt[slot], 'dma', ('d', slot))
            inc = (self.dsem[slot], 16)
        else:
            self.cnt[eng] += 1
            tok = (self.sems[eng], self.cnt[eng], eng, ('e', eng))
            inc = (self.sems[eng], 1)
        self.ops[eng].append((fn, waits, inc))
        for k in writes:
            self.last_w[k] = tok
            self.readers[k] = {}
        for k in reads:
            if k in writes:
                continue
            r = self.readers.setdefault(k, {})
            if tok[3] not in r or r[tok[3]][1] < tok[1]:
                r[tok[3]] = tok
        return tok

    def pe(self, fn, reads=(), writes=()):
        return self.op('pe', fn, reads, writes)

    def dve(self, fn, reads=(), writes=()):
        return self.op('dve', fn, reads, writes)

    def act(self, fn, reads=(), writes=()):
        return self.op('act', fn, reads, writes)

    def pool(self, fn, reads=(), writes=()):
        return self.op('pool', fn, reads, writes)

    def dma(self, out, in_, reads=(), writes=(), eng='sp'):
        return self.op(eng, lambda e: e.dma_start(out=out, in_=in_), reads, writes, dma=True)

    def emit(self):
        nc = self.nc
        fin = []
        for e in ENGS:
            if e != 'sp' and self.cnt[e] > 0:
                fin.append((self.sems[e], self.cnt[e]))
        for i in range(NDMA + NSW):
            if self.dcnt[i] > 0:
                fin.append((self.dsem[i], self.dcnt[i]))
        ops = self.ops

        def run(e, lst):
            for fn, waits, inc in lst:
                for s, v in waits:
                    e.wait_ge(s, v)
                ins = fn(e)
                ins.then_inc(inc[0], inc[1])

        with nc.Block() as block:
            @block.sync
            def _(e):
                run(e, ops['sp'])
                for s, v in fin:
                    e.wait_ge(s, v)

            @block.tensor
            def _(e):
                run(e, ops['pe'])

            @block.vector
            def _(e):
                run(e, ops['dve'])

            @block.scalar
            def _(e):
                run(e, ops['act'])

            @block.gpsimd
            def _(e):
                run(e, ops['pool'])
        self.ops = {e: [] for e in ENGS}
        self.last_w = {}
        self.readers = {}
        for e in ENGS:
            for sid in list(self.seen[e].keys()):
                pass


def build(dbg=False, upto=3, n1=16, n2=16, n3=16):
    nc = bass.Bass("TRN2", target_bir_lowering=False)

    def din(name, shape, dt=F32):
        return nc.dram_tensor(name, shape, dt, kind="ExternalInput").ap()

    xfull = din("xfull", [8192, 1024])
    xown = din("xown", [2048, 1024])
    memb = din("memb", [256, 1024])
    wcat = din("wcat", [1024, 2328])
    gvec = din("gvec", [128, 48])
    bgate = din("bgate", [128, 24])
    lrup = din("lrup", [128, 36])
    wabd = din("wabd", [128, 512])
    wibd = din("wibd", [128, 512])
    w1k = din("w1k", [128, 4096])
    w1v = din("w1v", [128, 4096])
    w2k = din("w2k", [128, 128])
    w2v = din("w2v", [128, 128])
    posk = din("posk", [128, 32])
    posv = din("posv", [128, 32])
    ovl = din("ovl", [512, 128])
    ctab = din("ctab", [128, 148])
    tq4 = din("tq4", [16, 128, 512])
    fbt = din("fbt", [16, 128, 128])
    w_out = din("w_out", [1024, 1024])
    w_mq = din("w_mq", [1024, 1024])
    w_mk = din("w_mk", [1024, 1024])
    w_mv = din("w_mv", [1024, 1024])
    w_mo = din("w_mo", [1024, 1024])
    w_pq = din("w_pq", [1024, 2048])
    skT = din("skT", [128, 2048])
    gffn = din("gffn", [1, 1024])
    gfin = din("gfin", [1, 1024])
    peer_u = din("peer_u", [16384, 1024])
    peer_v = din("peer_v", [16384, 1024])
    x1d = nc.dram_tensor("x1d", [2048, 1024], F32, kind="Internal").ap()
    uv16 = nc.dram_tensor("uv16", [16384, 2048], BF16, kind="Internal").ap()
    yout = nc.dram_tensor("yout", [2048, 1024], F32, kind="ExternalOutput").ap()
    dbgo = {}
    if dbg:
        for nm, shp in [("d_yl", [128, 4 * 2048]), ("d_kc", [128, 512]), ("d_vc", [128, 4 * 2 * 196]),
                        ("d_yn", [2048, 512]), ("d_x2", [2048, 1024]), ("d_ks", [128, 8192]),
                        ("d_vs", [128, 64 * 132])]:
            dbgo[nm] = nc.dram_tensor(nm, shp, F32, kind="ExternalOutput").ap()

    with ExitStack() as ctx:
        P = Prog(nc, ctx)

        def sbt(c, name, shape, dt=F32):
            return c.enter_context(nc.sbuf_tensor(name, shape, dt))

        def pst(c, name, shape, dt=F32):
            return c.enter_context(nc.psum_tensor(name, shape, dt))

        def mm(out, lhsT, rhs, start, stop, r, w):
            P.pe(lambda e: e.matmul(out, lhsT=lhsT, rhs=rhs, start=start, stop=stop), reads=r, writes=w)

        def trp(out, in_, ident, r, w):
            P.pe(lambda e: e.transpose(out, in_, ident), reads=r, writes=w)

        def actf(out, in_, func, r, w, bias=0.0, scale=1.0, accum=None):
            if accum is None:
                P.act(lambda e: e.activation(out=out, in_=in_, func=func, bias=bias, scale=scale), reads=r, writes=w)
            else:
                P.act(lambda e: e.activation(out=out, in_=in_, func=func, bias=bias, scale=scale, accum_out=accum),
                      reads=r, writes=w)

        def ts(out, in0, s1, s2, op0, op1, r, w, eng='dve'):
            if op1 is None:
                P.op(eng, lambda e: e.tensor_scalar(out=out, in0=in0, scalar1=s1, scalar2=None, op0=op0), r, w)
            else:
                P.op(eng, lambda e: e.tensor_scalar(out=out, in0=in0, scalar1=s1, scalar2=s2, op0=op0, op1=op1), r, w)

        def tt(out, in0, in1, op, r, w, eng='dve'):
            P.op(eng, lambda e: e.tensor_tensor(out=out, in0=in0, in1=in1, op=op), r, w)

        def stt(out, in0, scalar, in1, op0, op1, r, w, accum=None):
            if accum is None:
                P.dve(lambda e: e.scalar_tensor_tensor(out=out, in0=in0, scalar=scalar, in1=in1, op0=op0, op1=op1), r, w)
            else:
                P.dve(lambda e: e.scalar_tensor_tensor(out=out, in0=in0, scalar=scalar, in1=in1, op0=op0, op1=op1,
                                                       accum_out=accum), r, w)

        def cp(out, in_, r, w, eng='dve'):
            if eng == 'act':
                P.act(lambda e: e.activation(out=out, in_=in_, func=AF.Copy), r, w)
            else:
                P.op(eng, lambda e: e.tensor_copy(out=out, in_=in_), r, w)

        def mset(ap, val, w, eng='dve'):
            P.op(eng, lambda e: e.memset(ap, val), (), w)

        identf = sbt(ctx, "identf", [128, 128])
        identb = sbt(ctx, "identb", [128, 128], BF16)
        ones_b = sbt(ctx, "ones_b", [128, 128], BF16)
        mhalf = sbt(ctx, "mhalf", [128, 4])
        cneg = sbt(ctx, "cneg", [128, 4], BF16)
        ssq = sbt(ctx, "ssq", [128, 8])
        rstd = sbt(ctx, "rstd", [128, 8])
        xnb = sbt(ctx, "xnb", [128, 1024], BF16)
        stage = [sbt(ctx, "stage0", [128, 1024]), sbt(ctx, "stage1", [128, 1024])]
        stg_i = [0]
        A = pst(ctx, "psA", [128, 1024])
        B = pst(ctx, "psB", [128, 1024])
        c0 = pst(ctx, "psc0", [128, 512])[:]
        c1 = pst(ctx, "psc1", [128, 512])[:]
        c2 = pst(ctx, "psc2", [128, 512])[:]
        ptb = pst(ctx, "ptb", [128, 1024], BF16)
        A0, A1 = A[:, 0:512], A[:, 512:1024]
        B0, B1 = B[:, 0:512], B[:, 512:1024]

        mset(identf[:], 1.0, ["identf"])
        P.pool(lambda e: e.affine_select(out=identf[:], in_=identf[:], pattern=[[-1, 128]], compare_op=ALU.is_equal,
                                         fill=0.0, base=0, channel_multiplier=1), ["identf"], ["identf"])
        cp(identb[:], identf[:], ["identf"], ["identb"])
        mset(ones_b[:], 1.0, ["ones_b"])
        mset(mhalf[:], -0.5, ["mhalf"])
        Rsel = sbt(ctx, "Rsel", [128, 256])
        mset(Rsel[:], 0.0, ["Rsel"])
        mset(Rsel[:, 127:128], 1.0, ["Rsel"])
        cnegM = sbt(ctx, "cnegM", [128, 2, 128], BF16)
        mset(cnegM[:], 0.0, ["cnegM"])
        mset(cnegM[0:64, 0, :], -1.0 / 16, ["cnegM"])
        mset(cnegM[64:128, 1, :], -1.0 / 16, ["cnegM"])
        mset(cneg[:], 0.0, ["cneg"])
        mset(cneg[0:64, 0:1], -1.0 / 16, ["cneg"])
        mset(cneg[64:128, 1:2], -1.0 / 16, ["cneg"])
        mset(cneg[:, 2:3], -1.0 / 32, ["cneg"])

        def calc_rstd(col, n):
            ts(ssq[:, col:col + 1], ssq[:, col:col + 1], 1.0 / n, EPS, ALU.mult, ALU.add, ["ssq%d" % col], ["ssq%d" % col])
            tt(rstd[:, col:col + 1], ssq[:, col:col + 1], mhalf[:, 0:1], ALU.pow, ["ssq%d" % col, "mhalf"],
               ["rstd%d" % col], eng='pool')

        xnb_alt = [None]

        def norm_T(x_sb, kx, dstT, kdst, gain=None, xnf=None, par=0, xbo=None):
            xb_, kxb, col = (xnb[:], "xnb", 0) if par == 0 else (stage[1][:].bitcast(BF16)[:, 0:1024], "stage1", 4)
            if xbo is not None:
                xb_, kxb = xbo
            actf(xb_, x_sb, AF.Square, [kx], [kxb, "ssq%d" % col], accum=ssq[:, col:col + 1])
            calc_rstd(col, 1024)
            if gain is None:
                actf(xb_, x_sb, AF.Copy, [kx, "rstd%d" % col], [kxb], scale=rstd[:, col:col + 1])
            else:
                stt(xnf, x_sb, rstd[:, col:col + 1], gain, ALU.mult, ALU.mult, [kx, "rstd%d" % col, "gain"], ["xnf"])
                cp(xb_, xnf, ["xnf"], [kxb], eng='act')
            for c in range(8):
                trp(ptb[:, 128 * c:128 * c + 128], xb_[:, 128 * c:128 * c + 128], identb[:], [kxb, "identb"], ["ptb"])
            cp(dstT, ptb[:].rearrange("p (c t) -> p c t", c=8), ["ptb"], [kdst])

        def load_w(dst, kdst, src, ncols, col0=0, gain=None, engs=('dve', 'pool')):
            k = 0
            for c in range(8):
                for o in range(0, ncols, 1024):
                    w = min(1024, ncols - o)
                    si = stg_i[0] % 2
                    stg_i[0] += 1
                    st = stage[si]
                    P.dma(st[:, 0:w], src[128 * c:128 * c + 128, col0 + o:col0 + o + w], writes=["stage%d" % si])
                    eng = engs[k % len(engs)]
                    k += 1
                    if gain is None:
                        cp(dst[:, c, o:o + w], st[:, 0:w], ["stage%d" % si], [kdst], eng=eng)
                    else:
                        ts(dst[:, c, o:o + w], st[:, 0:w], gain[:, c:c + 1], None, ALU.mult, None,
                           ["stage%d" % si, "gv"], [kdst], eng=eng)

        def load_small(dst, kdst, src, w, eng='dve'):
            for o in range(0, w, 1024):
                ww = min(1024, w - o)
                si = stg_i[0] % 2
                stg_i[0] += 1
                st = stage[si]
                P.dma(st[:, 0:ww], src[:, o:o + ww], writes=["stage%d" % si])
                cp(dst[:, o:o + ww], st[:, 0:ww], ["stage%d" % si], [kdst], eng=eng)

        gv = sbt(ctx, "gv", [128, 48])
        P.dma(gv[:], gvec, writes=["gv"])
        ctb = sbt(ctx, "ctb", [128, 148])
        P.dma(ctb[:], ctab, writes=["ctb"])
        cend = ctb[:, 0:4]
        kpos = ctb[:, 4:68]
        kpos512 = ctb[:, 68:132]

        with ExitStack() as cA:
            ksT = sbt(cA, "ksT", [128, 8192], BF16)
            kwT = sbt(cA, "kwT", [128, 8192], BF16)
            vs = sbt(cA, "vs", [128, 64, 2, 66], BF16)
            vw = sbt(cA, "vw", [128, 64, 2, 66], BF16)
            yl_own = sbt(cA, "yl_own", [128, 4, 2048], BF16)
            KC = sbt(cA, "KC", [128, 512], BF16)
            VCx = sbt(cA, "VCx", [128, 4, 2, 196], BF16)
            kmx = sbt(cA, "kmx", [128, 2])
            mset(kmx[:], 0.0, ["kmx"])
            mset(vs[:, :, :, 64:65], 1.0, ["vs"])
            mset(vw[:, :, :, 64:65], 1.0, ["vw"])
            mset(VCx[:, :, :, 64:65], 1.0, ["VCx"])
            for nt in range(4):
                si = stg_i[0] % 2
                stg_i[0] += 1
                P.dma(stage[si][:, 0:128], ovl[128 * nt:128 * nt + 128, :], writes=["stage%d" % si])
                for kv in range(2):
                    cp(VCx[:, nt, kv, 68:196], stage[si][:, 0:128], ["stage%d" % si], ["VCx"])

            with ExitStack() as c1x:
                wfs = sbt(c1x, "wfs", [128, 8, 1280], BF16)
                xt2 = [sbt(c1x, "xt0", [128, 1024]), sbt(c1x, "xt1", [128, 1024])]
                xTm2 = [sbt(c1x, "xTm0", [128, 8, 512], BF16), sbt(c1x, "xTm1", [128, 8, 512], BF16)]
                kcr2 = [sbt(c1x, "kcr0", [128, 528], BF16), sbt(c1x, "kcr1", [128, 528], BF16)]
                vcr2 = [sbt(c1x, "vcr0", [128, 528], BF16), sbt(c1x, "vcr1", [128, 528], BF16)]
                xl2 = [sbt(c1x, "xl0", [128, 4, 516]), sbt(c1x, "xl1", [128, 4, 516])]
                xc = sbt(c1x, "xc", [128, 4, 256])
                xcb = sbt(c1x, "xcb", [128, 4, 256], BF16)
                rr_ = sbt(c1x, "r", [128, 4, 256])
                sq = sbt(c1x, "sq", [128, 4, 256])
                ig = sbt(c1x, "ig", [128, 4, 256])
                hb = sbt(c1x, "hb", [128, 4, 256])
                ysel = sbt(c1x, "ysel", [128, 4, 128])
                state = sbt(c1x, "state", [128, 4])
                lp = sbt(c1x, "lp", [128, 36])
                cl = sbt(c1x, "cl", [128, 8])
                wa = sbt(c1x, "wa", [128, 4, 128], BF16)
                wi = sbt(c1x, "wi", [128, 4, 128], BF16)
                W1k = sbt(c1x, "W1k", [128, 32, 128], BF16)
                W1v = sbt(c1x, "W1v", [128, 32, 128], BF16)
                W2k = sbt(c1x, "W2k", [128, 128], BF16)
                W2v = sbt(c1x, "W2v", [128, 128], BF16)
                pk = sbt(c1x, "pk", [128, 32], BF16)
                pv_ = sbt(c1x, "pv", [128, 32], BF16)
                c1b = sbt(c1x, "c1b", [128, 2])
                Gk = sbt(c1x, "Gk", [128, 32], BF16)
                Gv = sbt(c1x, "Gv", [128, 128], BF16)
                sqk = sbt(c1x, "sqk", [128, 512], BF16)
                km1 = sbt(c1x, "km1", [128, 1])

                load_w(wfs, "wfs", wcat, 1280, 0, gain=gv[:, 0:8], engs=('dve',))
                P.dma(lp[:], lrup, writes=["lp"])
                load_small(wa[:].rearrange("p a b -> p (a b)"), "wa", wabd, 512)
                load_small(wi[:].rearrange("p a b -> p (a b)"), "wi", wibd, 512)
                load_small(W1k[:].rearrange("p a b -> p (a b)"), "W1k", w1k, 4096, eng='pool')
                load_small(W1v[:].rearrange("p a b -> p (a b)"), "W1v", w1v, 4096, eng='pool')
                load_small(W2k[:], "W2k", w2k, 128)
                load_small(W2v[:], "W2v", w2v, 128)
                load_small(pk[:], "pk", posk, 32)
                load_small(pv_[:], "pv", posv, 32)
                cw = lp[:, 0:16].rearrange("p (c k) -> p c k", c=4)
                cb = lp[:, 16:20]
                ba = lp[:, 20:24]
                bi = lp[:, 24:28]
                lam = lp[:, 28:32]
                selc = lp[:, 32:36]
                actf(cl[:, 0:4], lam, AF.Exp, ["lp"], ["cl"], scale=-1.0)
                actf(cl[:, 0:4], cl[:, 0:4], AF.Ln, ["cl"], ["cl"], bias=1.0)
                ts(cl[:, 4:8], cl[:, 0:4], -16.0, None, ALU.mult, None, ["cl"], ["cl2"])
                ts(cl[:, 0:4], cl[:, 0:4], -8.0, None, ALU.mult, None, ["cl", "cl2"], ["cl"])
                mset(state[:], 0.0, ["state"])
                mset(xl2[0][:, :, 0:3], 0.0, ["xl0"])
                mset(kcr2[0][:, 0:16], 0.0, ["kcr0"])
                mset(vcr2[0][:, 0:16], 0.0, ["vcr0"])
                for (W1, kW1, pp, kpp, col) in [(W1k, "W1k", pk, "pk", 0), (W1v, "W1v", pv_, "pv", 1)]:
                    for l in range(32):
                        mm(c0[:, col:col + 1], W1[:, l, :], pp[:, l:l + 1], l == 0, l == 31, [kW1, kpp], ["c0"])
                    cp(c1b[:, col:col + 1], c0[:, col:col + 1], ["c0"], ["c1b"])

                def stage_a(i):
                    xTm_, kx = xTm2[i % 2], "xTm%d" % (i % 2)
                    xl_, kxl = xl2[i % 2], "xl%d" % (i % 2)
                    kcr_, kkc = kcr2[i % 2], "kcr%d" % (i % 2)
                    vcr_, kvc = vcr2[i % 2], "vcr%d" % (i % 2)
                    for j in range(4):
                        blk = 4 * i + j
                        xt_, kxt = xt2[j % 2], "xt%d" % (j % 2)
                        P.dma(xt_[:], xfull[128 * blk:128 * blk + 128, :], writes=[kxt])
                        norm_T(xt_[:], kxt, xTm_[:, :, 128 * j:128 * j + 128], kx, par=j % 2)
                    for m in range(8):
                        pt_, kp = (A0, "A0") if m % 2 == 0 else (A1, "A1")
                        for c in range(8):
                            mm(pt_, wfs[:, c, 128 * m:128 * m + 128], xTm_[:, c, :], c == 0, c == 7, ["wfs", kx], [kp])
                        if m < 4:
                            cp(xl_[:, m, 3:515], pt_, [kp], [kxl], eng='act')
                        elif m == 4:
                            cp(kcr_[:, 16:528], pt_, [kp], [kkc])
                        elif m == 5:
                            cp(vcr_[:, 16:528], pt_, [kp], [kvc])
                        elif m == 6:
                            cp(ksT[:, 512 * i:512 * i + 512], pt_, [kp], ["ksT"])
                        else:
                            cp(kwT[:, 512 * i:512 * i + 512], pt_, [kp], ["kwT"])
                    for j in range(4):
                        blk = 4 * i + j
                        for c in range(8):
                            mm(B0[:, 0:256], xTm_[:, c, 128 * j:128 * j + 128], wfs[:, c, 1024:1280], c == 0, c == 7,
                               ["wfs", kx], ["B0"])
                        cp(vs[:, blk, :, 0:64], B0[:, 0:128].rearrange("p (a b) -> p a b", a=2), ["B0"], ["vs"])
                        cp(vw[:, blk, :, 0:64], B0[:, 128:256].rearrange("p (a b) -> p a b", a=2), ["B0"], ["vw"])

                def stage_b(i):
                    xl_, kxl = xl2[i % 2], "xl%d" % (i % 2)
                    xln, kxln = xl2[(i + 1) % 2], "xl%d" % ((i + 1) % 2)
                    for (kt, kk) in [(ksT, "ksT"), (kwT, "kwT")]:
                        actf(sqk[:], kt[:, 512 * i:512 * i + 512], AF.Square, [kk], ["sqk"])
                        mm(B1, ones_b[:], sqk[:], True, True, ["ones_b", "sqk"], ["B1"])
                        P.dve(lambda e: e.tensor_reduce(out=km1[:], in_=B1, axis=AX.X, op=ALU.max), ["B1"], ["km1"])
                        tt(kmx[:, 0:1], kmx[:, 0:1], km1[:], ALU.max, ["kmx", "km1"], ["kmx"])
                    cp(xln[:, :, 0:3], xl_[:, :, 512:515], [kxl], [kxln])
                    for hf in range(2):
                        o = 256 * hf
                        for ch in range(4):
                            ts(xc[:, ch, :], xl_[:, ch, o:o + 256], cw[:, ch, 0:1], cb[:, ch:ch + 1], ALU.mult, ALU.add,
                               [kxl, "lp"], ["xc%d" % ch])
                            for k in range(1, 4):
                                stt(xc[:, ch, :], xl_[:, ch, o + k:o + k + 256], cw[:, ch, k:k + 1], xc[:, ch, :],
                                    ALU.mult, ALU.add, [kxl, "lp", "xc%d" % ch], ["xc%d" % ch])
                            cp(xcb[:, ch, :], xc[:, ch, :], ["xc%d" % ch], ["xcb%d" % ch], eng='pool')
                        for ch in range(4):
                            pa_, kpa = (c0, "c0") if ch % 2 == 0 else (c1, "c1")
                            mm(pa_[:, 0:256], wa[:, ch, :], xcb[:, ch, :], True, False, ["wa", "xcb%d" % ch], [kpa])
                            mm(pa_[:, 256:512], wi[:, ch, :], xcb[:, ch, :], False, True, ["wi", "xcb%d" % ch], [kpa])
                            actf(rr_[:, ch, :], pa_[:, 0:256], AF.Sigmoid, [kpa, "lp"], ["r%d" % ch], bias=ba[:, ch:ch + 1])
                            actf(ig[:, ch, :], pa_[:, 256:512], AF.Sigmoid, [kpa, "lp"], ["ig%d" % ch], bias=bi[:, ch:ch + 1])
                        for ch in range(4):
                            actf(sq[:, ch, :], rr_[:, ch, :], AF.Exp, ["r%d" % ch, "cl2"], ["sq%d" % ch], scale=cl[:, 4 + ch:5 + ch])
                            actf(rr_[:, ch, :], rr_[:, ch, :], AF.Exp, ["r%d" % ch, "cl"], ["r%d" % ch], scale=cl[:, ch:ch + 1])
                        K4 = lambda nm: [nm + "%d" % c_ for c_ in range(4)]
                        actf(sq[:].rearrange("p a b -> p (a b)"), sq[:].rearrange("p a b -> p (a b)"), AF.Sqrt, K4("sq"), K4("sq"),
                             bias=1.0, scale=-1.0)
                        for ch in range(4):
                            tt(ig[:, ch, :], ig[:, ch, :], xc[:, ch, :], ALU.mult, ["ig%d" % ch, "xc%d" % ch], ["ig%d" % ch])
                            tt(ig[:, ch, :], ig[:, ch, :], sq[:, ch, :], ALU.mult, ["ig%d" % ch, "sq%d" % ch], ["ig%d" % ch])
                            P.dve((lambda ch: lambda e: e.tensor_tensor_scan(
                                out=hb[:, ch, :], data0=rr_[:, ch, :], data1=ig[:, ch, :], initial=state[:, ch:ch + 1],
                                op0=ALU.mult, op1=ALU.add))(ch), ["r%d" % ch, "ig%d" % ch, "state"], ["hb%d" % ch])
                        cp(state[:], hb[:, :, 255], K4("hb"), ["state"])
                        for r2 in range(2):
                            ri = 2 * hf + r2
                            src = hb[:, :, 128 * r2:128 * r2 + 128]
                            if ri == 0:
                                ts(ysel[:], src, selc[:, 0:1], None, ALU.mult, None, K4("hb") + ["lp"], ["ysel"])
                            else:
                                stt(ysel[:], src, selc[:, ri:ri + 1], ysel[:], ALU.mult, ALU.add, K4("hb") + ["lp", "ysel"], ["ysel"])
                        if hf == 1:
                            cp(yl_own[:, :, 128 * i:128 * i + 128], ysel[:], ["ysel"], ["yl_own"])
                    p0 = 32 * (i % 4)
                    nt = i // 4
                    for (raw2, kr, W1, kW1, col, G, kG) in [(kcr2, "kcr", W1k, "W1k", 0, Gk, "Gk"),
                                                           (vcr2, "vcr", W1v, "W1v", 1, Gv, "Gv")]:
                        raw, kraw = raw2[i % 2], kr + "%d" % (i % 2)
                        rawn, krawn = raw2[(i + 1) % 2], kr + "%d" % ((i + 1) % 2)
                        for l in range(32):
                            mm(c2[:, 0:32], W1[:, l, :], raw[:, l:l + 497:16], l == 0, l == 31, [kW1, kraw], ["c2"])
                        gdst = G[:] if col == 0 else G[:, p0:p0 + 32]
                        actf(gdst, c2[:, 0:32], AF.Gelu_apprx_tanh, ["c2", "c1b"], [kG], bias=c1b[:, col:col + 1])
                        cp(rawn[:, 0:16], raw[:, 512:528], [kraw], [krawn])
                    mm(c2[:, 64:96], W2k[:], Gk[:], True, True, ["W2k", "Gk"], ["c2"])
                    cp(KC[:, 32 * i:32 * i + 32], c2[:, 64:96], ["c2"], ["KC"])
                    if i % 4 == 3:
                        mm(c2[:, 128:256], Gv[:], W2v[:], True, True, ["W2v", "Gv"], ["c2"])
                        cp(VCx[:, nt, :, 0:64], c2[:, 128:256].rearrange("p (a b) -> p a b", a=2), ["c2"], ["VCx"])

                for i in range(n1 + 1):
                    if i < n1:
                        stage_a(i)
                    if i >= 1:
                        stage_b(i - 1)
                actf(sqk[:], KC[:], AF.Square, ["KC"], ["sqk"])
                mm(B1, ones_b[:], sqk[:], True, True, ["ones_b", "sqk"], ["B1"])
                P.dve(lambda e: e.tensor_reduce(out=km1[:], in_=B1, axis=AX.X, op=ALU.max), ["B1"], ["km1"])
                tt(kmx[:, 0:1], kmx[:, 0:1], km1[:], ALU.max, ["kmx", "km1"], ["kmx"])
                ts(kmx[:, 1:2], kmx[:, 0:1], -1.0 / 16, None, ALU.mult, None, ["kmx"], ["expb"])
                if dbg:
                    for nm, src_ap, kk, w_ in [("d_ks", ksT[:], "ksT", 8192), ("d_kc", KC[:], "KC", 512),
                                               ("d_yl", yl_own[:].rearrange("p a b -> p (a b)"), "yl_own", 8192),
                                               ("d_vc", VCx[:].rearrange("p a b c -> p (a b c)"), "VCx", 1568),
                                               ("d_vs", vs[:].rearrange("p a b c -> p (a b c)"), "vs", 8448)]:
                        for o in range(0, w_, 1024):
                            ww = min(1024, w_ - o)
                            cp(stage[0][:, 0:ww], src_ap[:, o:o + ww], [kk], ["stage0"])
                            P.dma(dbgo[nm][:, o:o + ww], stage[0][:, 0:ww], reads=["stage0"])
                P.emit()

            with ExitStack() as c2x:
                wown = sbt(c2x, "wown", [128, 8, 1048], BF16)
                wout = sbt(c2x, "wout", [128, 8, 1024], BF16)
                Ebig = sbt(c2x, "Ebig", [128, 8192], BF16)
                xo = sbt(c2x, "xo", [128, 1024])
                xoT = sbt(c2x, "xoT", [128, 8, 128], BF16)
                QT = sbt(c2x, "QT", [128, 512], BF16)
                Qsq = sbt(c2x, "Qsq", [128, 512], BF16)
                nbS = sbt(c2x, "nbS", [128, 2, 512], BF16)
                gl = sbt(c2x, "gl", [128, 4, 128])
                ylf = sbt(c2x, "ylf", [128, 4, 128])
                ylb = sbt(c2x, "ylb", [128, 4, 128], BF16)
                ylsq = sbt(c2x, "ylsq", [128, 4, 128], BF16)
                gates = sbt(c2x, "gates", [128, 24])
                bg = sbt(c2x, "bg", [128, 24])
                tq = sbt(c2x, "tq", [128, 512])
                fb = sbt(c2x, "fb", [128, 128])
                cmpb = sbt(c2x, "cmpb", [128, 4, 512], BF16)
                causb = sbt(c2x, "causb", [128, 4, 512], BF16)
                winb = sbt(c2x, "winb", [128, 8, 512], BF16)
                wtmp = sbt(c2x, "wtmp", [128, 2, 512])
                PT = [sbt(c2x, "PT0", [128, 512], BF16), sbt(c2x, "PT1", [128, 512], BF16), sbt(c2x, "PT2", [128, 512], BF16)]
                rz = sbt(c2x, "rz", [128, 4])
                coef = sbt(c2x, "coef", [128, 4])
                otmp = sbt(c2x, "otmp", [128, 4, 64])
                ynsa = sbt(c2x, "ynsa", [128, 512])
                ynb = sbt(c2x, "ynb", [128, 512], BF16)
                ynT = sbt(c2x, "ynT", [128, 4, 128], BF16)
                imp = sbt(c2x, "imp", [128, 128])
                imp2 = sbt(c2x, "imp2", [128, 128])
                m8 = sbt(c2x, "m8", [128, 16])
                selb = sbt(c2x, "selb", [128, 128], BF16)
                selT4 = sbt(c2x, "selT4", [128, 4, 128], BF16)
                x1 = sbt(c2x, "x1", [128, 1024])
                expb = kmx[:, 1:2]
                cvf = [sbt(c2x, "cvf0", [128, 512]), sbt(c2x, "cvf1", [128, 512])]
                cvo = [sbt(c2x, "cvo0", [128, 512], BF16), sbt(c2x, "cvo1", [128, 512], BF16)]
                conv_list = [(srct, co, r, h) for (srct, co) in [(peer_u, 0), (peer_v, 1024)] for r in range(128)
                             for h in range(2)]
                conv_pos = [0]

                def conv_some(n):
                    for _ in range(n):
                        if conv_pos[0] >= len(conv_list):
                            return
                        srct, co, r, h = conv_list[conv_pos[0]]
                        k = conv_pos[0] % 2
                        conv_pos[0] += 1
                        P.dma(cvf[k][:], srct[128 * r:128 * r + 128, 512 * h:512 * h + 512], writes=["cvf%d" % k])
                        cp(cvo[k][:], cvf[k][:], ["cvf%d" % k], ["cvo%d" % k], eng='pool')
                        P.dma(uv16[128 * r:128 * r + 128, co + 512 * h:co + 512 * h + 512], cvo[k][:], reads=["cvo%d" % k],
                              writes=["tb16"])

                if upto >= 2:
                    load_w(wown, "wown", wcat, 1048, 1280, gain=gv[:, 0:8])
                    load_w(wout, "wout", w_out, 1024, 0, gain=gv[:, 8:16])
                    P.dma(bg[:], bgate, writes=["bg"])
                    mset(Ebig[:], 1.0, ["Ebig"], eng='pool')
                    P.pool(lambda e: e.affine_select(out=Ebig[:], in_=Ebig[:], pattern=[[1, 8192]], compare_op=ALU.is_ge,
                                                     fill=0.0, base=0, channel_multiplier=-64), ["Ebig"], ["Ebig"])
                    P.pool(lambda e: e.affine_select(out=Ebig[:], in_=Ebig[:], pattern=[[-1, 8192]], compare_op=ALU.is_ge,
                                                     fill=0.0, base=63, channel_multiplier=64), ["Ebig"], ["Ebig"])
                tile_ctr = [0]

                SB = [(A0, "A0"), (A1, "A1"), (c0, "c0")]

                def score_tile(kT, kkT, kvs, kb, kv, extra):
                    n = tile_ctr[0]
                    tile_ctr[0] += 1
                    sp_, ksp = SB[n % 3]
                    pt_, kpt = PT[n % 3], "PT%d" % (n % 3)
                    mm(sp_, kT[kvs, 128 * kb:128 * kb + 128], QT[kvs, :], True, False, [kkT, "QT"], [ksp])
                    for k_, (l_, r_, rd_) in enumerate(extra):
                        mm(sp_, l_, r_, False, k_ == len(extra) - 1, rd_, [ksp])
                    actf(pt_[:], sp_, AF.Exp, [ksp, "expb"], [kpt], bias=expb)
                    return pt_, kpt

                def run_branch(tiles, acc, kacc, imp_rhs=None):
                    prev = None
                    nt_ = len(tiles)
                    for idx in range(nt_ + 1):
                        cur = None
                        if idx < nt_:
                            sa, vrhs, kvk, irhs = tiles[idx]
                            pt_, kpt = score_tile(*sa)
                            cur = (pt_, kpt, vrhs, kvk, irhs, idx)
                        if prev is not None:
                            pt_, kpt, vrhs, kvk, irhs, j = prev
                            for g in range(4):
                                mm(acc[:, 65 * g:65 * g + 65], pt_[:, 128 * g:128 * g + 128], vrhs, j == 0 and g == 0,
                                   j == nt_ - 1 and g == 3, [kpt, kvk], [kacc])
                            if irhs is not None:
                                for g in range(4):
                                    mm(B1[:, 128 * g:128 * g + 128], pt_[:, 128 * g:128 * g + 128], irhs,
                                       j == 0 and g == 0, j == nt_ - 1 and g == 3, [kpt, kvk], ["B1"])
                        prev = cur

                def combine(kv, br, first, acc, kacc):
                    o3 = acc[:, 0:260].rearrange("p (g d) -> p g d", g=4)
                    ts(rz[:], o3[:, :, 64], 1e-30, None, ALU.max, None, [kacc], ["rz"])
                    P.dve(lambda e: e.reciprocal(out=rz[:], in_=rz[:]), ["rz"], ["rz"])
                    tt(coef[:], rz[:], gates[:, 12 * kv + br:12 * kv + 12:3], ALU.mult, ["rz", "gates"], ["coef"])
                    y3 = ynsa[:, 256 * kv:256 * kv + 256].rearrange("p (g d) -> p g d", g=4)
                    cb_ = coef[:].unsqueeze(2).to_broadcast([128, 4, 64])
                    if first:
                        tt(y3, o3[:, :, 0:64], cb_, ALU.mult, [kacc, "coef"], ["ynsa"])
                    else:
                        tt(otmp[:], o3[:, :, 0:64], cb_, ALU.mult, [kacc, "coef"], ["otmp"])
                        tt(y3, y3, otmp[:], ALU.add, ["ynsa", "otmp"], ["ynsa"])

                for i in range(n2 if upto >= 2 else 0):
                    P.dma(xo[:], xown[128 * i:128 * i + 128, :], writes=["xo"])
                    P.dma(tq[:], tq4[i], writes=["tq"])
                    P.dma(fb[:], fbt[i], writes=["fb"])
                    norm_T(xo[:], "xo", xoT[:], "xoT")
                    conv_some((512 + n2 - 1) // n2)
                    for g in range(4):
                        for c in range(8):
                            mm(c0[:, 128 * g:128 * g + 128], wown[:, c, 512 + 128 * g:512 + 128 * g + 128], xoT[:, c, :],
                               g == 0 and c == 0, g == 3 and c == 7, ["wown", "xoT"], ["c0"])
                    actf(QT[:], c0, AF.Copy, ["c0"], ["QT"], scale=0.125)
                    actf(Qsq[:], c0, AF.Square, ["c0"], ["Qsq"])
                    mm(c1, cnegM[:, 0, :], Qsq[:], True, True, ["cnegM", "Qsq"], ["c1"])
                    mm(c2, cnegM[:, 1, :], Qsq[:], True, True, ["cnegM", "Qsq"], ["c2"])
                    cp(nbS[:, 0, :], c1, ["c1"], ["nbS"])
                    cp(nbS[:, 1, :], c2, ["c2"], ["nbS"])
                    for ch in range(4):
                        for c in range(8):
                            mm(c1[:, 128 * ch:128 * ch + 128], wown[:, c, 128 * ch:128 * ch + 128], xoT[:, c, :],
                               ch == 0 and c == 0, ch == 3 and c == 7, ["wown", "xoT"], ["c1"])
                    actf(gl[:].rearrange("p a b -> p (a b)"), c1, AF.Gelu_apprx_tanh, ["c1"], ["gl"])
                    tt(ylf[:], gl[:], yl_own[:, :, 128 * i:128 * i + 128], ALU.mult, ["gl", "yl_own"], ["ylf"])
                    cp(ylb[:].rearrange("p a b -> p (a b)"), ylf[:].rearrange("p a b -> p (a b)"), ["ylf"], ["ylb"], eng='pool')
                    actf(ylsq[:].rearrange("p a b -> p (a b)"), ylf[:].rearrange("p a b -> p (a b)"), AF.Square, ["ylf"], ["ylsq"])
                    for ch in range(4):
                        mm(c2[:, 0:1], ylsq[:, ch, :], ones_b[:, 0:1], ch == 0, ch == 3, ["ylsq", "ones_b"], ["c2"])
                    cp(ssq[:, 1:2], c2[:, 0:1], ["c2"], ["ssq1"])
                    calc_rstd(1, 512)
                    for c in range(8):
                        mm(c2[:, 32:56], xoT[:, c, :], wown[:, c, 1024:1048], c == 0, c == 7, ["wown", "xoT"], ["c2"])
                    tt(gates[:], c2[:, 32:56], bg[:], ALU.add, ["c2", "bg"], ["gates"])
                    actf(gates[:], gates[:], AF.Sigmoid, ["gates"], ["gates"])
                    NT = (32 * i + 31 + 127) // 128
                    for r in range(4):
                        kb = 4 * i + r
                        ts(causb[:, r, :], tq[:], kpos[:, kb:kb + 1], NEG, ALU.is_lt, ALU.mult, ["tq", "ctb"], ["causb"])
                    for kv in range(2):
                        kvs = slice(64 * kv, 64 * kv + 64)
                        for nt in range(NT):
                            ts(wtmp[:, 0, :], tq[:], cend[:, nt:nt + 1], None, ALU.is_lt, None, ["tq", "ctb"], ["wtmp0"])
                            stt(cmpb[:, nt, :], wtmp[:, 0, :], NEG, nbS[:, kv, :], ALU.mult, ALU.add, ["wtmp0", "nbS"], ["cmpb"])
                        for r8 in range(8):
                            kb = 4 * i - 4 + r8
                            if kb < 0:
                                continue
                            ts(wtmp[:, 0, :], tq[:], kpos[:, kb:kb + 1], None, ALU.is_lt, None, ["tq", "ctb"], ["wtmp0"])
                            stt(wtmp[:, 1, :], tq[:], kpos512[:, kb:kb + 1], wtmp[:, 0, :], ALU.is_ge, ALU.add, ["tq", "ctb", "wtmp0"], ["wtmp1"])
                            stt(winb[:, r8, :], wtmp[:, 1, :], NEG, nbS[:, kv, :], ALU.mult, ALU.add, ["wtmp1", "nbS"], ["winb"])
                        tiles = [((KC, "KC", kvs, nt, kv, [(identb[:], cmpb[:, nt, :], ["identb", "cmpb"])]),
                                  VCx[:, nt, kv, 0:65], "VCx", VCx[:, nt, kv, 68:196]) for nt in range(NT)]
                        run_branch(tiles, B0, "B0")
                        combine(kv, 0, True, B0, "B0")
                        ts(imp[:], B1[:, 0:128], rz[:, 0:1], None, ALU.mult, None, ["B1", "rz"], ["imp"])
                        for g in range(1, 4):
                            stt(imp[:], B1[:, 128 * g:128 * g + 128], rz[:, g:g + 1], imp[:], ALU.mult, ALU.add,
                                ["B1", "rz", "imp"], ["imp"])
                        tt(imp[:], imp[:], fb[:], ALU.add, ["imp", "fb"], ["imp"])
                        P.dve(lambda e: e.max(out=m8[:, 0:8], in_=imp[:]), ["imp"], ["m8"])
                        P.dve(lambda e: e.match_replace(out=imp2[:], in_to_replace=m8[:, 0:8], in_values=imp[:],
                                                        imm_value=-3e9), ["imp", "m8"], ["imp2"])
                        P.dve(lambda e: e.max(out=m8[:, 8:16], in_=imp2[:]), ["imp2"], ["m8"])
                        ts(imp2[:], imp[:], m8[:, 15:16], None, ALU.is_ge, None, ["imp", "m8"], ["imp2"])
                        stt(imp2[:], imp[:], -1.0, imp2[:], ALU.is_gt, ALU.mult, ["imp", "imp2"], ["imp2"])
                        ts(selb[:], imp2[:], 1.0, -NEG, ALU.subtract, ALU.mult, ["imp2"], ["selb"])
                        kbs = [4 * i - 4 + r8 for r8 in range(8) if 4 * i - 4 + r8 >= 0]
                        tiles = [((kwT, "kwT", kvs, kb, kv, [(identb[:], winb[:, kb - (4 * i - 4), :], ["identb", "winb"])]),
                                  vw[:, kb, kv, 0:65], "vw", None) for kb in kbs]
                        run_branch(tiles, c1, "c1")
                        combine(kv, 2, False, c1, "c1")
                        trp(ptb[:, 0:128], selb[:], identb[:], ["selb", "identb"], ["ptb"])
                        tt(selT4[:], ptb[:, 0:128].unsqueeze(1).to_broadcast([128, 4, 128]),
                           nbS[:, kv, :].rearrange("p (g q) -> p g q", g=4), ALU.add, ["ptb", "nbS"], ["selT4"])
                        sel_rhs = selT4[:].rearrange("p a b -> p (a b)")
                        nkb = 4 * i + 4
                        tiles = []
                        for kb in range(nkb):
                            extra = [(Ebig[:, 128 * kb:128 * kb + 128], sel_rhs, ["Ebig", "selT4"])]
                            if kb >= 4 * i:
                                extra.append((identb[:], causb[:, kb - 4 * i, :], ["identb", "causb"]))
                            tiles.append(((ksT, "ksT", kvs, kb, kv, extra), vs[:, kb, kv, 0:65], "vs", None))
                        run_branch(tiles, c2, "c2")
                        combine(kv, 1, False, c2, "c2")
                    if dbg:
                        P.dma(dbgo["d_yn"][128 * i:128 * i + 128, :], ynsa[:], reads=["ynsa"])
                    actf(ynb[:], ynsa[:], AF.Square, ["ynsa"], ["ynb", "ssq2"], accum=ssq[:, 2:3])
                    calc_rstd(2, 512)
                    cp(ynb[:], ynsa[:], ["ynsa"], ["ynb"])
                    for c in range(4):
                        trp(ptb[:, 128 * c:128 * c + 128], ynb[:, 128 * c:128 * c + 128], identb[:], ["ynb", "identb"], ["ptb"])
                    cp(ynT[:], ptb[:, 0:512].rearrange("p (c t) -> p c t", c=4), ["ptb"], ["ynT"])
                    for hh, (pl, kpl, pn, kpn) in enumerate([(B0, "B0", c0, "c0"), (B1, "B1", c1, "c1")]):
                        cs = slice(512 * hh, 512 * hh + 512)
                        for ch in range(4):
                            mm(pl, ylb[:, ch, :], wout[:, ch, cs], ch == 0, ch == 3, ["ylb", "wout"], [kpl])
                        for ch in range(4):
                            mm(pn, ynT[:, ch, :], wout[:, 4 + ch, cs], ch == 0, ch == 3, ["ynT", "wout"], [kpn])
                        stt(x1[:, cs], pl, rstd[:, 1:2], xo[:, cs], ALU.mult, ALU.add, [kpl, "rstd1", "xo"], ["x1"])
                        stt(x1[:, cs], pn, rstd[:, 2:3], x1[:, cs], ALU.mult, ALU.add, [kpn, "rstd2", "x1"], ["x1"])
                    P.dma(x1d[128 * i:128 * i + 128, :], x1[:], reads=["x1"], writes=["x1d"])
                conv_some(512)
                P.emit()

        with ExitStack() as c3x:
            wmq = sbt(c3x, "wmq", [128, 8, 1024], BF16)
            wmo = sbt(c3x, "wmo", [128, 8, 1024], BF16)
            wpq = sbt(c3x, "wpq", [128, 8, 2048], BF16)
            KmT = sbt(c3x, "KmT", [128, 8, 256], BF16)
            Vm = sbt(c3x, "Vm", [128, 2, 1024], BF16)
            sk = sbt(c3x, "sk", [128, 16, 128], BF16)
            gff = sbt(c3x, "gff", [128, 1024])
            gfi = sbt(c3x, "gfi", [128, 1024])
            memT = sbt(c3x, "memT", [128, 8, 256], BF16)
            kmm = sbt(c3x, "kmm", [128, 4])
            xa = sbt(c3x, "xa", [128, 1024])
            xb2 = sbt(c3x, "xb2", [128, 1024])
            xnf = sbt(c3x, "xnf", [128, 1024])
            xT = sbt(c3x, "xT", [128, 8, 128], BF16)
            qmT = sbt(c3x, "qmT", [128, 8, 128], BF16)
            qmsq = sbt(c3x, "qmsq", [128, 8, 128], BF16)
            negbm = sbt(c3x, "negbm", [1, 512], BF16)
            PTm = [sbt(c3x, "PTm0", [128, 512], BF16), sbt(c3x, "PTm1", [128, 512], BF16)]
            rzb = sbt(c3x, "rzb", [128, 512])
            oT = sbt(c3x, "oT", [128, 8, 128], BF16)
            qpT = sbt(c3x, "qpT", [128, 16, 128], BF16)
            s_sb = sbt(c3x, "s_sb", [128, 16, 128])
            s2 = sbt(c3x, "s2", [128, 128])
            v16 = sbt(c3x, "v16", [128, 16, 16])
            i16 = sbt(c3x, "i16", [128, 16, 16], U32)
            i16f = sbt(c3x, "i16f", [128, 16, 16])
            cand = sbt(c3x, "cand", [128, 8, 256])
            cand2 = sbt(c3x, "cand2", [128, 256])
            cid = sbt(c3x, "cid", [128, 8, 256])
            top = sbt(c3x, "top", [128, 8, 16])
            eid = sbt(c3x, "eid", [128, 128])
            pos = sbt(c3x, "pos", [128, 8, 16], U32)
            posf = sbt(c3x, "posf", [128, 128])
            ai = sbt(c3x, "ai", [128, 128], I32)
            af = sbt(c3x, "af", [128, 128])
            bfl = sbt(c3x, "bfl", [128, 128])
            e1 = sbt(c3x, "e1", [128, 128])
            e2 = sbt(c3x, "e2", [128, 128])
            gw = sbt(c3x, "gw", [128, 8, 16])
            gsum = sbt(c3x, "gsum", [128, 8])
            eidT = sbt(c3x, "eidT", [128, 128], I32)
            gT = sbt(c3x, "gT", [128, 128])
            eidT2 = sbt(c3x, "eidT2", [128, 128], I32)
            gT2 = sbt(c3x, "gT2", [128, 128])
            NB = 8
            UVg = [sbt(c3x, "UVg%d" % k, [128, 2048], BF16) for k in range(NB)]
            gh = sbt(c3x, "gh", [128, 128])
            hTa = sbt(c3x, "hTa", [128, 128])
            hTb = sbt(c3x, "hTb", [128, 128])
            jk = [sbt(c3x, "jk0", [128, 512], BF16), sbt(c3x, "jk1", [128, 512], BF16)]
            selt = [sbt(c3x, "selt0", [128, 128], BF16), sbt(c3x, "selt1", [128, 128], BF16)]
            Dt = [sbt(c3x, "Dt0", [128, 128], BF16), sbt(c3x, "Dt1", [128, 128], BF16)]
            yo = xnf

            if upto >= 3:
                load_w(wmq, "wmq", w_mq, 1024, 0, gain=gv[:, 16:24])
                load_w(wpq, "wpq", w_pq, 2048, 0)
                load_small(sk[:].rearrange("p a b -> p (a b)"), "sk", skT, 2048)
                P.dma(gff[:], gffn.partition_broadcast(128), writes=["gain"])
                P.dma(gfi[:], gfin.partition_broadcast(128), writes=["gfi"])
                for mb in range(2):
                    P.dma(xa[:], memb[128 * mb:128 * mb + 128, :], writes=["xa"])
                    norm_T(xa[:], "xa", memT[:, :, 128 * mb:128 * mb + 128], "memT")
                P.emit()
                load_w(wmo, "wmo", w_mk, 1024, 0, gain=gv[:, 24:32])
                for oc in range(8):
                    for c in range(8):
                        mm(c0[:, 0:256], wmo[:, c, 128 * oc:128 * oc + 128], memT[:, c, :], c == 0, c == 7, ["wmo", "memT"], ["c0"])
                    cp(KmT[:, oc, :], c0[:, 0:256], ["c0"], ["KmT"])
                sqm = sbt(c3x, "sqm", [128, 8, 256], BF16)
                actf(sqm[:].rearrange("p a b -> p (a b)"), KmT[:].rearrange("p a b -> p (a b)"), AF.Square, ["KmT"], ["sqm"])
                for hm in range(4):
                    for dc in range(2):
                        mm(c1[:, 0:256], ones_b[:], sqm[:, 2 * hm + dc, :], dc == 0, dc == 1, ["ones_b", "sqm"], ["c1"])
                    P.dve((lambda hm: lambda e: e.tensor_reduce(out=kmm[:, hm:hm + 1], in_=c1[:, 0:256], axis=AX.X, op=ALU.max))(hm),
                          ["c1"], ["kmm"])
                P.dve(lambda e: e.tensor_reduce(out=kmm[:, 0:1], in_=kmm[:, 0:4], axis=AX.X, op=ALU.max), ["kmm"], ["kmm"])
                ts(kmm[:, 1:2], kmm[:, 0:1], -1.0 / 32, None, ALU.mult, None, ["kmm"], ["expbm"])
                expbm = kmm[:, 1:2]
                P.emit()
                load_w(wmo, "wmo", w_mv, 1024, 0, gain=gv[:, 24:32])
                for mb in range(2):
                    for hh in range(2):
                        for c in range(8):
                            mm(c0, memT[:, c, 128 * mb:128 * mb + 128], wmo[:, c, 512 * hh:512 * hh + 512], c == 0, c == 7,
                               ["wmo", "memT"], ["c0"])
                        cp(Vm[:, mb, 512 * hh:512 * hh + 512], c0, ["c0"], ["Vm"])
                P.emit()
                load_w(wmo, "wmo", w_mo, 1024, 0)

            sqmf = sqm[:].rearrange("p a b -> p (a b)") if upto >= 3 else None
            n3e = n3 if upto >= 3 else 0
            AB2 = [(A0, "A0"), (A1, "A1")]

            def bufs(i):
                p2 = i % 2
                xb2_, kxb2 = (xb2[:], "xb2") if p2 == 0 else (stage[1][:], "stage1")
                xnb_, kxnb = (xnb[:], "xnbP0") if p2 == 0 else (sqmf[:, 0:1024], "xnbP1")
                eidT_, keid = (eidT, "eidT0") if p2 == 0 else (eidT2, "eidT1")
                gT_, kgT = (gT, "gT0") if p2 == 0 else (gT2, "gT1")
                return xb2_, kxb2, xnb_, kxnb, eidT_, keid, gT_, kgT

            def stage_S(i):
                xb2_, kxb2, xnb_, kxnb, eidT_, keid, gT_, kgT = bufs(i)
                P.dma(xa[:], x1d[128 * i:128 * i + 128, :], reads=["x1d"], writes=["xa"])
                norm_T(xa[:], "xa", xT[:], "xT", xbo=(sqmf[:, 1024:2048], "xnbS"))
                for oc in range(8):
                    pt_, kp = AB2[oc // 4]
                    for c in range(8):
                        mm(pt_[:, 128 * (oc % 4):128 * (oc % 4) + 128], wmq[:, c, 128 * oc:128 * oc + 128], xT[:, c, :],
                           oc % 4 == 0 and c == 0, oc % 4 == 3 and c == 7, ["wmq", "xT"], [kp])
                for hh, (pt_, kp) in enumerate(AB2):
                    actf(qmT[:, 4 * hh:4 * hh + 4, :].rearrange("p a b -> p (a b)"), pt_, AF.Copy, [kp], ["qmT"], scale=1.0 / 16)
                    actf(qmsq[:, 4 * hh:4 * hh + 4, :].rearrange("p a b -> p (a b)"), pt_, AF.Square, [kp], ["qmsq"])
                for hm in range(4):
                    for dc in range(2):
                        mm(c2[0:1, 128 * hm:128 * hm + 128], cneg[:, 2:3], qmsq[:, 2 * hm + dc, :], hm == 0 and dc == 0,
                           hm == 3 and dc == 1, ["cneg", "qmsq"], ["c2"])
                cp(negbm[0:1, :], c2[0:1, :], ["c2"], ["negbm"])
                for mc in range(2):
                    sp_, ksp = AB2[mc]
                    for hm in range(4):
                        for dc in range(2):
                            mm(sp_[:, 128 * hm:128 * hm + 128], KmT[:, 2 * hm + dc, 128 * mc:128 * mc + 128],
                               qmT[:, 2 * hm + dc, :], hm == 0 and dc == 0, False, ["KmT", "qmT"], [ksp])
                    mm(sp_, ones_b[0:1, :], negbm[0:1, :], False, True, ["ones_b", "negbm"], [ksp])
                    actf(PTm[mc][:], sp_, AF.Exp, [ksp, "expbm"], ["PTm%d" % mc], bias=expbm)
                for oc in range(8):
                    hm = oc // 2
                    pt_, kp = AB2[oc // 4]
                    for mc in range(2):
                        mm(pt_[:, 128 * (oc % 4):128 * (oc % 4) + 128], Vm[:, mc, 128 * oc:128 * oc + 128],
                           PTm[mc][:, 128 * hm:128 * hm + 128], oc % 4 == 0 and mc == 0, oc % 4 == 3 and mc == 1,
                           ["Vm", "PTm%d" % mc], [kp])
                for hm in range(4):
                    for mc in range(2):
                        mm(c2[:, 128 * hm:128 * hm + 128], ones_b[:], PTm[mc][:, 128 * hm:128 * hm + 128],
                           hm == 0 and mc == 0, hm == 3 and mc == 1, ["ones_b", "PTm%d" % mc], ["c2"])
                P.dve(lambda e: e.reciprocal(out=rzb[:], in_=c2), ["c2"], ["rzb"])
                for oc in range(8):
                    hm = oc // 2
                    pt_, kp = AB2[oc // 4]
                    tt(oT[:, oc, :], pt_[:, 128 * (oc % 4):128 * (oc % 4) + 128], rzb[:, 128 * hm:128 * hm + 128], ALU.mult,
                       [kp, "rzb"], ["oT"])
                for hh, (pt_, kp) in enumerate(AB2):
                    cs = slice(512 * hh, 512 * hh + 512)
                    for oc in range(8):
                        mm(pt_, oT[:, oc, :], wmo[:, oc, cs], oc == 0, oc == 7, ["oT", "wmo"], [kp])
                    tt(xb2_[:, cs], pt_, xa[:, cs], ALU.add, [kp, "xa"], [kxb2])
                if dbg:
                    P.dma(dbgo["d_x2"][128 * i:128 * i + 128, :], xb2_, reads=[kxb2])
                norm_T(xb2_, kxb2, xT[:], "xT", gain=gff[:], xnf=xnf[:], xbo=(xnb_, kxnb))
                for half in range(2):
                    for h8 in range(8):
                        hc = 8 * half + h8
                        pt_, kp = AB2[h8 // 4]
                        for c in range(8):
                            mm(pt_[:, 128 * (hc % 4):128 * (hc % 4) + 128], wpq[:, c, 128 * hc:128 * hc + 128], xT[:, c, :],
                               hc % 4 == 0 and c == 0, hc % 4 == 3 and c == 7, ["wpq", "xT"], [kp])
                    for q2, (pt_, kp) in enumerate(AB2):
                        q4 = 2 * half + q2
                        cp(qpT[:, 4 * q4:4 * q4 + 4, :].rearrange("p a b -> p (a b)"), pt_, [kp], ["qpT"],
                           eng='act' if q4 % 2 else 'dve')
                for half in range(2):
                    for h8 in range(8):
                        hc = 8 * half + h8
                        pt_, kp = AB2[h8 // 4]
                        mm(pt_[:, 128 * (hc % 4):128 * (hc % 4) + 128], qpT[:, hc, :], sk[:, hc, :], hc % 4 == 0, hc % 4 == 3,
                           ["qpT", "sk"], [kp])
                    for q2, (pt_, kp) in enumerate(AB2):
                        q4 = 2 * half + q2
                        cp(s_sb[:, 4 * q4:4 * q4 + 4, :].rearrange("p a b -> p (a b)"), pt_, [kp], ["s_sb"],
                           eng='act' if q4 % 2 else 'dve')
                for hc in range(16):
                    sv = s_sb[:, hc, :]
                    P.dve((lambda hc, sv: lambda e: e.max(out=v16[:, hc, 0:8], in_=sv))(hc, sv), ["s_sb"], ["v16"])
                    P.dve((lambda hc, sv: lambda e: e.max_index(out=i16[:, hc, 0:8], in_max=v16[:, hc, 0:8], in_values=sv))(hc, sv),
                          ["s_sb", "v16"], ["i16"])
                    P.dve((lambda hc, sv: lambda e: e.match_replace(out=s2[:], in_to_replace=v16[:, hc, 0:8], in_values=sv,
                                                                     imm_value=-1e30))(hc, sv), ["s_sb", "v16"], ["s2"])
                    P.dve((lambda hc: lambda e: e.max(out=v16[:, hc, 8:16], in_=s2[:]))(hc), ["s2"], ["v16"])
                    P.dve((lambda hc: lambda e: e.max_index(out=i16[:, hc, 8:16], in_max=v16[:, hc, 8:16], in_values=s2[:]))(hc),
                          ["s2", "v16"], ["i16"])
                cp(i16f[:], i16[:], ["i16"], ["i16f"])
                for h in range(8):
                    tt(cand[:, h, :].rearrange("p (a b) -> p a b", a=16),
                       v16[:, 2 * h, :].unsqueeze(2).to_broadcast([128, 16, 16]),
                       v16[:, 2 * h + 1, :].unsqueeze(1).to_broadcast([128, 16, 16]), ALU.add, ["v16"], ["cand"])
                for h in range(8):
                    cv = cand[:, h, :]
                    P.dve((lambda h, cv: lambda e: e.max(out=top[:, h, 0:8], in_=cv))(h, cv), ["cand"], ["top"])
                    P.dve((lambda h, cv: lambda e: e.max_index(out=pos[:, h, 0:8], in_max=top[:, h, 0:8], in_values=cv))(h, cv),
                          ["cand", "top"], ["pos"])
                    P.dve((lambda h, cv: lambda e: e.match_replace(out=cand2[:], in_to_replace=top[:, h, 0:8], in_values=cv,
                                                                    imm_value=-1e30))(h, cv), ["cand", "top"], ["cand2"])
                    P.dve((lambda h: lambda e: e.max(out=top[:, h, 8:16], in_=cand2[:]))(h), ["cand2"], ["top"])
                    P.dve((lambda h: lambda e: e.max_index(out=pos[:, h, 8:16], in_max=top[:, h, 8:16], in_values=cand2[:]))(h),
                          ["cand2", "top"], ["pos"])
                cp(posf[:], pos[:].rearrange("p a b -> p (a b)"), ["pos"], ["posf"])
                ts(ai[:], posf[:], -7.5, 0.0625, ALU.add, ALU.mult, ["posf"], ["ai"])
                cp(af[:], ai[:], ["ai"], ["af"])
                stt(bfl[:], af[:], -16.0, posf[:], ALU.mult, ALU.add, ["af", "posf"], ["bfl"])
                eq3 = cid[:].rearrange("p h (k a) -> p (h k) a", a=16)
                eq4 = cid[:].rearrange("p h (k a) -> p h k a", a=16)
                iob = ctb[:, 132:148].unsqueeze(1).to_broadcast([128, 128, 16])
                for (src_, par, dst_) in [(af, 0, e1), (bfl, 1, e2)]:
                    tt(eq3, src_[:].unsqueeze(2).to_broadcast([128, 128, 16]), iob, ALU.is_equal, [("af" if par == 0 else "bfl"), "ctb"], ["cid"])
                    tt(eq4, eq4, i16f[:, par:16:2, :].unsqueeze(2).to_broadcast([128, 8, 16, 16]), ALU.mult, ["cid", "i16f"], ["cid"])
                    P.dve((lambda dst_: lambda e: e.tensor_reduce(out=dst_[:], in_=eq3, axis=AX.X, op=ALU.add))(dst_), ["cid"],
                          ["e%d" % par])
                stt(eid[:], e1[:], 128.0, e2[:], ALU.mult, ALU.add, ["e0", "e1"], ["eid"])
                tt(gw[:], top[:], top[:, :, 0:1].to_broadcast([128, 8, 16]), ALU.subtract, ["top"], ["gw"])
                actf(gw[:].rearrange("p a b -> p (a b)"), gw[:].rearrange("p a b -> p (a b)"), AF.Exp, ["gw"], ["gw"])
                P.dve(lambda e: e.tensor_reduce(out=gsum[:], in_=gw[:], axis=AX.X, op=ALU.add), ["gw"], ["gsum"])
                P.dve(lambda e: e.reciprocal(out=gsum[:], in_=gsum[:]), ["gsum"], ["gsum"])
                tt(gw[:], gw[:], gsum[:].unsqueeze(2).to_broadcast([128, 8, 16]), ALU.mult, ["gw", "gsum"], ["gw"])
                ts(eid[:], eid[:], 16383.0, 0.0, ALU.min, ALU.max, ["eid"], ["eid"])
                trp(A0[:, 0:128], eid[:], identf[:], ["eid", "identf"], ["A0"])
                cp(eidT_[:], A0[:, 0:128], ["A0"], [keid])
                trp(A1[:, 0:128], gw[:].rearrange("p a b -> p (a b)"), identf[:], ["gw", "identf"], ["A1"])
                cp(gT_[:], A1[:, 0:128], ["A1"], [kgT])

            LAG = 3

            def token_loop(i, q):
                xb2_, kxb2, xnb_, kxnb, eidT_, keid, gT_, kgT = bufs(i)
                (pa, ka), (pb, kb_) = (c0, "c0"), (c1, "c1")
                nsteps = 128 + LAG
                caps = {'pe': 10, 'dve': 4, 'act': 3, 'pool': 2, 'sp': 2}
                wstep, weng, rstep, load = {}, {}, {}, {}
                lastst = {e_: 0 for e_ in ENGS}
                buckets = [[] for _ in range(nsteps)]
                tail = []
                for item in q:
                    eng_, _fn, reads_, writes_, _dma = item
                    s_ = lastst[eng_]
                    for k_ in tuple(reads_) + tuple(writes_):
                        if k_ in wstep:
                            s_ = max(s_, wstep[k_] + (1 if weng[k_] != eng_ else 0))
                    for k_ in writes_:
                        for e2_, st_ in rstep.get(k_, {}).items():
                            s_ = max(s_, st_ + (1 if e2_ != eng_ else 0))
                    while load.get((s_, eng_), 0) >= caps[eng_]:
                        s_ += 1
                    load[(s_, eng_)] = load.get((s_, eng_), 0) + 1
                    lastst[eng_] = s_
                    for k_ in writes_:
                        wstep[k_] = s_
                        weng[k_] = eng_ if not _dma else 'dma'
                        rstep[k_] = {}
                    for k_ in reads_:
                        if k_ not in writes_:
                            d_ = rstep.setdefault(k_, {})
                            d_[eng_] = max(d_.get(eng_, 0), s_)
                    (buckets[s_] if s_ < nsteps else tail).append(item)

                def u_side(t):
                    u = t % NB
                    P.op('pool', (lambda t, u: lambda e: e.indirect_dma_start(
                        out=UVg[u][:], out_offset=None, in_=uv16,
                        in_offset=bass.IndirectOffsetOnAxis(ap=eidT_[:, t:t + 1], axis=0)))(t, u),
                        [keid], ["UVg%d" % u], dma=True)
                    s2_ = t % 2
                    actf(selt[s2_][:], ones_b[:], AF.Copy, ["ones_b", "identf"], ["selt%d" % s2_], scale=identf[:, t:t + 1])
                    mm(pa, selt[s2_][:], xnb_[:, 0:512], True, True, ["selt%d" % s2_, kxnb], [ka])
                    mm(pb, selt[s2_][:], xnb_[:, 512:1024], True, True, ["selt%d" % s2_, kxnb], [kb_])
                    stt(jk[0][:], UVg[u][:, 0:512], 1.0, pa, ALU.mult, ALU.mult, ["UVg%d" % u, ka], ["ha%d" % t, "jk0"],
                        accum=hTa[:, t:t + 1])
                    stt(jk[1][:], UVg[u][:, 512:1024], 1.0, pb, ALU.mult, ALU.mult, ["UVg%d" % u, kb_], ["hb%d" % t, "jk1"],
                        accum=hTb[:, t:t + 1])

                def v_pre(t):
                    actf(gh[:, t:t + 1], hTa[:, t:t + 1], AF.Gelu_apprx_tanh, ["ha%d" % t, "hb%d" % t], ["gh%d" % t],
                         bias=hTb[:, t:t + 1])
                    d2 = t % 2
                    actf(gh[:, t:t + 1], gh[:, t:t + 1], AF.Copy, ["gh%d" % t, kgT], ["gh%d" % t], scale=gT_[:, t:t + 1])
                    actf(Dt[d2][:], Rsel[:, 127 - t:255 - t], AF.Copy, ["Rsel", "gh%d" % t], ["Dt%d" % d2],
                         scale=gh[:, t:t + 1])

                def v_mm(t):
                    u = t % NB
                    d2 = t % 2
                    mm(B0, Dt[d2][:], UVg[u][:, 1024:1536], t == 0, t == 127, ["Dt%d" % d2, "UVg%d" % u], ["B0"])
                    mm(B1, Dt[d2][:], UVg[u][:, 1536:2048], t == 0, t == 127, ["Dt%d" % d2, "UVg%d" % u], ["B1"])

                for step in range(128 + LAG):
                    P.drain(buckets[step], len(buckets[step]))
                    if step >= LAG:
                        v_pre(step - LAG)
                    if step < 128:
                        u_side(step)
                    if step >= LAG:
                        v_mm(step - LAG)
                P.drain(tail, len(tail))
                tt(xa[:], B[:, :], xb2_, ALU.add, ["B0", "B1", kxb2], ["xa"])
                actf(cid[:].rearrange("p a b -> p (a b)")[:, 0:1024], xa[:], AF.Square, ["xa"], ["cid", "ssq3"], accum=ssq[:, 3:4])
                calc_rstd(3, 1024)
                stt(yo[:], xa[:], rstd[:, 3:4], gfi[:], ALU.mult, ALU.mult, ["xa", "rstd3", "gfi"], ["xnf"])
                P.dma(yout[128 * i:128 * i + 128, :], yo[:], reads=["xnf"])

            if n3e > 0:
                stage_S(0)
            for i in range(n3e):
                q = []
                if i + 1 < n3e:
                    P.capture_begin()
                    stage_S(i + 1)
                    q = P.capture_end()
                token_loop(i, q)
            P.emit()
    return nc


_NC_CACHE = {}


def _prep_shared(inp):
    f = np.float32
    w_in = np.asarray(inp['w_in'][0], f)
    offs = np.cumsum([0, 512, 512, 512, 128, 128, 128, 128, 128, 128, 24])
    x_lru, gate, q, k_c, v_c, k_s, v_s, k_w, v_w, g_raw = [w_in[:, offs[j]:offs[j + 1]] for j in range(10)]
    qp = q.reshape(1024, 2, 4, 64).transpose(0, 2, 1, 3).reshape(1024, 512)
    wcat = np.ascontiguousarray(np.concatenate([x_lru, k_c, v_c, k_s, k_w, v_s, v_w, gate, qp, g_raw], axis=1))

    def pc(v):
        return np.asarray(v, f).reshape(8, 128).T

    gout = np.concatenate([np.asarray(inp['g_out_lru'][0], f), np.asarray(inp['g_out_nsa'][0], f)])
    gvec = np.zeros((128, 48), f)
    gvec[:, 0:8] = pc(inp['g_mix'][0])
    gvec[:, 8:16] = pc(gout)
    gvec[:, 16:24] = pc(inp['g_mem_q'][0])
    gvec[:, 24:32] = pc(inp['g_mem_kv'][0])
    bgate = np.tile(np.asarray(inp['b_gate'][0], f)[None, :], (128, 1))
    lrup = np.zeros((128, 36), f)
    cwv = np.asarray(inp['conv_w'][0], f)
    lrup[:, 0:16] = cwv.reshape(4, 4, 128).transpose(2, 1, 0).reshape(128, 16)
    lrup[:, 16:20] = np.asarray(inp['conv_b'][0], f).reshape(4, 128).T
    lrup[:, 20:24] = np.asarray(inp['b_rg_a'][0], f).reshape(4, 128).T
    lrup[:, 24:28] = np.asarray(inp['b_rg_i'][0], f).reshape(4, 128).T
    lrup[:, 28:32] = np.asarray(inp['lam'][0], f).reshape(4, 128).T

    def bd(w):
        o = np.zeros((128, 4, 128), f)
        for ch in range(4):
            o[0:64, ch, 0:64] = w[2 * ch]
            o[64:128, ch, 64:128] = w[2 * ch + 1]
        return o.reshape(128, 512)

    wabd = bd(np.asarray(inp['w_rg_a'][0], f))
    wibd = bd(np.asarray(inp['w_rg_i'][0], f))

    def w1bd(w1):
        w = np.asarray(w1, f).reshape(32, 64, 64)
        o = np.zeros((128, 32, 128), f)
        o[0:64, :, 0:64] = w.transpose(1, 0, 2)
        o[64:128, :, 64:128] = w.transpose(1, 0, 2)
        return o.reshape(128, 4096)

    def w2bd(w2):
        o = np.zeros((128, 128), f)
        o[0:64, 0:64] = w2
        o[64:128, 64:128] = w2
        return o

    def posT(p):
        return np.ascontiguousarray(np.tile(np.asarray(p, f).T, (2, 1)))

    npr = np.arange(512)
    ci = (npr - 1) * 16
    bj = np.arange(128) * 64
    ovl = ((ci[:, None] < bj[None, :] + 64) & (ci[:, None] + 32 > bj[None, :])).astype(f)
    ovl[0, :] = 0.0
    ctab = np.zeros((128, 148), f)
    ctab[:, 132:148] = np.arange(16, dtype=f)[None, :]
    p = np.arange(128)
    for nt in range(4):
        ctab[:, nt] = 16.0 * (128 * nt + p) + 15.0
    ctab[0, 0] = 1e9
    for kb in range(64):
        ctab[:, 4 + kb] = 128.0 * kb + p
        ctab[:, 68 + kb] = 128.0 * kb + p + 512.0
    skT = np.ascontiguousarray(np.asarray(inp['sub_keys'][0], f).reshape(16, 128, 128).transpose(2, 0, 1).reshape(128, 2048))
    sh = dict(
        wcat=wcat, gvec=gvec, bgate=bgate, wabd=wabd, wibd=wibd,
        w1k=w1bd(inp['cmp_w1_k'][0]), w1v=w1bd(inp['cmp_w1_v'][0]),
        w2k=w2bd(np.asarray(inp['cmp_w2_k'][0], f)), w2v=w2bd(np.asarray(inp['cmp_w2_v'][0], f)),
        posk=posT(inp['cmp_pos_k'][0]), posv=posT(inp['cmp_pos_v'][0]), ovl=ovl, ctab=ctab,
        w_out=np.ascontiguousarray(np.asarray(inp['w_out'][0], f)),
        w_mq=np.ascontiguousarray(np.asarray(inp['w_mq'][0], f)),
        w_mk=np.ascontiguousarray(np.asarray(inp['w_mk'][0], f)),
        w_mv=np.ascontiguousarray(np.asarray(inp['w_mv'][0], f)),
        w_mo=np.ascontiguousarray(np.asarray(inp['w_mo'][0], f)),
        w_pq=np.ascontiguousarray(np.asarray(inp['w_pq'][0], f)),
        skT=skT,
        gffn=np.asarray(inp['g_ffn'][0], f).reshape(1, 1024),
        gfin=np.asarray(inp['g_final'], f).reshape(1, 1024),
        peer_u=np.ascontiguousarray(np.asarray(inp['peer_u'][0], f)),
        peer_v=np.ascontiguousarray(np.asarray(inp['peer_v'][0], f)),
    )
    return sh, lrup


def _own_idx(c):
    return np.concatenate([128 * (4 * i + c) + np.arange(128) for i in range(16)])


def _prep_core(inp, sh, lrup, core):
    f = np.float32
    b, c = core // 4, core % 4
    own = _own_idx(c)
    x = np.asarray(inp['x'], f)
    m = dict(sh)
    m['xfull'] = np.ascontiguousarray(x[b])
    m['xown'] = np.ascontiguousarray(x[b][own])
    m['memb'] = np.ascontiguousarray(np.asarray(inp['mem'], f)[b])
    lp = lrup.copy()
    lp[:, 32 + c] = 1.0
    m['lrup'] = lp
    tq = own.reshape(16, 128).astype(f)
    m['tq4'] = np.ascontiguousarray(np.broadcast_to(np.tile(tq, (1, 4))[:, None, :], (16, 128, 512)))
    cur = (own // 64).reshape(16, 128)
    j = np.arange(128)[None, None, :]
    cu = cur[:, :, None]
    fbt = np.where(j > cu, -1e9, 0.0).astype(f)
    forced = ((j == 0) | (j == cu) | (j == cu - 1)) & (j <= cu)
    fbt = np.where(forced, 1e6 * (1.0 + j), fbt).astype(f)
    m['fbt'] = np.ascontiguousarray(fbt)
    return m


def kernel(**inputs):
    if 'nc' not in _NC_CACHE:
        _NC_CACHE['nc'] = build(False)
    nc = _NC_CACHE['nc']
    sh, lrup = _prep_shared(inputs)
    in_maps = [_prep_core(inputs, sh, lrup, core) for core in range(8)]
    res = run_bass_kernel_spmd(nc, in_maps, core_ids=list(range(8)))
    out = np.zeros((2, 8192, 1024), np.float32)
    for core in range(8):
        b, c = core // 4, core % 4
        out[b, _own_idx(c)] = res.results[core]["yout"]
    return out
```

```python
import numpy as np
import concourse.bass as bass
import concourse.mybir as mybir
from concourse.bass_utils import run_bass_kernel_spmd
from contextlib import ExitStack

F32 = mybir.dt.float32
BF16 = mybir.dt.bfloat16
U32 = mybir.dt.uint32
I32 = mybir.dt.int32
AF = mybir.ActivationFunctionType
ALU = mybir.AluOpType
AX = mybir.AxisListType

ENGS = ['pe', 'dve', 'act', 'pool', 'sp']
NDMA = 24
NSW = 8
SAME_ENG_SYNC = {'pe': False, 'dve': True, 'act': True, 'pool': True, 'sp': True}
NEG = -30000.0
_CUT = 99
EPS = 1e-6


class Prog:
    def __init__(self, nc, ctx):
        self.nc = nc
        self.ops = {e: [] for e in ENGS}
        self.cnt = {e: 0 for e in ENGS}
        self.sems = {e: ctx.enter_context(nc.semaphore("s_" + e)) for e in ENGS}
        self.dsem = [ctx.enter_context(nc.semaphore("d%d" % i)) for i in range(NDMA + NSW)]
        self.dcnt = [0] * (NDMA + NSW)
        self.drr = 0
        self.srr = 0
        self.last_w = {}
        self.readers = {}
        self.seen = {e: {} for e in ENGS}
        self.defer = None

    def capture_begin(self):
        self.defer = []

    def capture_end(self):
        q, self.defer = self.defer, None
        return q

    def drain(self, q, n):
        for _ in range(min(n, len(q))):
            self.op(*q.pop(0))

    def op(self, eng, fn, reads=(), writes=(), dma=False):
        if self.defer is not None:
            self.defer.append((eng, fn, tuple(reads), tuple(writes), dma))
            return None
        deps = {}

        def add(tok):
            if tok is None:
                return
            sem, val, teng, sid = tok
            if teng == eng and not SAME_ENG_SYNC[eng]:
                return
            if sid not in deps or deps[sid][1] < val:
                deps[sid] = tok

        for k in reads:
            add(self.last_w.get(k))
        for k in writes:
            add(self.last_w.get(k))
            for t in self.readers.get(k, {}).values():
                add(t)
        slot = None
        if dma:
            if eng == 'pool':
                slot = NDMA + self.srr
                self.srr = (self.srr + 1) % NSW
            else:
                slot = self.drr
                self.drr = (self.drr + 1) % NDMA
            if self.dcnt[slot] > 0:
                add((self.dsem[slot], self.dcnt[slot], 'dma', ('d', slot)))
        waits = []
        seen = self.seen[eng]
        for sid, tok in deps.items():
            if seen.get(sid, 0) >= tok[1]:
                continue
            seen[sid] = tok[1]
            waits.append((tok[0], tok[1]))
        if dma:
            self.dcnt[slot] += 16
            tok = (self.dsem[slot], self.dcnt[slot], 'dma', ('d', slot))
            inc = (self.dsem[slot], 16)
        else:
            self.cnt[eng] += 1
            tok = (self.sems[eng], self.cnt[eng], eng, ('e', eng))
            inc = (self.sems[eng], 1)
        self.ops[eng].append((fn, waits, inc))
        for k in writes:
            self.last_w[k] = tok
            self.readers[k] = {}
        for k in reads:
            if k in writes:
                continue
            r = self.readers.setdefault(k, {})
            if tok[3] not in r or r[tok[3]][1] < tok[1]:
                r[tok[3]] = tok
        return tok

    def pe(self, fn, reads=(), writes=()):
        return self.op('pe', fn, reads, writes)

    def dve(self, fn, reads=(), writes=()):
        return self.op('dve', fn, reads, writes)

    def act(self, fn, reads=(), writes=()):
        return self.op('act', fn, reads, writes)

    def pool(self, fn, reads=(), writes=()):
        return self.op('pool', fn, reads, writes)

    def dma(self, out, in_, reads=(), writes=(), eng='sp'):
        return self.op(eng, lambda e: e.dma_start(out=out, in_=in_), reads, writes, dma=True)

    def emit(self):
        nc = self.nc
        fin = []
        for e in ENGS:
            if e != 'sp' and self.cnt[e] > 0:
                fin.append((self.sems[e], self.cnt[e]))
        for i in range(NDMA + NSW):
            if self.dcnt[i] > 0:
                fin.append((self.dsem[i], self.dcnt[i]))
        ops = self.ops

        def run(e, lst):
            for fn, waits, inc in lst:
                for s, v in waits:
                    e.wait_ge(s, v)
                ins = fn(e)
                ins.then_inc(inc[0], inc[1])

        with nc.Block() as block:
            @block.sync
            def _(e):
                run(e, ops['sp'])
                for s, v in fin:
                    e.wait_ge(s, v)

            @block.tensor
            def _(e):
                run(e, ops['pe'])

            @block.vector
            def _(e):
                run(e, ops['dve'])

            @block.scalar
            def _(e):
                run(e, ops['act'])

            @block.gpsimd
            def _(e):
                run(e, ops['pool'])
        self.ops = {e: [] for e in ENGS}
        self.last_w = {}
        self.readers = {}
        for e in ENGS:
            for sid in list(self.seen[e].keys()):
                pass


def build(dbg=False, upto=3, n1=16, n2=16, n3=16):
    nc = bass.Bass("TRN2", target_bir_lowering=False)

    def din(name, shape, dt=F32):
        return nc.dram_tensor(name, shape, dt, kind="ExternalInput").ap()

    xfull = din("xfull", [8192, 1024])
    xown = din("xown", [2048, 1024])
    memb = din("memb", [256, 1024])
    wcat = din("wcat", [1024, 2328])
    gvec = din("gvec", [128, 48])
    bgate = din("bgate", [128, 24])
    lrup = din("lrup", [128, 36])
    wabd = din("wabd", [128, 512])
    wibd = din("wibd", [128, 512])
    w1k = din("w1k", [128, 4096])
    w1v = din("w1v", [128, 4096])
    w2k = din("w2k", [128, 128])
    w2v = din("w2v", [128, 128])
    posk = din("posk", [128, 32])
    posv = din("posv", [128, 32])
    ovl = din("ovl", [512, 128])
    ctab = din("ctab", [128, 148])
    tq4 = din("tq4", [16, 128, 512])
    fbt = din("fbt", [16, 128, 128])
    w_out = din("w_out", [1024, 1024])
    w_mq = din("w_mq", [1024, 1024])
    w_mk = din("w_mk", [1024, 1024])
    w_mv = din("w_mv", [1024, 1024])
    w_mo = din("w_mo", [1024, 1024])
    w_pq = din("w_pq", [1024, 2048])
    skT = din("skT", [128, 2048])
    gffn = din("gffn", [1, 1024])
    gfin = din("gfin", [1, 1024])
    peer_u = din("peer_u", [16384, 1024])
    peer_v = din("peer_v", [16384, 1024])
    x1d = nc.dram_tensor("x1d", [2048, 1024], F32, kind="Internal").ap()
    uv16 = nc.dram_tensor("uv16", [16384, 2048], BF16, kind="Internal").ap()
    yout = nc.dram_tensor("yout", [2048, 1024], F32, kind="ExternalOutput").ap()
    dbgo = {}
    if dbg:
        for nm, shp in [("d_yl", [128, 4 * 2048]), ("d_kc", [128, 512]), ("d_vc", [128, 4 * 2 * 196]),
                        ("d_yn", [2048, 512]), ("d_x2", [2048, 1024]), ("d_ks", [128, 8192]),
                        ("d_vs", [128, 64 * 132])]:
            dbgo[nm] = nc.dram_tensor(nm, shp, F32, kind="ExternalOutput").ap()

    with ExitStack() as ctx:
        P = Prog(nc, ctx)

        def sbt(c, name, shape, dt=F32):
            return c.enter_context(nc.sbuf_tensor(name, shape, dt))

        def pst(c, name, shape, dt=F32):
            return c.enter_context(nc.psum_tensor(name, shape, dt))

        def mm(out, lhsT, rhs, start, stop, r, w):
            P.pe(lambda e: e.matmul(out, lhsT=lhsT, rhs=rhs, start=start, stop=stop), reads=r, writes=w)

        def trp(out, in_, ident, r, w):
            P.pe(lambda e: e.transpose(out, in_, ident), reads=r, writes=w)

        def actf(out, in_, func, r, w, bias=0.0, scale=1.0, accum=None):
            if accum is None:
                P.act(lambda e: e.activation(out=out, in_=in_, func=func, bias=bias, scale=scale), reads=r, writes=w)
            else:
                P.act(lambda e: e.activation(out=out, in_=in_, func=func, bias=bias, scale=scale, accum_out=accum),
                      reads=r, writes=w)

        def ts(out, in0, s1, s2, op0, op1, r, w, eng='dve'):
            if op1 is None:
                P.op(eng, lambda e: e.tensor_scalar(out=out, in0=in0, scalar1=s1, scalar2=None, op0=op0), r, w)
            else:
                P.op(eng, lambda e: e.tensor_scalar(out=out, in0=in0, scalar1=s1, scalar2=s2, op0=op0, op1=op1), r, w)

        def tt(out, in0, in1, op, r, w, eng='dve'):
            P.op(eng, lambda e: e.tensor_tensor(out=out, in0=in0, in1=in1, op=op), r, w)

        def stt(out, in0, scalar, in1, op0, op1, r, w, accum=None):
            if accum is None:
                P.dve(lambda e: e.scalar_tensor_tensor(out=out, in0=in0, scalar=scalar, in1=in1, op0=op0, op1=op1), r, w)
            else:
                P.dve(lambda e: e.scalar_tensor_tensor(out=out, in0=in0, scalar=scalar, in1=in1, op0=op0, op1=op1,
                                                       accum_out=accum), r, w)

        def cp(out, in_, r, w, eng='dve'):
            if eng == 'act':
                P.act(lambda e: e.activation(out=out, in_=in_, func=AF.Copy), r, w)
            else:
                P.op(eng, lambda e: e.tensor_copy(out=out, in_=in_), r, w)

        def mset(ap, val, w, eng='dve'):
            P.op(eng, lambda e: e.memset(ap, val), (), w)

        identf = sbt(ctx, "identf", [128, 128])
        identb = sbt(ctx, "identb", [128, 128], BF16)
        ones_b = sbt(ctx, "ones_b", [128, 128], BF16)
        mhalf = sbt(ctx, "mhalf", [128, 4])
        cneg = sbt(ctx, "cneg", [128, 4], BF16)
        ssq = sbt(ctx, "ssq", [128, 8])
        rstd = sbt(ctx, "rstd", [128, 8])
        xnb = sbt(ctx, "xnb", [128, 1024], BF16)
        stage = [sbt(ctx, "stage0", [128, 1024]), sbt(ctx, "stage1", [128, 1024])]
        stg_i = [0]
        A = pst(ctx, "psA", [128, 1024])
        B = pst(ctx, "psB", [128, 1024])
        c0 = pst(ctx, "psc0", [128, 512])[:]
        c1 = pst(ctx, "psc1", [128, 512])[:]
        c2 = pst(ctx, "psc2", [128, 512])[:]
        ptb = pst(ctx, "ptb", [128, 1024], BF16)
        A0, A1 = A[:, 0:512], A[:, 512:1024]
        B0, B1 = B[:, 0:512], B[:, 512:1024]

        mset(identf[:], 1.0, ["identf"])
        P.pool(lambda e: e.affine_select(out=identf[:], in_=identf[:], pattern=[[-1, 128]], compare_op=ALU.is_equal,
                                         fill=0.0, base=0, channel_multiplier=1), ["identf"], ["identf"])
        cp(identb[:], identf[:], ["identf"], ["identb"])
        mset(ones_b[:], 1.0, ["ones_b"])
        mset(mhalf[:], -0.5, ["mhalf"])
        Rsel = sbt(ctx, "Rsel", [128, 256])
        mset(Rsel[:], 0.0, ["Rsel"])
        mset(Rsel[:, 127:128], 1.0, ["Rsel"])
        cnegM = sbt(ctx, "cnegM", [128, 2, 128], BF16)
        mset(cnegM[:], 0.0, ["cnegM"])
        mset(cnegM[0:64, 0, :], -1.0 / 16, ["cnegM"])
        mset(cnegM[64:128, 1, :], -1.0 / 16, ["cnegM"])
        mset(cneg[:], 0.0, ["cneg"])
        mset(cneg[0:64, 0:1], -1.0 / 16, ["cneg"])
        mset(cneg[64:128, 1:2], -1.0 / 16, ["cneg"])
        mset(cneg[:, 2:3], -1.0 / 32, ["cneg"])

        def calc_rstd(col, n):
            ts(ssq[:, col:col + 1], ssq[:, col:col + 1], 1.0 / n, EPS, ALU.mult, ALU.add, ["ssq%d" % col], ["ssq%d" % col])
            tt(rstd[:, col:col + 1], ssq[:, col:col + 1], mhalf[:, 0:1], ALU.pow, ["ssq%d" % col, "mhalf"],
               ["rstd%d" % col], eng='pool')

        xnb_alt = [None]

        def norm_T(x_sb, kx, dstT, kdst, gain=None, xnf=None, par=0, xbo=None):
            xb_, kxb, col = (xnb[:], "xnb", 0) if par == 0 else (stage[1][:].bitcast(BF16)[:, 0:1024], "stage1", 4)
            if xbo is not None:
                xb_, kxb = xbo
            actf(xb_, x_sb, AF.Square, [kx], [kxb, "ssq%d" % col], accum=ssq[:, col:col + 1])
            calc_rstd(col, 1024)
            if gain is None:
                actf(xb_, x_sb, AF.Copy, [kx, "rstd%d" % col], [kxb], scale=rstd[:, col:col + 1])
            else:
                stt(xnf, x_sb, rstd[:, col:col + 1], gain, ALU.mult, ALU.mult, [kx, "rstd%d" % col, "gain"], ["xnf"])
                cp(xb_, xnf, ["xnf"], [kxb], eng='act')
            for c in range(8):
                trp(ptb[:, 128 * c:128 * c + 128], xb_[:, 128 * c:128 * c + 128], identb[:], [kxb, "identb"], ["ptb"])
            cp(dstT, ptb[:].rearrange("p (c t) -> p c t", c=8), ["ptb"], [kdst])

        def load_w(dst, kdst, src, ncols, col0=0, gain=None, engs=('dve', 'pool')):
            k = 0
            for c in range(8):
                for o in range(0, ncols, 1024):
                    w = min(1024, ncols - o)
                    si = stg_i[0] % 2
                    stg_i[0] += 1
                    st = stage[si]
                    P.dma(st[:, 0:w], src[128 * c:128 * c + 128, col0 + o:col0 + o + w], writes=["stage%d" % si])
                    eng = engs[k % len(engs)]
                    k += 1
                    if gain is None:
                        cp(dst[:, c, o:o + w], st[:, 0:w], ["stage%d" % si], [kdst], eng=eng)
                    else:
                        ts(dst[:, c, o:o + w], st[:, 0:w], gain[:, c:c + 1], None, ALU.mult, None,
                           ["stage%d" % si, "gv"], [kdst], eng=eng)

        def load_small(dst, kdst, src, w, eng='dve'):
            for o in range(0, w, 1024):
                ww = min(1024, w - o)
                si = stg_i[0] % 2
                stg_i[0] += 1
                st = stage[si]
                P.dma(st[:, 0:ww], src[:, o:o + ww], writes=["stage%d" % si])
                cp(dst[:, o:o + ww], st[:, 0:ww], ["stage%d" % si], [kdst], eng=eng)

        gv = sbt(ctx, "gv", [128, 48])
        P.dma(gv[:], gvec, writes=["gv"])
        ctb = sbt(ctx, "ctb", [128, 148])
        P.dma(ctb[:], ctab, writes=["ctb"])
        cend = ctb[:, 0:4]
        kpos = ctb[:, 4:68]
        kpos512 = ctb[:, 68:132]

        with ExitStack() as cA:
            ksT = sbt(cA, "ksT", [128, 8192], BF16)
            kwT = sbt(cA, "kwT", [128, 8192], BF16)
            vs = sbt(cA, "vs", [128, 64, 2, 66], BF16)
            vw = sbt(cA, "vw", [128, 64, 2, 66], BF16)
            yl_own = sbt(cA, "yl_own", [128, 4, 2048], BF16)
            KC = sbt(cA, "KC", [128, 512], BF16)
            VCx = sbt(cA, "VCx", [128, 4, 2, 196], BF16)
            kmx = sbt(cA, "kmx", [128, 2])
            mset(kmx[:], 0.0, ["kmx"])
            mset(vs[:, :, :, 64:65], 1.0, ["vs"])
            mset(vw[:, :, :, 64:65], 1.0, ["vw"])
            mset(VCx[:, :, :, 64:65], 1.0, ["VCx"])
            for nt in range(4):
                si = stg_i[0] % 2
                stg_i[0] += 1
                P.dma(stage[si][:, 0:128], ovl[128 * nt:128 * nt + 128, :], writes=["stage%d" % si])
                for kv in range(2):
                    cp(VCx[:, nt, kv, 68:196], stage[si][:, 0:128], ["stage%d" % si], ["VCx"])

            with ExitStack() as c1x:
                wfs = sbt(c1x, "wfs", [128, 8, 1280], BF16)
                xt2 = [sbt(c1x, "xt0", [128, 1024]), sbt(c1x, "xt1", [128, 1024])]
                xTm2 = [sbt(c1x, "xTm0", [128, 8, 512], BF16), sbt(c1x, "xTm1", [128, 8, 512], BF16)]
                kcr2 = [sbt(c1x, "kcr0", [128, 528], BF16), sbt(c1x, "kcr1", [128, 528], BF16)]
                vcr2 = [sbt(c1x, "vcr0", [128, 528], BF16), sbt(c1x, "vcr1", [128, 528], BF16)]
                xl2 = [sbt(c1x, "xl0", [128, 4, 516]), sbt(c1x, "xl1", [128, 4, 516])]
                xc = sbt(c1x, "xc", [128, 4, 256])
                xcb = sbt(c1x, "xcb", [128, 4, 256], BF16)
                rr_ = sbt(c1x, "r", [128, 4, 256])
                sq = sbt(c1x, "sq", [128, 4, 256])
                ig = sbt(c1x, "ig", [128, 4, 256])
                hb = sbt(c1x, "hb", [128, 4, 256])
                ysel = sbt(c1x, "ysel", [128, 4, 128])
                state = sbt(c1x, "state", [128, 4])
                lp = sbt(c1x, "lp", [128, 36])
                cl = sbt(c1x, "cl", [128, 8])
                wa = sbt(c1x, "wa", [128, 4, 128], BF16)
                wi = sbt(c1x, "wi", [128, 4, 128], BF16)
                W1k = sbt(c1x, "W1k", [128, 32, 128], BF16)
                W1v = sbt(c1x, "W1v", [128, 32, 128], BF16)
                W2k = sbt(c1x, "W2k", [128, 128], BF16)
                W2v = sbt(c1x, "W2v", [128, 128], BF16)
                pk = sbt(c1x, "pk", [128, 32], BF16)
                pv_ = sbt(c1x, "pv", [128, 32], BF16)
                c1b = sbt(c1x, "c1b", [128, 2])
                Gk = sbt(c1x, "Gk", [128, 32], BF16)
                Gv = sbt(c1x, "Gv", [128, 128], BF16)
                sqk = sbt(c1x, "sqk", [128, 512], BF16)
                km1 = sbt(c1x, "km1", [128, 1])

                load_w(wfs, "wfs", wcat, 1280, 0, gain=gv[:, 0:8], engs=('dve',))
                P.dma(lp[:], lrup, writes=["lp"])
                load_small(wa[:].rearrange("p a b -> p (a b)"), "wa", wabd, 512)
                load_small(wi[:].rearrange("p a b -> p (a b)"), "wi", wibd, 512)
                load_small(W1k[:].rearrange("p a b -> p (a b)"), "W1k", w1k, 4096, eng='pool')
                load_small(W1v[:].rearrange("p a b -> p (a b)"), "W1v", w1v, 4096, eng='pool')
                load_small(W2k[:], "W2k", w2k, 128)
                load_small(W2v[:], "W2v", w2v, 128)
                load_small(pk[:], "pk", posk, 32)
                load_small(pv_[:], "pv", posv, 32)
                cw = lp[:, 0:16].rearrange("p (c k) -> p c k", c=4)
                cb = lp[:, 16:20]
                ba = lp[:, 20:24]
                bi = lp[:, 24:28]
                lam = lp[:, 28:32]
                selc = lp[:, 32:36]
                actf(cl[:, 0:4], lam, AF.Exp, ["lp"], ["cl"], scale=-1.0)
                actf(cl[:, 0:4], cl[:, 0:4], AF.Ln, ["cl"], ["cl"], bias=1.0)
                ts(cl[:, 4:8], cl[:, 0:4], -16.0, None, ALU.mult, None, ["cl"], ["cl2"])
                ts(cl[:, 0:4], cl[:, 0:4], -8.0, None, ALU.mult, None, ["cl", "cl2"], ["cl"])
                mset(state[:], 0.0, ["state"])
                mset(xl2[0][:, :, 0:3], 0.0, ["xl0"])
                mset(kcr2[0][:, 0:16], 0.0, ["kcr0"])
                mset(vcr2[0][:, 0:16], 0.0, ["vcr0"])
                for (W1, kW1, pp, kpp, col) in [(W1k, "W1k", pk, "pk", 0), (W1v, "W1v", pv_, "pv", 1)]:
                    for l in range(32):
                        mm(c0[:, col:col + 1], W1[:, l, :], pp[:, l:l + 1], l == 0, l == 31, [kW1, kpp], ["c0"])
                    cp(c1b[:, col:col + 1], c0[:, col:col + 1], ["c0"], ["c1b"])

                def stage_a(i):
                    xTm_, kx = xTm2[i % 2], "xTm%d" % (i % 2)
                    xl_, kxl = xl2[i % 2], "xl%d" % (i % 2)
                    kcr_, kkc = kcr2[i % 2], "kcr%d" % (i % 2)
                    vcr_, kvc = vcr2[i % 2], "vcr%d" % (i % 2)
                    for j in range(4):
                        blk = 4 * i + j
                        xt_, kxt = xt2[j % 2], "xt%d" % (j % 2)
                        P.dma(xt_[:], xfull[128 * blk:128 * blk + 128, :], writes=[kxt])
                        norm_T(xt_[:], kxt, xTm_[:, :, 128 * j:128 * j + 128], kx, par=j % 2)
                    for m in range(8):
                        pt_, kp = (A0, "A0") if m % 2 == 0 else (A1, "A1")
                        for c in range(8):
                            mm(pt_, wfs[:, c, 128 * m:128 * m + 128], xTm_[:, c, :], c == 0, c == 7, ["wfs", kx], [kp])
                        if m < 4:
                            cp(xl_[:, m, 3:515], pt_, [kp], [kxl], eng='act')
                        elif m == 4:
                            cp(kcr_[:, 16:528], pt_, [kp], [kkc])
                        elif m == 5:
                            cp(vcr_[:, 16:528], pt_, [kp], [kvc])
                        elif m == 6:
                            cp(ksT[:, 512 * i:512 * i + 512], pt_, [kp], ["ksT"])
                        else:
                            cp(kwT[:, 512 * i:512 * i + 512], pt_, [kp], ["kwT"])
                    for j in range(4):
                        blk = 4 * i + j
                        for c in range(8):
                            mm(B0[:, 0:256], xTm_[:, c, 128 * j:128 * j + 128], wfs[:, c, 1024:1280], c == 0, c == 7,
                               ["wfs", kx], ["B0"])
                        cp(vs[:, blk, :, 0:64], B0[:, 0:128].rearrange("p (a b) -> p a b", a=2), ["B0"], ["vs"])
                        cp(vw[:, blk, :, 0:64], B0[:, 128:256].rearrange("p (a b) -> p a b", a=2), ["B0"], ["vw"])

                def stage_b(i):
                    xl_, kxl = xl2[i % 2], "xl%d" % (i % 2)
                    xln, kxln = xl2[(i + 1) % 2], "xl%d" % ((i + 1) % 2)
                    for (kt, kk) in [(ksT, "ksT"), (kwT, "kwT")]:
                        actf(sqk[:], kt[:, 512 * i:512 * i + 512], AF.Square, [kk], ["sqk"])
                        mm(B1, ones_b[:], sqk[:], True, True, ["ones_b", "sqk"], ["B1"])
                        P.dve(lambda e: e.tensor_reduce(out=km1[:], in_=B1, axis=AX.X, op=ALU.max), ["B1"], ["km1"])
                        tt(kmx[:, 0:1], kmx[:, 0:1], km1[:], ALU.max, ["kmx", "km1"], ["kmx"])
                    cp(xln[:, :, 0:3], xl_[:, :, 512:515], [kxl], [kxln])
                    for hf in range(2):
                        o = 256 * hf
                        for ch in range(4):
                            ts(xc[:, ch, :], xl_[:, ch, o:o + 256], cw[:, ch, 0:1], cb[:, ch:ch + 1], ALU.mult, ALU.add,
                               [kxl, "lp"], ["xc%d" % ch])
                            for k in range(1, 4):
                                stt(xc[:, ch, :], xl_[:, ch, o + k:o + k + 256], cw[:, ch, k:k + 1], xc[:, ch, :],
                                    ALU.mult, ALU.add, [kxl, "lp", "xc%d" % ch], ["xc%d" % ch])
                            cp(xcb[:, ch, :], xc[:, ch, :], ["xc%d" % ch], ["xcb%d" % ch], eng='pool')
                        for ch in range(4):
                            pa_, kpa = (c0, "c0") if ch % 2 == 0 else (c1, "c1")
                            mm(pa_[:, 0:256], wa[:, ch, :], xcb[:, ch, :], True, False, ["wa", "xcb%d" % ch], [kpa])
                            mm(pa_[:, 256:512], wi[:, ch, :], xcb[:, ch, :], False, True, ["wi", "xcb%d" % ch], [kpa])
                            actf(rr_[:, ch, :], pa_[:, 0:256], AF.Sigmoid, [kpa, "lp"], ["r%d" % ch], bias=ba[:, ch:ch + 1])
                            actf(ig[:, ch, :], pa_[:, 256:512], AF.Sigmoid, [kpa, "lp"], ["ig%d" % ch], bias=bi[:, ch:ch + 1])
                        for ch in range(4):
                            actf(sq[:, ch, :], rr_[:, ch, :], AF.Exp, ["r%d" % ch, "cl2"], ["sq%d" % ch], scale=cl[:, 4 + ch:5 + ch])
                            actf(rr_[:, ch, :], rr_[:, ch, :], AF.Exp, ["r%d" % ch, "cl"], ["r%d" % ch], scale=cl[:, ch:ch + 1])
                        K4 = lambda nm: [nm + "%d" % c_ for c_ in range(4)]
                        actf(sq[:].rearrange("p a b -> p (a b)"), sq[:].rearrange("p a b -> p (a b)"), AF.Sqrt, K4("sq"), K4("sq"),
                             bias=1.0, scale=-1.0)
                        for ch in range(4):
                            tt(ig[:, ch, :], ig[:, ch, :], xc[:, ch, :], ALU.mult, ["ig%d" % ch, "xc%d" % ch], ["ig%d" % ch])
                            tt(ig[:, ch, :], ig[:, ch, :], sq[:, ch, :], ALU.mult, ["ig%d" % ch, "sq%d" % ch], ["ig%d" % ch])
                            P.dve((lambda ch: lambda e: e.tensor_tensor_scan(
                                out=hb[:, ch, :], data0=rr_[:, ch, :], data1=ig[:, ch, :], initial=state[:, ch:ch + 1],
                                op0=ALU.mult, op1=ALU.add))(ch), ["r%d" % ch, "ig%d" % ch, "state"], ["hb%d" % ch])
                        cp(state[:], hb[:, :, 255], K4("hb"), ["state"])
                        for r2 in range(2):
                            ri = 2 * hf + r2
                            src = hb[:, :, 128 * r2:128 * r2 + 128]
                            if ri == 0:
                                ts(ysel[:], src, selc[:, 0:1], None, ALU.mult, None, K4("hb") + ["lp"], ["ysel"])
                            else:
                                stt(ysel[:], src, selc[:, ri:ri + 1], ysel[:], ALU.mult, ALU.add, K4("hb") + ["lp", "ysel"], ["ysel"])
                        if hf == 1:
                            cp(yl_own[:, :, 128 * i:128 * i + 128], ysel[:], ["ysel"], ["yl_own"])
                    p0 = 32 * (i % 4)
                    nt = i // 4
                    for (raw2, kr, W1, kW1, col, G, kG) in [(kcr2, "kcr", W1k, "W1k", 0, Gk, "Gk"),
                                                           (vcr2, "vcr", W1v, "W1v", 1, Gv, "Gv")]:
                        raw, kraw = raw2[i % 2], kr + "%d" % (i % 2)
                        rawn, krawn = raw2[(i + 1) % 2], kr + "%d" % ((i + 1) % 2)
                        for l in range(32):
                            mm(c2[:, 0:32], W1[:, l, :], raw[:, l:l + 497:16], l == 0, l == 31, [kW1, kraw], ["c2"])
                        gdst = G[:] if col == 0 else G[:, p0:p0 + 32]
                        actf(gdst, c2[:, 0:32], AF.Gelu_apprx_tanh, ["c2", "c1b"], [kG], bias=c1b[:, col:col + 1])
                        cp(rawn[:, 0:16], raw[:, 512:528], [kraw], [krawn])
                    mm(c2[:, 64:96], W2k[:], Gk[:], True, True, ["W2k", "Gk"], ["c2"])
                    cp(KC[:, 32 * i:32 * i + 32], c2[:, 64:96], ["c2"], ["KC"])
                    if i % 4 == 3:
                        mm(c2[:, 128:256], Gv[:], W2v[:], True, True, ["W2v", "Gv"], ["c2"])
                        cp(VCx[:, nt, :, 0:64], c2[:, 128:256].rearrange("p (a b) -> p a b", a=2), ["c2"], ["VCx"])

                for i in range(n1 + 1):
                    if i < n1:
                        stage_a(i)
                    if i >= 1:
                        stage_b(i - 1)
                actf(sqk[:], KC[:], AF.Square, ["KC"], ["sqk"])
                mm(B1, ones_b[:], sqk[:], True, True, ["ones_b", "sqk"], ["B1"])
                P.dve(lambda e: e.tensor_reduce(out=km1[:], in_=B1, axis=AX.X, op=ALU.max), ["B1"], ["km1"])
                tt(kmx[:, 0:1], kmx[:, 0:1], km1[:], ALU.max, ["kmx", "km1"], ["kmx"])
                ts(kmx[:, 1:2], kmx[:, 0:1], -1.0 / 16, None, ALU.mult, None, ["kmx"], ["expb"])
                if dbg:
                    for nm, src_ap, kk, w_ in [("d_ks", ksT[:], "ksT", 8192), ("d_kc", KC[:], "KC", 512),
                                               ("d_yl", yl_own[:].rearrange("p a b -> p (a b)"), "yl_own", 8192),
                                               ("d_vc", VCx[:].rearrange("p a b c -> p (a b c)"), "VCx", 1568),
                                               ("d_vs", vs[:].rearrange("p a b c -> p (a b c)"), "vs", 8448)]:
                        for o in range(0, w_, 1024):
                            ww = min(1024, w_ - o)
                            cp(stage[0][:, 0:ww], src_ap[:, o:o + ww], [kk], ["stage0"])
                            P.dma(dbgo[nm][:, o:o + ww], stage[0][:, 0:ww], reads=["stage0"])
                P.emit()

            with ExitStack() as c2x:
                wown = sbt(c2x, "wown", [128, 8, 1048], BF16)
                wout = sbt(c2x, "wout", [128, 8, 1024], BF16)
                Ebig = sbt(c2x, "Ebig", [128, 8192], BF16)
                xo = sbt(c2x, "xo", [128, 1024])
                xoT = sbt(c2x, "xoT", [128, 8, 128], BF16)
                QT = sbt(c2x, "QT", [128, 512], BF16)
                Qsq = sbt(c2x, "Qsq", [128, 512], BF16)
                nbS = sbt(c2x, "nbS", [128, 2, 512], BF16)
                gl = sbt(c2x, "gl", [128, 4, 128])
                ylf = sbt(c2x, "ylf", [128, 4, 128])
                ylb = sbt(c2x, "ylb", [128, 4, 128], BF16)
                ylsq = sbt(c2x, "ylsq", [128, 4, 128], BF16)
                gates = sbt(c2x, "gates", [128, 24])
                bg = sbt(c2x, "bg", [128, 24])
                tq = sbt(c2x, "tq", [128, 512])
                fb = sbt(c2x, "fb", [128, 128])
                cmpb = sbt(c2x, "cmpb", [128, 4, 512], BF16)
                causb = sbt(c2x, "causb", [128, 4, 512], BF16)
                winb = sbt(c2x, "winb", [128, 8, 512], BF16)
                wtmp = sbt(c2x, "wtmp", [128, 2, 512])
                PT = [sbt(c2x, "PT0", [128, 512], BF16), sbt(c2x, "PT1", [128, 512], BF16), sbt(c2x, "PT2", [128, 512], BF16)]
                rz = sbt(c2x, "rz", [128, 4])
                coef = sbt(c2x, "coef", [128, 4])
                otmp = sbt(c2x, "otmp", [128, 4, 64])
                ynsa = sbt(c2x, "ynsa", [128, 512])
                ynb = sbt(c2x, "ynb", [128, 512], BF16)
                ynT = sbt(c2x, "ynT", [128, 4, 128], BF16)
                imp = sbt(c2x, "imp", [128, 128])
                imp2 = sbt(c2x, "imp2", [128, 128])
                m8 = sbt(c2x, "m8", [128, 16])
                selb = sbt(c2x, "selb", [128, 128], BF16)
                selT4 = sbt(c2x, "selT4", [128, 4, 128], BF16)
                x1 = sbt(c2x, "x1", [128, 1024])
                expb = kmx[:, 1:2]
                cvf = [sbt(c2x, "cvf0", [128, 512]), sbt(c2x, "cvf1", [128, 512])]
                cvo = [sbt(c2x, "cvo0", [128, 512], BF16), sbt(c2x, "cvo1", [128, 512], BF16)]
                conv_list = [(srct, co, r, h) for (srct, co) in [(peer_u, 0), (peer_v, 1024)] for r in range(128)
                             for h in range(2)]
                conv_pos = [0]

                def conv_some(n):
                    for _ in range(n):
                        if conv_pos[0] >= len(conv_list):
                            return
                        srct, co, r, h = conv_list[conv_pos[0]]
                        k = conv_pos[0] % 2
                        conv_pos[0] += 1
                        P.dma(cvf[k][:], srct[128 * r:128 * r + 128, 512 * h:512 * h + 512], writes=["cvf%d" % k])
                        cp(cvo[k][:], cvf[k][:], ["cvf%d" % k], ["cvo%d" % k], eng='pool')
                        P.dma(uv16[128 * r:128 * r + 128, co + 512 * h:co + 512 * h + 512], cvo[k][:], reads=["cvo%d" % k],
                              writes=["tb16"])

                if upto >= 2:
                    load_w(wown, "wown", wcat, 1048, 1280, gain=gv[:, 0:8])
                    load_w(wout, "wout", w_out, 1024, 0, gain=gv[:, 8:16])
                    P.dma(bg[:], bgate, writes=["bg"])
                    mset(Ebig[:], 1.0, ["Ebig"], eng='pool')
                    P.pool(lambda e: e.affine_select(out=Ebig[:], in_=Ebig[:], pattern=[[1, 8192]], compare_op=ALU.is_ge,
                                                     fill=0.0, base=0, channel_multiplier=-64), ["Ebig"], ["Ebig"])
                    P.pool(lambda e: e.affine_select(out=Ebig[:], in_=Ebig[:], pattern=[[-1, 8192]], compare_op=ALU.is_ge,
                                                     fill=0.0, base=63, channel_multiplier=64), ["Ebig"], ["Ebig"])
                tile_ctr = [0]

                SB = [(A0, "A0"), (A1, "A1"), (c0, "c0")]

                def score_tile(kT, kkT, kvs, kb, kv, extra):
                    n = tile_ctr[0]
                    tile_ctr[0] += 1
                    sp_, ksp = SB[n % 3]
                    pt_, kpt = PT[n % 3], "PT%d" % (n % 3)
                    mm(sp_, kT[kvs, 128 * kb:128 * kb + 128], QT[kvs, :], True, False, [kkT, "QT"], [ksp])
                    for k_, (l_, r_, rd_) in enumerate(extra):
                        mm(sp_, l_, r_, False, k_ == len(extra) - 1, rd_, [ksp])
                    actf(pt_[:], sp_, AF.Exp, [ksp, "expb"], [kpt], bias=expb)
                    return pt_, kpt

                def run_branch(tiles, acc, kacc, imp_rhs=None):
                    prev = None
                    nt_ = len(tiles)
                    for idx in range(nt_ + 1):
                        cur = None
                        if idx < nt_:
                            sa, vrhs, kvk, irhs = tiles[idx]
                            pt_, kpt = score_tile(*sa)
                            cur = (pt_, kpt, vrhs, kvk, irhs, idx)
                        if prev is not None:
                            pt_, kpt, vrhs, kvk, irhs, j = prev
                            for g in range(4):
                                mm(acc[:, 65 * g:65 * g + 65], pt_[:, 128 * g:128 * g + 128], vrhs, j == 0 and g == 0,
                                   j == nt_ - 1 and g == 3, [kpt, kvk], [kacc])
                            if irhs is not None:
                                for g in range(4):
                                    mm(B1[:, 128 * g:128 * g + 128], pt_[:, 128 * g:128 * g + 128], irhs,
                                       j == 0 and g == 0, j == nt_ - 1 and g == 3, [kpt, kvk], ["B1"])
                        prev = cur

                def combine(kv, br, first, acc, kacc):
                    o3 = acc[:, 0:260].rearrange("p (g d) -> p g d", g=4)
                    ts(rz[:], o3[:, :, 64], 1e-30, None, ALU.max, None, [kacc], ["rz"])
                    P.dve(lambda e: e.reciprocal(out=rz[:], in_=rz[:]), ["rz"], ["rz"])
                    tt(coef[:], rz[:], gates[:, 12 * kv + br:12 * kv + 12:3], ALU.mult, ["rz", "gates"], ["coef"])
                    y3 = ynsa[:, 256 * kv:256 * kv + 256].rearrange("p (g d) -> p g d", g=4)
                    cb_ = coef[:].unsqueeze(2).to_broadcast([128, 4, 64])
                    if first:
                        tt(y3, o3[:, :, 0:64], cb_, ALU.mult, [kacc, "coef"], ["ynsa"])
                    else:
                        tt(otmp[:], o3[:, :, 0:64], cb_, ALU.mult, [kacc, "coef"], ["otmp"])
                        tt(y3, y3, otmp[:], ALU.add, ["ynsa", "otmp"], ["ynsa"])

                for i in range(n2 if upto >= 2 else 0):
                    P.dma(xo[:], xown[128 * i:128 * i + 128, :], writes=["xo"])
                    P.dma(tq[:], tq4[i], writes=["tq"])
                    P.dma(fb[:], fbt[i], writes=["fb"])
                    norm_T(xo[:], "xo", xoT[:], "xoT")
                    conv_some((512 + n2 - 1) // n2)
                    for g in range(4):
                        for c in range(8):
                            mm(c0[:, 128 * g:128 * g + 128], wown[:, c, 512 + 128 * g:512 + 128 * g + 128], xoT[:, c, :],
                               g == 0 and c == 0, g == 3 and c == 7, ["wown", "xoT"], ["c0"])
                    actf(QT[:], c0, AF.Copy, ["c0"], ["QT"], scale=0.125)
                    actf(Qsq[:], c0, AF.Square, ["c0"], ["Qsq"])
                    mm(c1, cnegM[:, 0, :], Qsq[:], True, True, ["cnegM", "Qsq"], ["c1"])
                    mm(c2, cnegM[:, 1, :], Qsq[:], True, True, ["cnegM", "Qsq"], ["c2"])
                    cp(nbS[:, 0, :], c1, ["c1"], ["nbS"])
                    cp(nbS[:, 1, :], c2, ["c2"], ["nbS"])
                    for ch in range(4):
                        for c in range(8):
                            mm(c1[:, 128 * ch:128 * ch + 128], wown[:, c, 128 * ch:128 * ch + 128], xoT[:, c, :],
                               ch == 0 and c == 0, ch == 3 and c == 7, ["wown", "xoT"], ["c1"])
                    actf(gl[:].rearrange("p a b -> p (a b)"), c1, AF.Gelu_apprx_tanh, ["c1"], ["gl"])
                    tt(ylf[:], gl[:], yl_own[:, :, 128 * i:128 * i + 128], ALU.mult, ["gl", "yl_own"], ["ylf"])
                    cp(ylb[:].rearrange("p a b -> p (a b)"), ylf[:].rearrange("p a b -> p (a b)"), ["ylf"], ["ylb"], eng='pool')
                    actf(ylsq[:].rearrange("p a b -> p (a b)"), ylf[:].rearrange("p a b -> p (a b)"), AF.Square, ["ylf"], ["ylsq"])
                    for ch in range(4):
                        mm(c2[:, 0:1], ylsq[:, ch, :], ones_b[:, 0:1], ch == 0, ch == 3, ["ylsq", "ones_b"], ["c2"])
                    cp(ssq[:, 1:2], c2[:, 0:1], ["c2"], ["ssq1"])
                    calc_rstd(1, 512)
                    for c in range(8):
                        mm(c2[:, 32:56], xoT[:, c, :], wown[:, c, 1024:1048], c == 0, c == 7, ["wown", "xoT"], ["c2"])
                    tt(gates[:], c2[:, 32:56], bg[:], ALU.add, ["c2", "bg"], ["gates"])
                    actf(gates[:], gates[:], AF.Sigmoid, ["gates"], ["gates"])
                    NT = (32 * i + 31 + 127) // 128
                    for r in range(4):
                        kb = 4 * i + r
                        ts(causb[:, r, :], tq[:], kpos[:, kb:kb + 1], NEG, ALU.is_lt, ALU.mult, ["tq", "ctb"], ["causb"])
                    for kv in range(2):
                        kvs = slice(64 * kv, 64 * kv + 64)
                        for nt in range(NT):
                            ts(wtmp[:, 0, :], tq[:], cend[:, nt:nt + 1], None, ALU.is_lt, None, ["tq", "ctb"], ["wtmp0"])
                            stt(cmpb[:, nt, :], wtmp[:, 0, :], NEG, nbS[:, kv, :], ALU.mult, ALU.add, ["wtmp0", "nbS"], ["cmpb"])
                        for r8 in range(8):
                            kb = 4 * i - 4 + r8
                            if kb < 0:
                                continue
                            ts(wtmp[:, 0, :], tq[:], kpos[:, kb:kb + 1], None, ALU.is_lt, None, ["tq", "ctb"], ["wtmp0"])
                            stt(wtmp[:, 1, :], tq[:], kpos512[:, kb:kb + 1], wtmp[:, 0, :], ALU.is_ge, ALU.add, ["tq", "ctb", "wtmp0"], ["wtmp1"])
                            stt(winb[:, r8, :], wtmp[:, 1, :], NEG, nbS[:, kv, :], ALU.mult, ALU.add, ["wtmp1", "nbS"], ["winb"])
                        tiles = [((KC, "KC", kvs, nt, kv, [(identb[:], cmpb[:, nt, :], ["identb", "cmpb"])]),
                                  VCx[:, nt, kv, 0:65], "VCx", VCx[:, nt, kv, 68:196]) for nt in range(NT)]
                        run_branch(tiles, B0, "B0")
                        combine(kv, 0, True, B0, "B0")
                        ts(imp[:], B1[:, 0:128], rz[:, 0:1], None, ALU.mult, None, ["B1", "rz"], ["imp"])
                        for g in range(1, 4):
                            stt(imp[:], B1[:, 128 * g:128 * g + 128], rz[:, g:g + 1], imp[:], ALU.mult, ALU.add,
                                ["B1", "rz", "imp"], ["imp"])
                        tt(imp[:], imp[:], fb[:], ALU.add, ["imp", "fb"], ["imp"])
                        P.dve(lambda e: e.max(out=m8[:, 0:8], in_=imp[:]), ["imp"], ["m8"])
                        P.dve(lambda e: e.match_replace(out=imp2[:], in_to_replace=m8[:, 0:8], in_values=imp[:],
                                                        imm_value=-3e9), ["imp", "m8"], ["imp2"])
                        P.dve(lambda e: e.max(out=m8[:, 8:16], in_=imp2[:]), ["imp2"], ["m8"])
                        ts(imp2[:], imp[:], m8[:, 15:16], None, ALU.is_ge, None, ["imp", "m8"], ["imp2"])
                        stt(imp2[:], imp[:], -1.0, imp2[:], ALU.is_gt, ALU.mult, ["imp", "imp2"], ["imp2"])
                        ts(selb[:], imp2[:], 1.0, -NEG, ALU.subtract, ALU.mult, ["imp2"], ["selb"])
                        kbs = [4 * i - 4 + r8 for r8 in range(8) if 4 * i - 4 + r8 >= 0]
                        tiles = [((kwT, "kwT", kvs, kb, kv, [(identb[:], winb[:, kb - (4 * i - 4), :], ["identb", "winb"])]),
                                  vw[:, kb, kv, 0:65], "vw", None) for kb in kbs]
                        run_branch(tiles, c1, "c1")
                        combine(kv, 2, False, c1, "c1")
                        trp(ptb[:, 0:128], selb[:], identb[:], ["selb", "identb"], ["ptb"])
                        tt(selT4[:], ptb[:, 0:128].unsqueeze(1).to_broadcast([128, 4, 128]),
                           nbS[:, kv, :].rearrange("p (g q) -> p g q", g=4), ALU.add, ["ptb", "nbS"], ["selT4"])
                        sel_rhs = selT4[:].rearrange("p a b -> p (a b)")
                        nkb = 4 * i + 4
                        tiles = []
                        for kb in range(nkb):
                            extra = [(Ebig[:, 128 * kb:128 * kb + 128], sel_rhs, ["Ebig", "selT4"])]
                            if kb >= 4 * i:
                                extra.append((identb[:], causb[:, kb - 4 * i, :], ["identb", "causb"]))
                            tiles.append(((ksT, "ksT", kvs, kb, kv, extra), vs[:, kb, kv, 0:65], "vs", None))
                        run_branch(tiles, c2, "c2")
                        combine(kv, 1, False, c2, "c2")
                    if dbg:
                        P.dma(dbgo["d_yn"][128 * i:128 * i + 128, :], ynsa[:], reads=["ynsa"])
                    actf(ynb[:], ynsa[:], AF.Square, ["ynsa"], ["ynb", "ssq2"], accum=ssq[:, 2:3])
                    calc_rstd(2, 512)
                    cp(ynb[:], ynsa[:], ["ynsa"], ["ynb"])
                    for c in range(4):
                        trp(ptb[:, 128 * c:128 * c + 128], ynb[:, 128 * c:128 * c + 128], identb[:], ["ynb", "identb"], ["ptb"])
                    cp(ynT[:], ptb[:, 0:512].rearrange("p (c t) -> p c t", c=4), ["ptb"], ["ynT"])
                    for hh, (pl, kpl, pn, kpn) in enumerate([(B0, "B0", c0, "c0"), (B1, "B1", c1, "c1")]):
                        cs = slice(512 * hh, 512 * hh + 512)
                        for ch in range(4):
                            mm(pl, ylb[:, ch, :], wout[:, ch, cs], ch == 0, ch == 3, ["ylb", "wout"], [kpl])
                        for ch in range(4):
                            mm(pn, ynT[:, ch, :], wout[:, 4 + ch, cs], ch == 0, ch == 3, ["ynT", "wout"], [kpn])
                        stt(x1[:, cs], pl, rstd[:, 1:2], xo[:, cs], ALU.mult, ALU.add, [kpl, "rstd1", "xo"], ["x1"])
                        stt(x1[:, cs], pn, rstd[:, 2:3], x1[:, cs], ALU.mult, ALU.add, [kpn, "rstd2", "x1"], ["x1"])
                    P.dma(x1d[128 * i:128 * i + 128, :], x1[:], reads=["x1"], writes=["x1d"])
                conv_some(512)
                P.emit()

        with ExitStack() as c3x:
            wmq = sbt(c3x, "wmq", [128, 8, 1024], BF16)
            wmo = sbt(c3x, "wmo", [128, 8, 1024], BF16)
            wpq = sbt(c3x, "wpq", [128, 8, 2048], BF16)
            KmT = sbt(c3x, "KmT", [128, 8, 256], BF16)
            Vm = sbt(c3x, "Vm", [128, 2, 1024], BF16)
            sk = sbt(c3x, "sk", [128, 16, 128], BF16)
            gff = sbt(c3x, "gff", [128, 1024])
            gfi = sbt(c3x, "gfi", [128, 1024])
            memT = sbt(c3x, "memT", [128, 8, 256], BF16)
            kmm = sbt(c3x, "kmm", [128, 4])
            xa = sbt(c3x, "xa", [128, 1024])
            xb2 = sbt(c3x, "xb2", [128, 1024])
            xnf = sbt(c3x, "xnf", [128, 1024])
            xT = sbt(c3x, "xT", [128, 8, 128], BF16)
            qmT = sbt(c3x, "qmT", [128, 8, 128], BF16)
            qmsq = sbt(c3x, "qmsq", [128, 8, 128], BF16)
            negbm = sbt(c3x, "negbm", [1, 512], BF16)
            PTm = [sbt(c3x, "PTm0", [128, 512], BF16), sbt(c3x, "PTm1", [128, 512], BF16)]
            rzb = sbt(c3x, "rzb", [128, 512])
            oT = sbt(c3x, "oT", [128, 8, 128], BF16)
            qpT = sbt(c3x, "qpT", [128, 16, 128], BF16)
            s_sb = sbt(c3x, "s_sb", [128, 16, 128])
            s2 = sbt(c3x, "s2", [128, 128])
            v16 = sbt(c3x, "v16", [128, 16, 16])
            i16 = sbt(c3x, "i16", [128, 16, 16], U32)
            i16f = sbt(c3x, "i16f", [128, 16, 16])
            cand = sbt(c3x, "cand", [128, 8, 256])
            cand2 = sbt(c3x, "cand2", [128, 256])
            cid = sbt(c3x, "cid", [128, 8, 256])
            top = sbt(c3x, "top", [128, 8, 16])
            eid = sbt(c3x, "eid", [128, 128])
            pos = sbt(c3x, "pos", [128, 8, 16], U32)
            posf = sbt(c3x, "posf", [128, 128])
            ai = sbt(c3x, "ai", [128, 128], I32)
            af = sbt(c3x, "af", [128, 128])
            bfl = sbt(c3x, "bfl", [128, 128])
            e1 = sbt(c3x, "e1", [128, 128])
            e2 = sbt(c3x, "e2", [128, 128])
            gw = sbt(c3x, "gw", [128, 8, 16])
            gsum = sbt(c3x, "gsum", [128, 8])
            eidT = sbt(c3x, "eidT", [128, 128], I32)
            gT = sbt(c3x, "gT", [128, 128])
            eidT2 = sbt(c3x, "eidT2", [128, 128], I32)
            gT2 = sbt(c3x, "gT2", [128, 128])
            NB = 8
            UVg = [sbt(c3x, "UVg%d" % k, [128, 2048], BF16) for k in range(NB)]
            gh = sbt(c3x, "gh", [128, 128])
            hTa = sbt(c3x, "hTa", [128, 128])
            hTb = sbt(c3x, "hTb", [128, 128])
            jk = [sbt(c3x, "jk0", [128, 512], BF16), sbt(c3x, "jk1", [128, 512], BF16)]
            selt = [sbt(c3x, "selt0", [128, 128], BF16), sbt(c3x, "selt1", [128, 128], BF16)]
            Dt = [sbt(c3x, "Dt0", [128, 128], BF16), sbt(c3x, "Dt1", [128, 128], BF16)]
            yo = xnf

            if upto >= 3:
                load_w(wmq, "wmq", w_mq, 1024, 0, gain=gv[:, 16:24])
                load_w(wpq, "wpq", w_pq, 2048, 0)
                load_small(sk[:].rearrange("p a b -> p (a b)"), "sk", skT, 2048)
                P.dma(gff[:], gffn.partition_broadcast(128), writes=["gain"])
                P.dma(gfi[:], gfin.partition_broadcast(128), writes=["gfi"])
                for mb in range(2):
                    P.dma(xa[:], memb[128 * mb:128 * mb + 128, :], writes=["xa"])
                    norm_T(xa[:], "xa", memT[:, :, 128 * mb:128 * mb + 128], "memT")
                P.emit()
                load_w(wmo, "wmo", w_mk, 1024, 0, gain=gv[:, 24:32])
                for oc in range(8):
                    for c in range(8):
                        mm(c0[:, 0:256], wmo[:, c, 128 * oc:128 * oc + 128], memT[:, c, :], c == 0, c == 7, ["wmo", "memT"], ["c0"])
                    cp(KmT[:, oc, :], c0[:, 0:256], ["c0"], ["KmT"])
                sqm = sbt(c3x, "sqm", [128, 8, 256], BF16)
                actf(sqm[:].rearrange("p a b -> p (a b)"), KmT[:].rearrange("p a b -> p (a b)"), AF.Square, ["KmT"], ["sqm"])
                for hm in range(4):
                    for dc in range(2):
                        mm(c1[:, 0:256], ones_b[:], sqm[:, 2 * hm + dc, :], dc == 0, dc == 1, ["ones_b", "sqm"], ["c1"])
                    P.dve((lambda hm: lambda e: e.tensor_reduce(out=kmm[:, hm:hm + 1], in_=c1[:, 0:256], axis=AX.X, op=ALU.max))(hm),
                          ["c1"], ["kmm"])
                P.dve(lambda e: e.tensor_reduce(out=kmm[:, 0:1], in_=kmm[:, 0:4], axis=AX.X, op=ALU.max), ["kmm"], ["kmm"])
                ts(kmm[:, 1:2], kmm[:, 0:1], -1.0 / 32, None, ALU.mult, None, ["kmm"], ["expbm"])
                expbm = kmm[:, 1:2]
                P.emit()
                load_w(wmo, "wmo", w_mv, 1024, 0, gain=gv[:, 24:32])
                for mb in range(2):
                    for hh in range(2):
                        for c in range(8):
                            mm(c0, memT[:, c, 128 * mb:128 * mb + 128], wmo[:, c, 512 * hh:512 * hh + 512], c == 0, c == 7,
                               ["wmo", "memT"], ["c0"])
                        cp(Vm[:, mb, 512 * hh:512 * hh + 512], c0, ["c0"], ["Vm"])
                P.emit()
                load_w(wmo, "wmo", w_mo, 1024, 0)

            sqmf = sqm[:].rearrange("p a b -> p (a b)") if upto >= 3 else None
            n3e = n3 if upto >= 3 else 0
            AB2 = [(A0, "A0"), (A1, "A1")]

            def bufs(i):
                p2 = i % 2
                xb2_, kxb2 = (xb2[:], "xb2") if p2 == 0 else (stage[1][:], "stage1")
                xnb_, kxnb = (xnb[:], "xnbP0") if p2 == 0 else (sqmf[:, 0:1024], "xnbP1")
                eidT_, keid = (eidT, "eidT0") if p2 == 0 else (eidT2, "eidT1")
                gT_, kgT = (gT, "gT0") if p2 == 0 else (gT2, "gT1")
                return xb2_, kxb2, xnb_, kxnb, eidT_, keid, gT_, kgT

            def stage_S(i):
                xb2_, kxb2, xnb_, kxnb, eidT_, keid, gT_, kgT = bufs(i)
                P.dma(xa[:], x1d[128 * i:128 * i + 128, :], reads=["x1d"], writes=["xa"])
                norm_T(xa[:], "xa", xT[:], "xT", xbo=(sqmf[:, 1024:2048], "xnbS"))
                for oc in range(8):
                    pt_, kp = AB2[oc // 4]
                    for c in range(8):
                        mm(pt_[:, 128 * (oc % 4):128 * (oc % 4) + 128], wmq[:, c, 128 * oc:128 * oc + 128], xT[:, c, :],
                           oc % 4 == 0 and c == 0, oc % 4 == 3 and c == 7, ["wmq", "xT"], [kp])
                for hh, (pt_, kp) in enumerate(AB2):
                    actf(qmT[:, 4 * hh:4 * hh + 4, :].rearrange("p a b -> p (a b)"), pt_, AF.Copy, [kp], ["qmT"], scale=1.0 / 16)
                    actf(qmsq[:, 4 * hh:4 * hh + 4, :].rearrange("p a b -> p (a b)"), pt_, AF.Square, [kp], ["qmsq"])
                for hm in range(4):
                    for dc in range(2):
                        mm(A0[0:1, 128 * hm:128 * hm + 128], cneg[:, 2:3], qmsq[:, 2 * hm + dc, :], hm == 0 and dc == 0,
                           hm == 3 and dc == 1, ["cneg", "qmsq"], ["A0"])
                cp(negbm[0:1, :], A0[0:1, :], ["A0"], ["negbm"])
                for mc in range(2):
                    sp_, ksp = AB2[mc]
                    for hm in range(4):
                        for dc in range(2):
                            mm(sp_[:, 128 * hm:128 * hm + 128], KmT[:, 2 * hm + dc, 128 * mc:128 * mc + 128],
                               qmT[:, 2 * hm + dc, :], hm == 0 and dc == 0, False, ["KmT", "qmT"], [ksp])
                    mm(sp_, ones_b[0:1, :], negbm[0:1, :], False, True, ["ones_b", "negbm"], [ksp])
                    actf(PTm[mc][:], sp_, AF.Exp, [ksp, "expbm"], ["PTm%d" % mc], bias=expbm)
                for hm in range(4):
                    for mc in range(2):
                        mm(A0[:, 128 * hm:128 * hm + 128], ones_b[:], PTm[mc][:, 128 * hm:128 * hm + 128],
                           hm == 0 and mc == 0, hm == 3 and mc == 1, ["ones_b", "PTm%d" % mc], ["A0"])
                P.dve(lambda e: e.reciprocal(out=rzb[:], in_=A0), ["A0"], ["rzb"])
                for oc in range(8):
                    hm = oc // 2
                    pt_, kp = AB2[oc // 4]
                    for mc in range(2):
                        mm(pt_[:, 128 * (oc % 4):128 * (oc % 4) + 128], Vm[:, mc, 128 * oc:128 * oc + 128],
                           PTm[mc][:, 128 * hm:128 * hm + 128], oc % 4 == 0 and mc == 0, oc % 4 == 3 and mc == 1,
                           ["Vm", "PTm%d" % mc], [kp])
                for oc in range(8):
                    hm = oc // 2
                    pt_, kp = AB2[oc // 4]
                    tt(oT[:, oc, :], pt_[:, 128 * (oc % 4):128 * (oc % 4) + 128], rzb[:, 128 * hm:128 * hm + 128], ALU.mult,
                       [kp, "rzb"], ["oT"])
                for hh, (pt_, kp) in enumerate(AB2):
                    cs = slice(512 * hh, 512 * hh + 512)
                    for oc in range(8):
                        mm(pt_, oT[:, oc, :], wmo[:, oc, cs], oc == 0, oc == 7, ["oT", "wmo"], [kp])
                    tt(xb2_[:, cs], pt_, xa[:, cs], ALU.add, [kp, "xa"], [kxb2])
                if dbg:
                    P.dma(dbgo["d_x2"][128 * i:128 * i + 128, :], xb2_, reads=[kxb2])
                norm_T(xb2_, kxb2, xT[:], "xT", gain=gff[:], xnf=xnf[:], xbo=(xnb_, kxnb))
                for half in range(2):
                    for h8 in range(8):
                        hc = 8 * half + h8
                        pt_, kp = AB2[h8 // 4]
                        for c in range(8):
                            mm(pt_[:, 128 * (hc % 4):128 * (hc % 4) + 128], wpq[:, c, 128 * hc:128 * hc + 128], xT[:, c, :],
                               hc % 4 == 0 and c == 0, hc % 4 == 3 and c == 7, ["wpq", "xT"], [kp])
                    for q2, (pt_, kp) in enumerate(AB2):
                        q4 = 2 * half + q2
                        cp(qpT[:, 4 * q4:4 * q4 + 4, :].rearrange("p a b -> p (a b)"), pt_, [kp], ["qpT"],
                           eng='act' if q4 % 2 else 'dve')
                for half in range(2):
                    for h8 in range(8):
                        hc = 8 * half + h8
                        pt_, kp = AB2[h8 // 4]
                        mm(pt_[:, 128 * (hc % 4):128 * (hc % 4) + 128], qpT[:, hc, :], sk[:, hc, :], hc % 4 == 0, hc % 4 == 3,
                           ["qpT", "sk"], [kp])
                    for q2, (pt_, kp) in enumerate(AB2):
                        q4 = 2 * half + q2
                        cp(s_sb[:, 4 * q4:4 * q4 + 4, :].rearrange("p a b -> p (a b)"), pt_, [kp], ["s_sb"],
                           eng='act' if q4 % 2 else 'dve')
                for hc in range(16):
                    sv = s_sb[:, hc, :]
                    P.dve((lambda hc, sv: lambda e: e.max(out=v16[:, hc, 0:8], in_=sv))(hc, sv), ["s_sb"], ["v16"])
                    P.dve((lambda hc, sv: lambda e: e.max_index(out=i16[:, hc, 0:8], in_max=v16[:, hc, 0:8], in_values=sv))(hc, sv),
                          ["s_sb", "v16"], ["i16"])
                    P.dve((lambda hc, sv: lambda e: e.match_replace(out=s2[:], in_to_replace=v16[:, hc, 0:8], in_values=sv,
                                                                     imm_value=-1e30))(hc, sv), ["s_sb", "v16"], ["s2"])
                    P.dve((lambda hc: lambda e: e.max(out=v16[:, hc, 8:16], in_=s2[:]))(hc), ["s2"], ["v16"])
                    P.dve((lambda hc: lambda e: e.max_index(out=i16[:, hc, 8:16], in_max=v16[:, hc, 8:16], in_values=s2[:]))(hc),
                          ["s2", "v16"], ["i16"])
                cp(i16f[:], i16[:], ["i16"], ["i16f"])
                for h in range(8):
                    tt(cand[:, h, :].rearrange("p (a b) -> p a b", a=16),
                       v16[:, 2 * h, :].unsqueeze(2).to_broadcast([128, 16, 16]),
                       v16[:, 2 * h + 1, :].unsqueeze(1).to_broadcast([128, 16, 16]), ALU.add, ["v16"], ["cand"])
                for h in range(8):
                    cv = cand[:, h, :]
                    P.dve((lambda h, cv: lambda e: e.max(out=top[:, h, 0:8], in_=cv))(h, cv), ["cand"], ["top"])
                    P.dve((lambda h, cv: lambda e: e.max_index(out=pos[:, h, 0:8], in_max=top[:, h, 0:8], in_values=cv))(h, cv),
                          ["cand", "top"], ["pos"])
                    P.dve((lambda h, cv: lambda e: e.match_replace(out=cand2[:], in_to_replace=top[:, h, 0:8], in_values=cv,
                                                                    imm_value=-1e30))(h, cv), ["cand", "top"], ["cand2"])
                    P.dve((lambda h: lambda e: e.max(out=top[:, h, 8:16], in_=cand2[:]))(h), ["cand2"], ["top"])
                    P.dve((lambda h: lambda e: e.max_index(out=pos[:, h, 8:16], in_max=top[:, h, 8:16], in_values=cand2[:]))(h),
                          ["cand2", "top"], ["pos"])
                cp(posf[:], pos[:].rearrange("p a b -> p (a b)"), ["pos"], ["posf"])
                ts(ai[:], posf[:], -7.5, 0.0625, ALU.add, ALU.mult, ["posf"], ["ai"])
                cp(af[:], ai[:], ["ai"], ["af"])
                stt(bfl[:], af[:], -16.0, posf[:], ALU.mult, ALU.add, ["af", "posf"], ["bfl"])
                eq3 = cid[:].rearrange("p h (k a) -> p (h k) a", a=16)
                eq4 = cid[:].rearrange("p h (k a) -> p h k a", a=16)
                iob = ctb[:, 132:148].unsqueeze(1).to_broadcast([128, 128, 16])
                for (src_, par, dst_) in [(af, 0, e1), (bfl, 1, e2)]:
                    tt(eq3, src_[:].unsqueeze(2).to_broadcast([128, 128, 16]), iob, ALU.is_equal, [("af" if par == 0 else "bfl"), "ctb"], ["cid"])
                    tt(eq4, eq4, i16f[:, par:16:2, :].unsqueeze(2).to_broadcast([128, 8, 16, 16]), ALU.mult, ["cid", "i16f"], ["cid"])
                    P.dve((lambda dst_: lambda e: e.tensor_reduce(out=dst_[:], in_=eq3, axis=AX.X, op=ALU.add))(dst_), ["cid"],
                          ["e%d" % par])
                stt(eid[:], e1[:], 128.0, e2[:], ALU.mult, ALU.add, ["e0", "e1"], ["eid"])
                tt(gw[:], top[:], top[:, :, 0:1].to_broadcast([128, 8, 16]), ALU.subtract, ["top"], ["gw"])
                actf(gw[:].rearrange("p a b -> p (a b)"), gw[:].rearrange("p a b -> p (a b)"), AF.Exp, ["gw"], ["gw"])
                P.dve(lambda e: e.tensor_reduce(out=gsum[:], in_=gw[:], axis=AX.X, op=ALU.add), ["gw"], ["gsum"])
                P.dve(lambda e: e.reciprocal(out=gsum[:], in_=gsum[:]), ["gsum"], ["gsum"])
                tt(gw[:], gw[:], gsum[:].unsqueeze(2).to_broadcast([128, 8, 16]), ALU.mult, ["gw", "gsum"], ["gw"])
                ts(eid[:], eid[:], 16383.0, 0.0, ALU.min, ALU.max, ["eid"], ["eid"])
                trp(A0[:, 0:128], eid[:], identf[:], ["eid", "identf"], ["A0"])
                cp(eidT_[:], A0[:, 0:128], ["A0"], [keid])
                trp(A1[:, 0:128], gw[:].rearrange("p a b -> p (a b)"), identf[:], ["gw", "identf"], ["A1"])
                cp(gT_[:], A1[:, 0:128], ["A1"], [kgT])

            LAG = 3

            def token_loop(i, q):
                xb2_, kxb2, xnb_, kxnb, eidT_, keid, gT_, kgT = bufs(i)
                XA = [(c0, "c0"), (c2, "c2")]
                pb, kb_ = c1, "c1"
                nsteps = 128 + LAG
                caps = {'pe': 10, 'dve': 4, 'act': 3, 'pool': 2, 'sp': 2}
                wstep, weng, rstep, load = {}, {}, {}, {}
                lastst = {e_: 0 for e_ in ENGS}
                buckets = [[] for _ in range(nsteps)]
                tail = []
                for item in q:
                    eng_, _fn, reads_, writes_, _dma = item
                    s_ = lastst[eng_]
                    for k_ in tuple(reads_) + tuple(writes_):
                        if k_ in wstep:
                            s_ = max(s_, wstep[k_] + (1 if weng[k_] != eng_ else 0))
                    for k_ in writes_:
                        for e2_, st_ in rstep.get(k_, {}).items():
                            s_ = max(s_, st_ + (1 if e2_ != eng_ else 0))
                    while load.get((s_, eng_), 0) >= caps[eng_]:
                        s_ += 1
                    load[(s_, eng_)] = load.get((s_, eng_), 0) + 1
                    lastst[eng_] = s_
                    for k_ in writes_:
                        wstep[k_] = s_
                        weng[k_] = eng_ if not _dma else 'dma'
                        rstep[k_] = {}
                    for k_ in reads_:
                        if k_ not in writes_:
                            d_ = rstep.setdefault(k_, {})
                            d_[eng_] = max(d_.get(eng_, 0), s_)
                    (buckets[s_] if s_ < nsteps else tail).append(item)

                def u_side(t):
                    u = t % NB
                    P.op('pool', (lambda t, u: lambda e: e.indirect_dma_start(
                        out=UVg[u][:], out_offset=None, in_=uv16,
                        in_offset=bass.IndirectOffsetOnAxis(ap=eidT_[:, t:t + 1], axis=0)))(t, u),
                        [keid], ["UVg%d" % u], dma=True)
                    s2_ = t % 2
                    actf(selt[s2_][:], ones_b[:], AF.Copy, ["ones_b", "identf"], ["selt%d" % s2_], scale=identf[:, t:t + 1])
                    pa, ka = XA[s2_]
                    mm(pa, selt[s2_][:], xnb_[:, 0:512], True, True, ["selt%d" % s2_, kxnb], [ka])
                    mm(pb, selt[s2_][:], xnb_[:, 512:1024], True, True, ["selt%d" % s2_, kxnb], [kb_])
                    stt(jk[0][:], UVg[u][:, 0:512], 1.0, pa, ALU.mult, ALU.mult, ["UVg%d" % u, ka], ["ha%d" % t, "jk0"],
                        accum=hTa[:, t:t + 1])
                    stt(jk[1][:], UVg[u][:, 512:1024], 1.0, pb, ALU.mult, ALU.mult, ["UVg%d" % u, kb_], ["hb%d" % t, "jk1"],
                        accum=hTb[:, t:t + 1])

                def v_pre(t):
                    actf(gh[:, t:t + 1], hTa[:, t:t + 1], AF.Gelu_apprx_tanh, ["ha%d" % t, "hb%d" % t], ["gh%d" % t],
                         bias=hTb[:, t:t + 1])
                    d2 = t % 2
                    actf(gh[:, t:t + 1], gh[:, t:t + 1], AF.Copy, ["gh%d" % t, kgT], ["gh%d" % t], scale=gT_[:, t:t + 1])
                    actf(Dt[d2][:], Rsel[:, 127 - t:255 - t], AF.Copy, ["Rsel", "gh%d" % t], ["Dt%d" % d2],
                         scale=gh[:, t:t + 1])

                def v_mm(t):
                    u = t % NB
                    d2 = t % 2
                    mm(B0, Dt[d2][:], UVg[u][:, 1024:1536], t == 0, t == 127, ["Dt%d" % d2, "UVg%d" % u], ["B0"])
                    mm(B1, Dt[d2][:], UVg[u][:, 1536:2048], t == 0, t == 127, ["Dt%d" % d2, "UVg%d" % u], ["B1"])

                for step in range(128 + LAG):
                    P.drain(buckets[step], len(buckets[step]))
                    if step >= LAG:
                        v_pre(step - LAG)
                    if step < 128:
                        u_side(step)
                    if step >= LAG:
                        v_mm(step - LAG)
                P.drain(tail, len(tail))
                tt(xa[:], B[:, :], xb2_, ALU.add, ["B0", "B1", kxb2], ["xa"])
                actf(cid[:].rearrange("p a b -> p (a b)")[:, 0:1024], xa[:], AF.Square, ["xa"], ["cid", "ssq3"], accum=ssq[:, 3:4])
                calc_rstd(3, 1024)
                stt(yo[:], xa[:], rstd[:, 3:4], gfi[:], ALU.mult, ALU.mult, ["xa", "rstd3", "gfi"], ["xnf"])
                P.dma(yout[128 * i:128 * i + 128, :], yo[:], reads=["xnf"])

            if n3e > 0:
                stage_S(0)
            for i in range(n3e):
                q = []
                if i + 1 < n3e:
                    P.capture_begin()
                    stage_S(i + 1)
                    q = P.capture_end()
                token_loop(i, q)
            P.emit()
    return nc


_NC_CACHE = {}


def _prep_shared(inp):
    f = np.float32
    w_in = np.asarray(inp['w_in'][0], f)
    offs = np.cumsum([0, 512, 512, 512, 128, 128, 128, 128, 128, 128, 24])
    x_lru, gate, q, k_c, v_c, k_s, v_s, k_w, v_w, g_raw = [w_in[:, offs[j]:offs[j + 1]] for j in range(10)]
    qp = q.reshape(1024, 2, 4, 64).transpose(0, 2, 1, 3).reshape(1024, 512)
    wcat = np.ascontiguousarray(np.concatenate([x_lru, k_c, v_c, k_s, k_w, v_s, v_w, gate, qp, g_raw], axis=1))

    def pc(v):
        return np.asarray(v, f).reshape(8, 128).T

    gout = np.concatenate([np.asarray(inp['g_out_lru'][0], f), np.asarray(inp['g_out_nsa'][0], f)])
    gvec = np.zeros((128, 48), f)
    gvec[:, 0:8] = pc(inp['g_mix'][0])
    gvec[:, 8:16] = pc(gout)
    gvec[:, 16:24] = pc(inp['g_mem_q'][0])
    gvec[:, 24:32] = pc(inp['g_mem_kv'][0])
    bgate = np.tile(np.asarray(inp['b_gate'][0], f)[None, :], (128, 1))
    lrup = np.zeros((128, 36), f)
    cwv = np.asarray(inp['conv_w'][0], f)
    lrup[:, 0:16] = cwv.reshape(4, 4, 128).transpose(2, 1, 0).reshape(128, 16)
    lrup[:, 16:20] = np.asarray(inp['conv_b'][0], f).reshape(4, 128).T
    lrup[:, 20:24] = np.asarray(inp['b_rg_a'][0], f).reshape(4, 128).T
    lrup[:, 24:28] = np.asarray(inp['b_rg_i'][0], f).reshape(4, 128).T
    lrup[:, 28:32] = np.asarray(inp['lam'][0], f).reshape(4, 128).T

    def bd(w):
        o = np.zeros((128, 4, 128), f)
        for ch in range(4):
            o[0:64, ch, 0:64] = w[2 * ch]
            o[64:128, ch, 64:128] = w[2 * ch + 1]
        return o.reshape(128, 512)

    wabd = bd(np.asarray(inp['w_rg_a'][0], f))
    wibd = bd(np.asarray(inp['w_rg_i'][0], f))

    def w1bd(w1):
        w = np.asarray(w1, f).reshape(32, 64, 64)
        o = np.zeros((128, 32, 128), f)
        o[0:64, :, 0:64] = w.transpose(1, 0, 2)
        o[64:128, :, 64:128] = w.transpose(1, 0, 2)
        return o.reshape(128, 4096)

    def w2bd(w2):
        o = np.zeros((128, 128), f)
        o[0:64, 0:64] = w2
        o[64:128, 64:128] = w2
        return o

    def posT(p):
        return np.ascontiguousarray(np.tile(np.asarray(p, f).T, (2, 1)))

    npr = np.arange(512)
    ci = (npr - 1) * 16
    bj = np.arange(128) * 64
    ovl = ((ci[:, None] < bj[None, :] + 64) & (ci[:, None] + 32 > bj[None, :])).astype(f)
    ovl[0, :] = 0.0
    ctab = np.zeros((128, 148), f)
    ctab[:, 132:148] = np.arange(16, dtype=f)[None, :]
    p = np.arange(128)
    for nt in range(4):
        ctab[:, nt] = 16.0 * (128 * nt + p) + 15.0
    ctab[0, 0] = 1e9
    for kb in range(64):
        ctab[:, 4 + kb] = 128.0 * kb + p
        ctab[:, 68 + kb] = 128.0 * kb + p + 512.0
    skT = np.ascontiguousarray(np.asarray(inp['sub_keys'][0], f).reshape(16, 128, 128).transpose(2, 0, 1).reshape(128, 2048))
    sh = dict(
        wcat=wcat, gvec=gvec, bgate=bgate, wabd=wabd, wibd=wibd,
        w1k=w1bd(inp['cmp_w1_k'][0]), w1v=w1bd(inp['cmp_w1_v'][0]),
        w2k=w2bd(np.asarray(inp['cmp_w2_k'][0], f)), w2v=w2bd(np.asarray(inp['cmp_w2_v'][0], f)),
        posk=posT(inp['cmp_pos_k'][0]), posv=posT(inp['cmp_pos_v'][0]), ovl=ovl, ctab=ctab,
        w_out=np.ascontiguousarray(np.asarray(inp['w_out'][0], f)),
        w_mq=np.ascontiguousarray(np.asarray(inp['w_mq'][0], f)),
        w_mk=np.ascontiguousarray(np.asarray(inp['w_mk'][0], f)),
        w_mv=np.ascontiguousarray(np.asarray(inp['w_mv'][0], f)),
        w_mo=np.ascontiguousarray(np.asarray(inp['w_mo'][0], f)),
        w_pq=np.ascontiguousarray(np.asarray(inp['w_pq'][0], f)),
        skT=skT,
        gffn=np.asarray(inp['g_ffn'][0], f).reshape(1, 1024),
        gfin=np.asarray(inp['g_final'], f).reshape(1, 1024),
        peer_u=np.ascontiguousarray(np.asarray(inp['peer_u'][0], f)),
        peer_v=np.ascontiguousarray(np.asarray(inp['peer_v'][0], f)),
    )
    return sh, lrup


def _own_idx(c):
    return np.concatenate([128 * (4 * i + c) + np.arange(128) for i in range(16)])


def _prep_core(inp, sh, lrup, core):
    f = np.float32
    b, c = core // 4, core % 4
    own = _own_idx(c)
    x = np.asarray(inp['x'], f)
    m = dict(sh)
    m['xfull'] = np.ascontiguousarray(x[b])
    m['xown'] = np.ascontiguousarray(x[b][own])
    m['memb'] = np.ascontiguousarray(np.asarray(inp['mem'], f)[b])
    lp = lrup.copy()
    lp[:, 32 + c] = 1.0
    m['lrup'] = lp
    tq = own.reshape(16, 128).astype(f)
    m['tq4'] = np.ascontiguousarray(np.broadcast_to(np.tile(tq, (1, 4))[:, None, :], (16, 128, 512)))
    cur = (own // 64).reshape(16, 128)
    j = np.arange(128)[None, None, :]
    cu = cur[:, :, None]
    fbt = np.where(j > cu, -1e9, 0.0).astype(f)
    forced = ((j == 0) | (j == cu) | (j == cu - 1)) & (j <= cu)
    fbt = np.where(forced, 1e6 * (1.0 + j), fbt).astype(f)
    m['fbt'] = np.ascontiguousarray(fbt)
    return m


def kernel(**inputs):
    if 'nc' not in _NC_CACHE:
        _NC_CACHE['nc'] = build(False)
    nc = _NC_CACHE['nc']
    sh, lrup = _prep_shared(inputs)
    in_maps = [_prep_core(inputs, sh, lrup, core) for core in range(8)]
    res = run_bass_kernel_spmd(nc, in_maps, core_ids=list(range(8)))
    out = np.zeros((2, 8192, 1024), np.float32)
    for core in range(8):
        b, c = core // 4, core % 4
        out[b, _own_idx(c)] = res.results[core]["yout"]
    return out
```

```python
import numpy as np
import concourse.bass as bass
import concourse.mybir as mybir
from concourse.bass_utils import run_bass_kernel_spmd
from contextlib import ExitStack

F32 = mybir.dt.float32
BF16 = mybir.dt.bfloat16
U32 = mybir.dt.uint32
I32 = mybir.dt.int32
AF = mybir.ActivationFunctionType
ALU = mybir.AluOpType
AX = mybir.AxisListType

ENGS = ['pe', 'dve', 'act', 'pool', 'sp']
NDMA = 24
NSW = 8
SAME_ENG_SYNC = {'pe': False, 'dve': True, 'act': True, 'pool': True, 'sp': True}
NEG = -30000.0
_CUT = 99
EPS = 1e-6


class Prog:
    def __init__(self, nc, ctx):
        self.nc = nc
        self.ops = {e: [] for e in ENGS}
        self.cnt = {e: 0 for e in ENGS}
        self.sems = {e: ctx.enter_context(nc.semaphore("s_" + e)) for e in ENGS}
        self.dsem = [ctx.enter_context(nc.semaphore("d%d" % i)) for i in range(NDMA + NSW)]
        self.dcnt = [0] * (NDMA + NSW)
        self.drr = 0
        self.srr = 0
        self.last_w = {}
        self.readers = {}
        self.seen = {e: {} for e in ENGS}
        self.defer = None

    def capture_begin(self):
        self.defer = []

    def capture_end(self):
        q, self.defer = self.defer, None
        return q

    def drain(self, q, n):
        for _ in range(min(n, len(q))):
            self.op(*q.pop(0))

    def op(self, eng, fn, reads=(), writes=(), dma=False):
        if self.defer is not None:
            self.defer.append((eng, fn, tuple(reads), tuple(writes), dma))
            return None
        deps = {}

        def add(tok):
            if tok is None:
                return
            sem, val, teng, sid = tok
            if teng == eng and not SAME_ENG_SYNC[eng]:
                return
            if sid not in deps or deps[sid][1] < val:
                deps[sid] = tok

        for k in reads:
            add(self.last_w.get(k))
        for k in writes:
            add(self.last_w.get(k))
            for t in self.readers.get(k, {}).values():
                add(t)
        slot = None
        if dma:
            if eng == 'pool':
                slot = NDMA + self.srr
                self.srr = (self.srr + 1) % NSW
            else:
                slot = self.drr
                self.drr = (self.drr + 1) % NDMA
            if self.dcnt[slot] > 0:
                add((self.dsem[slot], self.dcnt[slot], 'dma', ('d', slot)))
        waits = []
        seen = self.seen[eng]
        for sid, tok in deps.items():
            if seen.get(sid, 0) >= tok[1]:
                continue
            seen[sid] = tok[1]
            waits.append((tok[0], tok[1]))
        if dma:
            self.dcnt[slot] += 16
            tok = (self.dsem[slot], self.dcnt[slot], 'dma', ('d', slot))
            inc = (self.dsem[slot], 16)
        else:
            self.cnt[eng] += 1
            tok = (self.sems[eng], self.cnt[eng], eng, ('e', eng))
            inc = (self.sems[eng], 1)
        self.ops[eng].append((fn, waits, inc))
        for k in writes:
            self.last_w[k] = tok
            self.readers[k] = {}
        for k in reads:
            if k in writes:
                continue
            r = self.readers.setdefault(k, {})
            if tok[3] not in r or r[tok[3]][1] < tok[1]:
                r[tok[3]] = tok
        return tok

    def pe(self, fn, reads=(), writes=()):
        return self.op('pe', fn, reads, writes)

    def dve(self, fn, reads=(), writes=()):
        return self.op('dve', fn, reads, writes)

    def act(self, fn, reads=(), writes=()):
        return self.op('act', fn, reads, writes)

    def pool(self, fn, reads=(), writes=()):
        return self.op('pool', fn, reads, writes)

    def dma(self, out, in_, reads=(), writes=(), eng='sp'):
        return self.op(eng, lambda e: e.dma_start(out=out, in_=in_), reads, writes, dma=True)

    def emit(self):
        nc = self.nc
        fin = []
        for e in ENGS:
            if e != 'sp' and self.cnt[e] > 0:
                fin.append((self.sems[e], self.cnt[e]))
        for i in range(NDMA + NSW):
            if self.dcnt[i] > 0:
                fin.append((self.dsem[i], self.dcnt[i]))
        ops = self.ops

        def run(e, lst):
            for fn, waits, inc in lst:
                for s, v in waits:
                    e.wait_ge(s, v)
                ins = fn(e)
                ins.then_inc(inc[0], inc[1])

        with nc.Block() as block:
            @block.sync
            def _(e):
                run(e, ops['sp'])
                for s, v in fin:
                    e.wait_ge(s, v)

            @block.tensor
            def _(e):
                run(e, ops['pe'])

            @block.vector
            def _(e):
                run(e, ops['dve'])

            @block.scalar
            def _(e):
                run(e, ops['act'])

            @block.gpsimd
            def _(e):
                run(e, ops['pool'])
        self.ops = {e: [] for e in ENGS}
        self.last_w = {}
        self.readers = {}
        for e in ENGS:
            for sid in list(self.seen[e].keys()):
                pass


def build(dbg=False, upto=3, n1=16, n2=16, n3=16):
    nc = bass.Bass("TRN2", target_bir_lowering=False)

    def din(name, shape, dt=F32):
        return nc.dram_tensor(name, shape, dt, kind="ExternalInput").ap()

    xfull = din("xfull", [8192, 1024])
    xown = din("xown", [2048, 1024])
    memb = din("memb", [256, 1024])
    wcat = din("wcat", [1024, 2328])
    gvec = din("gvec", [128, 48])
    bgate = din("bgate", [128, 24])
    lrup = din("lrup", [128, 36])
    wabd = din("wabd", [128, 512])
    wibd = din("wibd", [128, 512])
    w1k = din("w1k", [128, 4096])
    w1v = din("w1v", [128, 4096])
    w2k = din("w2k", [128, 128])
    w2v = din("w2v", [128, 128])
    posk = din("posk", [128, 32])
    posv = din("posv", [128, 32])
    ovl = din("ovl", [512, 128])
    ctab = din("ctab", [128, 148])
    tq4 = din("tq4", [16, 128, 512])
    fbt = din("fbt", [16, 128, 128])
    w_out = din("w_out", [1024, 1024])
    w_mq = din("w_mq", [1024, 1024])
    w_mk = din("w_mk", [1024, 1024])
    w_mv = din("w_mv", [1024, 1024])
    w_mo = din("w_mo", [1024, 1024])
    w_pq = din("w_pq", [1024, 2048])
    skT = din("skT", [128, 2048])
    gffn = din("gffn", [1, 1024])
    gfin = din("gfin", [1, 1024])
    peer_u = din("peer_u", [16384, 1024])
    peer_v = din("peer_v", [16384, 1024])
    x1d = nc.dram_tensor("x1d", [2048, 1024], F32, kind="Internal").ap()
    uv16 = nc.dram_tensor("uv16", [16384, 2048], BF16, kind="Internal").ap()
    yout = nc.dram_tensor("yout", [2048, 1024], F32, kind="ExternalOutput").ap()
    dbgo = {}
    if dbg:
        for nm, shp in [("d_yl", [128, 4 * 2048]), ("d_kc", [128, 512]), ("d_vc", [128, 4 * 2 * 196]),
                        ("d_yn", [2048, 512]), ("d_x2", [2048, 1024]), ("d_ks", [128, 8192]),
                        ("d_vs", [128, 64 * 132])]:
            dbgo[nm] = nc.dram_tensor(nm, shp, F32, kind="ExternalOutput").ap()

    with ExitStack() as ctx:
        P = Prog(nc, ctx)

        def sbt(c, name, shape, dt=F32):
            return c.enter_context(nc.sbuf_tensor(name, shape, dt))

        def pst(c, name, shape, dt=F32):
            return c.enter_context(nc.psum_tensor(name, shape, dt))

        def mm(out, lhsT, rhs, start, stop, r, w):
            P.pe(lambda e: e.matmul(out, lhsT=lhsT, rhs=rhs, start=start, stop=stop), reads=r, writes=w)

        def trp(out, in_, ident, r, w):
            P.pe(lambda e: e.transpose(out, in_, ident), reads=r, writes=w)

        def actf(out, in_, func, r, w, bias=0.0, scale=1.0, accum=None):
            if accum is None:
                P.act(lambda e: e.activation(out=out, in_=in_, func=func, bias=bias, scale=scale), reads=r, writes=w)
            else:
                P.act(lambda e: e.activation(out=out, in_=in_, func=func, bias=bias, scale=scale, accum_out=accum),
                      reads=r, writes=w)

        def ts(out, in0, s1, s2, op0, op1, r, w, eng='dve'):
            if op1 is None:
                P.op(eng, lambda e: e.tensor_scalar(out=out, in0=in0, scalar1=s1, scalar2=None, op0=op0), r, w)
            else:
                P.op(eng, lambda e: e.tensor_scalar(out=out, in0=in0, scalar1=s1, scalar2=s2, op0=op0, op1=op1), r, w)

        def tt(out, in0, in1, op, r, w, eng='dve'):
            P.op(eng, lambda e: e.tensor_tensor(out=out, in0=in0, in1=in1, op=op), r, w)

        def stt(out, in0, scalar, in1, op0, op1, r, w, accum=None):
            if accum is None:
                P.dve(lambda e: e.scalar_tensor_tensor(out=out, in0=in0, scalar=scalar, in1=in1, op0=op0, op1=op1), r, w)
            else:
                P.dve(lambda e: e.scalar_tensor_tensor(out=out, in0=in0, scalar=scalar, in1=in1, op0=op0, op1=op1,
                                                       accum_out=accum), r, w)

        def cp(out, in_, r, w, eng='dve'):
            if eng == 'act':
                P.act(lambda e: e.activation(out=out, in_=in_, func=AF.Copy), r, w)
            else:
                P.op(eng, lambda e: e.tensor_copy(out=out, in_=in_), r, w)

        def mset(ap, val, w, eng='dve'):
            P.op(eng, lambda e: e.memset(ap, val), (), w)

        identf = sbt(ctx, "identf", [128, 128])
        identb = sbt(ctx, "identb", [128, 128], BF16)
        ones_b = sbt(ctx, "ones_b", [128, 128], BF16)
        mhalf = sbt(ctx, "mhalf", [128, 4])
        cneg = sbt(ctx, "cneg", [128, 4], BF16)
        ssq = sbt(ctx, "ssq", [128, 8])
        rstd = sbt(ctx, "rstd", [128, 8])
        xnb = sbt(ctx, "xnb", [128, 1024], BF16)
        stage = [sbt(ctx, "stage0", [128, 1024]), sbt(ctx, "stage1", [128, 1024])]
        stg_i = [0]
        A = pst(ctx, "psA", [128, 1024])
        B = pst(ctx, "psB", [128, 1024])
        c0 = pst(ctx, "psc0", [128, 512])[:]
        c1 = pst(ctx, "psc1", [128, 512])[:]
        c2 = pst(ctx, "psc2", [128, 512])[:]
        ptb = pst(ctx, "ptb", [128, 1024], BF16)
        A0, A1 = A[:, 0:512], A[:, 512:1024]
        B0, B1 = B[:, 0:512], B[:, 512:1024]

        mset(identf[:], 1.0, ["identf"])
        P.pool(lambda e: e.affine_select(out=identf[:], in_=identf[:], pattern=[[-1, 128]], compare_op=ALU.is_equal,
                                         fill=0.0, base=0, channel_multiplier=1), ["identf"], ["identf"])
        cp(identb[:], identf[:], ["identf"], ["identb"])
        mset(ones_b[:], 1.0, ["ones_b"])
        mset(mhalf[:], -0.5, ["mhalf"])
        Rsel = sbt(ctx, "Rsel", [128, 256])
        mset(Rsel[:], 0.0, ["Rsel"])
        mset(Rsel[:, 127:128], 1.0, ["Rsel"])
        cnegM = sbt(ctx, "cnegM", [128, 2, 128], BF16)
        mset(cnegM[:], 0.0, ["cnegM"])
        mset(cnegM[0:64, 0, :], -1.0 / 16, ["cnegM"])
        mset(cnegM[64:128, 1, :], -1.0 / 16, ["cnegM"])
        mset(cneg[:], 0.0, ["cneg"])
        mset(cneg[0:64, 0:1], -1.0 / 16, ["cneg"])
        mset(cneg[64:128, 1:2], -1.0 / 16, ["cneg"])
        mset(cneg[:, 2:3], -1.0 / 32, ["cneg"])

        def calc_rstd(col, n):
            ts(ssq[:, col:col + 1], ssq[:, col:col + 1], 1.0 / n, EPS, ALU.mult, ALU.add, ["ssq%d" % col], ["ssq%d" % col])
            tt(rstd[:, col:col + 1], ssq[:, col:col + 1], mhalf[:, 0:1], ALU.pow, ["ssq%d" % col, "mhalf"],
               ["rstd%d" % col], eng='pool')

        xnb_alt = [None]

        def norm_T(x_sb, kx, dstT, kdst, gain=None, xnf=None, par=0, xbo=None):
            xb_, kxb, col = (xnb[:], "xnb", 0) if par == 0 else (stage[1][:].bitcast(BF16)[:, 0:1024], "stage1", 4)
            if xbo is not None:
                xb_, kxb = xbo
            actf(xb_, x_sb, AF.Square, [kx], [kxb, "ssq%d" % col], accum=ssq[:, col:col + 1])
            calc_rstd(col, 1024)
            if gain is None:
                actf(xb_, x_sb, AF.Copy, [kx, "rstd%d" % col], [kxb], scale=rstd[:, col:col + 1])
            else:
                stt(xnf, x_sb, rstd[:, col:col + 1], gain, ALU.mult, ALU.mult, [kx, "rstd%d" % col, "gain"], ["xnf"])
                cp(xb_, xnf, ["xnf"], [kxb], eng='act')
            for c in range(8):
                trp(ptb[:, 128 * c:128 * c + 128], xb_[:, 128 * c:128 * c + 128], identb[:], [kxb, "identb"], ["ptb"])
            cp(dstT, ptb[:].rearrange("p (c t) -> p c t", c=8), ["ptb"], [kdst])

        def load_w(dst, kdst, src, ncols, col0=0, gain=None, engs=('dve', 'pool')):
            k = 0
            for c in range(8):
                for o in range(0, ncols, 1024):
                    w = min(1024, ncols - o)
                    si = stg_i[0] % 2
                    stg_i[0] += 1
                    st = stage[si]
                    P.dma(st[:, 0:w], src[128 * c:128 * c + 128, col0 + o:col0 + o + w], writes=["stage%d" % si])
                    eng = engs[k % len(engs)]
                    k += 1
                    if gain is None:
                        cp(dst[:, c, o:o + w], st[:, 0:w], ["stage%d" % si], [kdst], eng=eng)
                    else:
                        ts(dst[:, c, o:o + w], st[:, 0:w], gain[:, c:c + 1], None, ALU.mult, None,
                           ["stage%d" % si, "gv"], [kdst], eng=eng)

        def load_small(dst, kdst, src, w, eng='dve'):
            for o in range(0, w, 1024):
                ww = min(1024, w - o)
                si = stg_i[0] % 2
                stg_i[0] += 1
                st = stage[si]
                P.dma(st[:, 0:ww], src[:, o:o + ww], writes=["stage%d" % si])
                cp(dst[:, o:o + ww], st[:, 0:ww], ["stage%d" % si], [kdst], eng=eng)

        gv = sbt(ctx, "gv", [128, 48])
        P.dma(gv[:], gvec, writes=["gv"])
        ctb = sbt(ctx, "ctb", [128, 148])
        P.dma(ctb[:], ctab, writes=["ctb"])
        cend = ctb[:, 0:4]
        kpos = ctb[:, 4:68]
        kpos512 = ctb[:, 68:132]

        with ExitStack() as cA:
            ksT = sbt(cA, "ksT", [128, 8192], BF16)
            kwT = sbt(cA, "kwT", [128, 8192], BF16)
            vs = sbt(cA, "vs", [128, 64, 2, 66], BF16)
            vw = sbt(cA, "vw", [128, 64, 2, 66], BF16)
            yl_own = sbt(cA, "yl_own", [128, 4, 2048], BF16)
            KC = sbt(cA, "KC", [128, 512], BF16)
            VCx = sbt(cA, "VCx", [128, 4, 2, 196], BF16)
            kmx = sbt(cA, "kmx", [128, 2])
            mset(kmx[:], 0.0, ["kmx"])
            mset(vs[:, :, :, 64:65], 1.0, ["vs"])
            mset(vw[:, :, :, 64:65], 1.0, ["vw"])
            mset(VCx[:, :, :, 64:65], 1.0, ["VCx"])
            for nt in range(4):
                si = stg_i[0] % 2
                stg_i[0] += 1
                P.dma(stage[si][:, 0:128], ovl[128 * nt:128 * nt + 128, :], writes=["stage%d" % si])
                for kv in range(2):
                    cp(VCx[:, nt, kv, 68:196], stage[si][:, 0:128], ["stage%d" % si], ["VCx"])

            with ExitStack() as c1x:
                wfs = sbt(c1x, "wfs", [128, 8, 1280], BF16)
                xt2 = [sbt(c1x, "xt0", [128, 1024]), sbt(c1x, "xt1", [128, 1024])]
                xTm2 = [sbt(c1x, "xTm0", [128, 8, 512], BF16), sbt(c1x, "xTm1", [128, 8, 512], BF16)]
                kcr2 = [sbt(c1x, "kcr0", [128, 528], BF16), sbt(c1x, "kcr1", [128, 528], BF16)]
                vcr2 = [sbt(c1x, "vcr0", [128, 528], BF16), sbt(c1x, "vcr1", [128, 528], BF16)]
                xl2 = [sbt(c1x, "xl0", [128, 4, 516]), sbt(c1x, "xl1", [128, 4, 516])]
                xc = sbt(c1x, "xc", [128, 4, 256])
                xcb = sbt(c1x, "xcb", [128, 4, 256], BF16)
                rr_ = sbt(c1x, "r", [128, 4, 256])
                sq = sbt(c1x, "sq", [128, 4, 256])
                ig = sbt(c1x, "ig", [128, 4, 256])
                hb = sbt(c1x, "hb", [128, 4, 256])
                ysel = sbt(c1x, "ysel", [128, 4, 128])
                state = sbt(c1x, "state", [128, 4])
                lp = sbt(c1x, "lp", [128, 36])
                cl = sbt(c1x, "cl", [128, 8])
                wa = sbt(c1x, "wa", [128, 4, 128], BF16)
                wi = sbt(c1x, "wi", [128, 4, 128], BF16)
                W1k = sbt(c1x, "W1k", [128, 32, 128], BF16)
                W1v = sbt(c1x, "W1v", [128, 32, 128], BF16)
                W2k = sbt(c1x, "W2k", [128, 128], BF16)
                W2v = sbt(c1x, "W2v", [128, 128], BF16)
                pk = sbt(c1x, "pk", [128, 32], BF16)
                pv_ = sbt(c1x, "pv", [128, 32], BF16)
                c1b = sbt(c1x, "c1b", [128, 2])
                Gk = sbt(c1x, "Gk", [128, 32], BF16)
                Gv = sbt(c1x, "Gv", [128, 128], BF16)
                sqk = sbt(c1x, "sqk", [128, 512], BF16)
                km1 = sbt(c1x, "km1", [128, 1])

                load_w(wfs, "wfs", wcat, 1280, 0, gain=gv[:, 0:8], engs=('dve',))
                P.dma(lp[:], lrup, writes=["lp"])
                load_small(wa[:].rearrange("p a b -> p (a b)"), "wa", wabd, 512)
                load_small(wi[:].rearrange("p a b -> p (a b)"), "wi", wibd, 512)
                load_small(W1k[:].rearrange("p a b -> p (a b)"), "W1k", w1k, 4096, eng='pool')
                load_small(W1v[:].rearrange("p a b -> p (a b)"), "W1v", w1v, 4096, eng='pool')
                load_small(W2k[:], "W2k", w2k, 128)
                load_small(W2v[:], "W2v", w2v, 128)
                load_small(pk[:], "pk", posk, 32)
                load_small(pv_[:], "pv", posv, 32)
                cw = lp[:, 0:16].rearrange("p (c k) -> p c k", c=4)
                cb = lp[:, 16:20]
                ba = lp[:, 20:24]
                bi = lp[:, 24:28]
                lam = lp[:, 28:32]
                selc = lp[:, 32:36]
                actf(cl[:, 0:4], lam, AF.Exp, ["lp"], ["cl"], scale=-1.0)
                actf(cl[:, 0:4], cl[:, 0:4], AF.Ln, ["cl"], ["cl"], bias=1.0)
                ts(cl[:, 4:8], cl[:, 0:4], -16.0, None, ALU.mult, None, ["cl"], ["cl2"])
                ts(cl[:, 0:4], cl[:, 0:4], -8.0, None, ALU.mult, None, ["cl", "cl2"], ["cl"])
                mset(state[:], 0.0, ["state"])
                mset(xl2[0][:, :, 0:3], 0.0, ["xl0"])
                mset(kcr2[0][:, 0:16], 0.0, ["kcr0"])
                mset(vcr2[0][:, 0:16], 0.0, ["vcr0"])
                for (W1, kW1, pp, kpp, col) in [(W1k, "W1k", pk, "pk", 0), (W1v, "W1v", pv_, "pv", 1)]:
                    for l in range(32):
                        mm(c0[:, col:col + 1], W1[:, l, :], pp[:, l:l + 1], l == 0, l == 31, [kW1, kpp], ["c0"])
                    cp(c1b[:, col:col + 1], c0[:, col:col + 1], ["c0"], ["c1b"])

                def stage_a(i):
                    xTm_, kx = xTm2[i % 2], "xTm%d" % (i % 2)
                    xl_, kxl = xl2[i % 2], "xl%d" % (i % 2)
                    kcr_, kkc = kcr2[i % 2], "kcr%d" % (i % 2)
                    vcr_, kvc = vcr2[i % 2], "vcr%d" % (i % 2)
                    for j in range(4):
                        blk = 4 * i + j
                        xt_, kxt = xt2[j % 2], "xt%d" % (j % 2)
                        P.dma(xt_[:], xfull[128 * blk:128 * blk + 128, :], writes=[kxt])
                        norm_T(xt_[:], kxt, xTm_[:, :, 128 * j:128 * j + 128], kx, par=j % 2)
                    for m in range(8):
                        pt_, kp = (A0, "A0") if m % 2 == 0 else (A1, "A1")
                        for c in range(8):
                            mm(pt_, wfs[:, c, 128 * m:128 * m + 128], xTm_[:, c, :], c == 0, c == 7, ["wfs", kx], [kp])
                        if m < 4:
                            cp(xl_[:, m, 3:515], pt_, [kp], [kxl], eng='act')
                        elif m == 4:
                            cp(kcr_[:, 16:528], pt_, [kp], [kkc])
                        elif m == 5:
                            cp(vcr_[:, 16:528], pt_, [kp], [kvc])
                        elif m == 6:
                            cp(ksT[:, 512 * i:512 * i + 512], pt_, [kp], ["ksT"])
                        else:
                            cp(kwT[:, 512 * i:512 * i + 512], pt_, [kp], ["kwT"])
                    for j in range(4):
                        blk = 4 * i + j
                        for c in range(8):
                            mm(B0[:, 0:256], xTm_[:, c, 128 * j:128 * j + 128], wfs[:, c, 1024:1280], c == 0, c == 7,
                               ["wfs", kx], ["B0"])
                        cp(vs[:, blk, :, 0:64], B0[:, 0:128].rearrange("p (a b) -> p a b", a=2), ["B0"], ["vs"])
                        cp(vw[:, blk, :, 0:64], B0[:, 128:256].rearrange("p (a b) -> p a b", a=2), ["B0"], ["vw"])

                def stage_b(i):
                    xl_, kxl = xl2[i % 2], "xl%d" % (i % 2)
                    xln, kxln = xl2[(i + 1) % 2], "xl%d" % ((i + 1) % 2)
                    for (kt, kk) in [(ksT, "ksT"), (kwT, "kwT")]:
                        actf(sqk[:], kt[:, 512 * i:512 * i + 512], AF.Square, [kk], ["sqk"])
                        mm(B1, ones_b[:], sqk[:], True, True, ["ones_b", "sqk"], ["B1"])
                        P.dve(lambda e: e.tensor_reduce(out=km1[:], in_=B1, axis=AX.X, op=ALU.max), ["B1"], ["km1"])
                        tt(kmx[:, 0:1], kmx[:, 0:1], km1[:], ALU.max, ["kmx", "km1"], ["kmx"])
                    cp(xln[:, :, 0:3], xl_[:, :, 512:515], [kxl], [kxln])
                    for hf in range(2):
                        o = 256 * hf
                        for ch in range(4):
                            ts(xc[:, ch, :], xl_[:, ch, o:o + 256], cw[:, ch, 0:1], cb[:, ch:ch + 1], ALU.mult, ALU.add,
                               [kxl, "lp"], ["xc%d" % ch])
                            for k in range(1, 4):
                                stt(xc[:, ch, :], xl_[:, ch, o + k:o + k + 256], cw[:, ch, k:k + 1], xc[:, ch, :],
                                    ALU.mult, ALU.add, [kxl, "lp", "xc%d" % ch], ["xc%d" % ch])
                            cp(xcb[:, ch, :], xc[:, ch, :], ["xc%d" % ch], ["xcb%d" % ch], eng='pool')
                        for ch in range(4):
                            pa_, kpa = (c0, "c0") if ch % 2 == 0 else (c1, "c1")
                            mm(pa_[:, 0:256], wa[:, ch, :], xcb[:, ch, :], True, False, ["wa", "xcb%d" % ch], [kpa])
                            mm(pa_[:, 256:512], wi[:, ch, :], xcb[:, ch, :], False, True, ["wi", "xcb%d" % ch], [kpa])
                            actf(rr_[:, ch, :], pa_[:, 0:256], AF.Sigmoid, [kpa, "lp"], ["r%d" % ch], bias=ba[:, ch:ch + 1])
                            actf(ig[:, ch, :], pa_[:, 256:512], AF.Sigmoid, [kpa, "lp"], ["ig%d" % ch], bias=bi[:, ch:ch + 1])
                        for ch in range(4):
                            actf(sq[:, ch, :], rr_[:, ch, :], AF.Exp, ["r%d" % ch, "cl2"], ["sq%d" % ch], scale=cl[:, 4 + ch:5 + ch])
                            actf(rr_[:, ch, :], rr_[:, ch, :], AF.Exp, ["r%d" % ch, "cl"], ["r%d" % ch], scale=cl[:, ch:ch + 1])
                        K4 = lambda nm: [nm + "%d" % c_ for c_ in range(4)]
                        actf(sq[:].rearrange("p a b -> p (a b)"), sq[:].rearrange("p a b -> p (a b)"), AF.Sqrt, K4("sq"), K4("sq"),
                             bias=1.0, scale=-1.0)
                        for ch in range(4):
                            tt(ig[:, ch, :], ig[:, ch, :], xc[:, ch, :], ALU.mult, ["ig%d" % ch, "xc%d" % ch], ["ig%d" % ch])
                            tt(ig[:, ch, :], ig[:, ch, :], sq[:, ch, :], ALU.mult, ["ig%d" % ch, "sq%d" % ch], ["ig%d" % ch])
                            P.dve((lambda ch: lambda e: e.tensor_tensor_scan(
                                out=hb[:, ch, :], data0=rr_[:, ch, :], data1=ig[:, ch, :], initial=state[:, ch:ch + 1],
                                op0=ALU.mult, op1=ALU.add))(ch), ["r%d" % ch, "ig%d" % ch, "state"], ["hb%d" % ch])
                        cp(state[:], hb[:, :, 255], K4("hb"), ["state"])
                        for r2 in range(2):
                            ri = 2 * hf + r2
                            src = hb[:, :, 128 * r2:128 * r2 + 128]
                            if ri == 0:
                                ts(ysel[:], src, selc[:, 0:1], None, ALU.mult, None, K4("hb") + ["lp"], ["ysel"])
                            else:
                                stt(ysel[:], src, selc[:, ri:ri + 1], ysel[:], ALU.mult, ALU.add, K4("hb") + ["lp", "ysel"], ["ysel"])
                        if hf == 1:
                            cp(yl_own[:, :, 128 * i:128 * i + 128], ysel[:], ["ysel"], ["yl_own"])
                    p0 = 32 * (i % 4)
                    nt = i // 4
                    for (raw2, kr, W1, kW1, col, G, kG) in [(kcr2, "kcr", W1k, "W1k", 0, Gk, "Gk"),
                                                           (vcr2, "vcr", W1v, "W1v", 1, Gv, "Gv")]:
                        raw, kraw = raw2[i % 2], kr + "%d" % (i % 2)
                        rawn, krawn = raw2[(i + 1) % 2], kr + "%d" % ((i + 1) % 2)
                        for l in range(32):
                            mm(c2[:, 0:32], W1[:, l, :], raw[:, l:l + 497:16], l == 0, l == 31, [kW1, kraw], ["c2"])
                        gdst = G[:] if col == 0 else G[:, p0:p0 + 32]
                        actf(gdst, c2[:, 0:32], AF.Gelu_apprx_tanh, ["c2", "c1b"], [kG], bias=c1b[:, col:col + 1])
                        cp(rawn[:, 0:16], raw[:, 512:528], [kraw], [krawn])
                    mm(c2[:, 64:96], W2k[:], Gk[:], True, True, ["W2k", "Gk"], ["c2"])
                    cp(KC[:, 32 * i:32 * i + 32], c2[:, 64:96], ["c2"], ["KC"])
                    if i % 4 == 3:
                        mm(c2[:, 128:256], Gv[:], W2v[:], True, True, ["W2v", "Gv"], ["c2"])
                        cp(VCx[:, nt, :, 0:64], c2[:, 128:256].rearrange("p (a b) -> p a b", a=2), ["c2"], ["VCx"])

                for i in range(n1 + 1):
                    if i < n1:
                        stage_a(i)
                    if i >= 1:
                        stage_b(i - 1)
                actf(sqk[:], KC[:], AF.Square, ["KC"], ["sqk"])
                mm(B1, ones_b[:], sqk[:], True, True, ["ones_b", "sqk"], ["B1"])
                P.dve(lambda e: e.tensor_reduce(out=km1[:], in_=B1, axis=AX.X, op=ALU.max), ["B1"], ["km1"])
                tt(kmx[:, 0:1], kmx[:, 0:1], km1[:], ALU.max, ["kmx", "km1"], ["kmx"])
                ts(kmx[:, 1:2], kmx[:, 0:1], -1.0 / 16, None, ALU.mult, None, ["kmx"], ["expb"])
                if dbg:
                    for nm, src_ap, kk, w_ in [("d_ks", ksT[:], "ksT", 8192), ("d_kc", KC[:], "KC", 512),
                                               ("d_yl", yl_own[:].rearrange("p a b -> p (a b)"), "yl_own", 8192),
                                               ("d_vc", VCx[:].rearrange("p a b c -> p (a b c)"), "VCx", 1568),
                                               ("d_vs", vs[:].rearrange("p a b c -> p (a b c)"), "vs", 8448)]:
                        for o in range(0, w_, 1024):
                            ww = min(1024, w_ - o)
                            cp(stage[0][:, 0:ww], src_ap[:, o:o + ww], [kk], ["stage0"])
                            P.dma(dbgo[nm][:, o:o + ww], stage[0][:, 0:ww], reads=["stage0"])
                P.emit()

            with ExitStack() as c2x:
                wown = sbt(c2x, "wown", [128, 8, 1048], BF16)
                wout = sbt(c2x, "wout", [128, 8, 1024], BF16)
                Ebig = sbt(c2x, "Ebig", [128, 8192], BF16)
                xo = sbt(c2x, "xo", [128, 1024])
                xoT = sbt(c2x, "xoT", [128, 8, 128], BF16)
                QT = sbt(c2x, "QT", [128, 512], BF16)
                Qsq = sbt(c2x, "Qsq", [128, 512], BF16)
                nbS = sbt(c2x, "nbS", [128, 2, 512], BF16)
                gl = sbt(c2x, "gl", [128, 4, 128])
                ylf = sbt(c2x, "ylf", [128, 4, 128])
                ylb = sbt(c2x, "ylb", [128, 4, 128], BF16)
                ylsq = sbt(c2x, "ylsq", [128, 4, 128], BF16)
                gates = sbt(c2x, "gates", [128, 24])
                bg = sbt(c2x, "bg", [128, 24])
                tq = sbt(c2x, "tq", [128, 512])
                fb = sbt(c2x, "fb", [128, 128])
                cmpb = sbt(c2x, "cmpb", [128, 4, 512], BF16)
                causb = sbt(c2x, "causb", [128, 4, 512], BF16)
                winb = sbt(c2x, "winb", [128, 8, 512], BF16)
                wtmp = sbt(c2x, "wtmp", [128, 2, 512])
                PT = [sbt(c2x, "PT0", [128, 512], BF16), sbt(c2x, "PT1", [128, 512], BF16), sbt(c2x, "PT2", [128, 512], BF16)]
                rz = sbt(c2x, "rz", [128, 4])
                coef = sbt(c2x, "coef", [128, 4])
                otmp = sbt(c2x, "otmp", [128, 4, 64])
                ynsa = sbt(c2x, "ynsa", [128, 512])
                ynb = sbt(c2x, "ynb", [128, 512], BF16)
                ynT = sbt(c2x, "ynT", [128, 4, 128], BF16)
                imp = sbt(c2x, "imp", [128, 128])
                imp2 = sbt(c2x, "imp2", [128, 128])
                m8 = sbt(c2x, "m8", [128, 16])
                selb = sbt(c2x, "selb", [128, 128], BF16)
                selT4 = sbt(c2x, "selT4", [128, 4, 128], BF16)
                x1 = sbt(c2x, "x1", [128, 1024])
                expb = kmx[:, 1:2]
                cvf = [sbt(c2x, "cvf0", [128, 512]), sbt(c2x, "cvf1", [128, 512])]
                cvo = [sbt(c2x, "cvo0", [128, 512], BF16), sbt(c2x, "cvo1", [128, 512], BF16)]
                conv_list = [(srct, co, r, h) for (srct, co) in [(peer_u, 0), (peer_v, 1024)] for r in range(128)
                             for h in range(2)]
                conv_pos = [0]

                def conv_some(n):
                    for _ in range(n):
                        if conv_pos[0] >= len(conv_list):
                            return
                        srct, co, r, h = conv_list[conv_pos[0]]
                        k = conv_pos[0] % 2
                        conv_pos[0] += 1
                        P.dma(cvf[k][:], srct[128 * r:128 * r + 128, 512 * h:512 * h + 512], writes=["cvf%d" % k])
                        cp(cvo[k][:], cvf[k][:], ["cvf%d" % k], ["cvo%d" % k], eng='pool')
                        P.dma(uv16[128 * r:128 * r + 128, co + 512 * h:co + 512 * h + 512], cvo[k][:], reads=["cvo%d" % k],
                              writes=["tb16"])

                if upto >= 2:
                    load_w(wown, "wown", wcat, 1048, 1280, gain=gv[:, 0:8])
                    load_w(wout, "wout", w_out, 1024, 0, gain=gv[:, 8:16])
                    P.dma(bg[:], bgate, writes=["bg"])
                    mset(Ebig[:], 1.0, ["Ebig"], eng='pool')
                    P.pool(lambda e: e.affine_select(out=Ebig[:], in_=Ebig[:], pattern=[[1, 8192]], compare_op=ALU.is_ge,
                                                     fill=0.0, base=0, channel_multiplier=-64), ["Ebig"], ["Ebig"])
                    P.pool(lambda e: e.affine_select(out=Ebig[:], in_=Ebig[:], pattern=[[-1, 8192]], compare_op=ALU.is_ge,
                                                     fill=0.0, base=63, channel_multiplier=64), ["Ebig"], ["Ebig"])
                tile_ctr = [0]

                SB = [(A0, "A0"), (A1, "A1"), (c0, "c0")]

                def score_tile(kT, kkT, kvs, kb, kv, extra):
                    n = tile_ctr[0]
                    tile_ctr[0] += 1
                    sp_, ksp = SB[n % 3]
                    pt_, kpt = PT[n % 3], "PT%d" % (n % 3)
                    mm(sp_, kT[kvs, 128 * kb:128 * kb + 128], QT[kvs, :], True, False, [kkT, "QT"], [ksp])
                    for k_, (l_, r_, rd_) in enumerate(extra):
                        mm(sp_, l_, r_, False, k_ == len(extra) - 1, rd_, [ksp])
                    actf(pt_[:], sp_, AF.Exp, [ksp, "expb"], [kpt], bias=expb)
                    return pt_, kpt

                def run_branch(tiles, acc, kacc, imp_rhs=None):
                    prev = None
                    nt_ = len(tiles)
                    for idx in range(nt_ + 1):
                        cur = None
                        if idx < nt_:
                            sa, vrhs, kvk, irhs = tiles[idx]
                            pt_, kpt = score_tile(*sa)
                            cur = (pt_, kpt, vrhs, kvk, irhs, idx)
                        if prev is not None:
                            pt_, kpt, vrhs, kvk, irhs, j = prev
                            for g in range(4):
                                mm(acc[:, 65 * g:65 * g + 65], pt_[:, 128 * g:128 * g + 128], vrhs, j == 0 and g == 0,
                                   j == nt_ - 1 and g == 3, [kpt, kvk], [kacc])
                            if irhs is not None:
                                for g in range(4):
                                    mm(B1[:, 128 * g:128 * g + 128], pt_[:, 128 * g:128 * g + 128], irhs,
                                       j == 0 and g == 0, j == nt_ - 1 and g == 3, [kpt, kvk], ["B1"])
                        prev = cur

                def combine(kv, br, first, acc, kacc):
                    o3 = acc[:, 0:260].rearrange("p (g d) -> p g d", g=4)
                    ts(rz[:], o3[:, :, 64], 1e-30, None, ALU.max, None, [kacc], ["rz"])
                    P.dve(lambda e: e.reciprocal(out=rz[:], in_=rz[:]), ["rz"], ["rz"])
                    tt(coef[:], rz[:], gates[:, 12 * kv + br:12 * kv + 12:3], ALU.mult, ["rz", "gates"], ["coef"])
                    y3 = ynsa[:, 256 * kv:256 * kv + 256].rearrange("p (g d) -> p g d", g=4)
                    cb_ = coef[:].unsqueeze(2).to_broadcast([128, 4, 64])
                    if first:
                        tt(y3, o3[:, :, 0:64], cb_, ALU.mult, [kacc, "coef"], ["ynsa"])
                    else:
                        tt(otmp[:], o3[:, :, 0:64], cb_, ALU.mult, [kacc, "coef"], ["otmp"])
                        tt(y3, y3, otmp[:], ALU.add, ["ynsa", "otmp"], ["ynsa"])

                for i in range(n2 if upto >= 2 else 0):
                    P.dma(xo[:], xown[128 * i:128 * i + 128, :], writes=["xo"])
                    P.dma(tq[:], tq4[i], writes=["tq"])
                    P.dma(fb[:], fbt[i], writes=["fb"])
                    norm_T(xo[:], "xo", xoT[:], "xoT")
                    conv_some((512 + n2 - 1) // n2)
                    for g in range(4):
                        for c in range(8):
                            mm(c0[:, 128 * g:128 * g + 128], wown[:, c, 512 + 128 * g:512 + 128 * g + 128], xoT[:, c, :],
                               g == 0 and c == 0, g == 3 and c == 7, ["wown", "xoT"], ["c0"])
                    actf(QT[:], c0, AF.Copy, ["c0"], ["QT"], scale=0.125)
                    actf(Qsq[:], c0, AF.Square, ["c0"], ["Qsq"])
                    mm(c1, cnegM[:, 0, :], Qsq[:], True, True, ["cnegM", "Qsq"], ["c1"])
                    mm(c2, cnegM[:, 1, :], Qsq[:], True, True, ["cnegM", "Qsq"], ["c2"])
                    cp(nbS[:, 0, :], c1, ["c1"], ["nbS"])
                    cp(nbS[:, 1, :], c2, ["c2"], ["nbS"])
                    for ch in range(4):
                        for c in range(8):
                            mm(c1[:, 128 * ch:128 * ch + 128], wown[:, c, 128 * ch:128 * ch + 128], xoT[:, c, :],
                               ch == 0 and c == 0, ch == 3 and c == 7, ["wown", "xoT"], ["c1"])
                    actf(gl[:].rearrange("p a b -> p (a b)"), c1, AF.Gelu_apprx_tanh, ["c1"], ["gl"])
                    tt(ylf[:], gl[:], yl_own[:, :, 128 * i:128 * i + 128], ALU.mult, ["gl", "yl_own"], ["ylf"])
                    cp(ylb[:].rearrange("p a b -> p (a b)"), ylf[:].rearrange("p a b -> p (a b)"), ["ylf"], ["ylb"], eng='pool')
                    actf(ylsq[:].rearrange("p a b -> p (a b)"), ylf[:].rearrange("p a b -> p (a b)"), AF.Square, ["ylf"], ["ylsq"])
                    for ch in range(4):
                        mm(c2[:, 0:1], ylsq[:, ch, :], ones_b[:, 0:1], ch == 0, ch == 3, ["ylsq", "ones_b"], ["c2"])
                    cp(ssq[:, 1:2], c2[:, 0:1], ["c2"], ["ssq1"])
                    calc_rstd(1, 512)
                    for c in range(8):
                        mm(c2[:, 32:56], xoT[:, c, :], wown[:, c, 1024:1048], c == 0, c == 7, ["wown", "xoT"], ["c2"])
                    tt(gates[:], c2[:, 32:56], bg[:], ALU.add, ["c2", "bg"], ["gates"])
                    actf(gates[:], gates[:], AF.Sigmoid, ["gates"], ["gates"])
                    NT = (32 * i + 31 + 127) // 128
                    for r in range(4):
                        kb = 4 * i + r
                        ts(causb[:, r, :], tq[:], kpos[:, kb:kb + 1], NEG, ALU.is_lt, ALU.mult, ["tq", "ctb"], ["causb"])
                    for kv in range(2):
                        kvs = slice(64 * kv, 64 * kv + 64)
                        for nt in range(NT):
                            ts(wtmp[:, 0, :], tq[:], cend[:, nt:nt + 1], None, ALU.is_lt, None, ["tq", "ctb"], ["wtmp0"])
                            stt(cmpb[:, nt, :], wtmp[:, 0, :], NEG, nbS[:, kv, :], ALU.mult, ALU.add, ["wtmp0", "nbS"], ["cmpb"])
                        for r8 in range(8):
                            kb = 4 * i - 4 + r8
                            if kb < 0:
                                continue
                            ts(wtmp[:, 0, :], tq[:], kpos[:, kb:kb + 1], None, ALU.is_lt, None, ["tq", "ctb"], ["wtmp0"])
                            stt(wtmp[:, 1, :], tq[:], kpos512[:, kb:kb + 1], wtmp[:, 0, :], ALU.is_ge, ALU.add, ["tq", "ctb", "wtmp0"], ["wtmp1"])
                            stt(winb[:, r8, :], wtmp[:, 1, :], NEG, nbS[:, kv, :], ALU.mult, ALU.add, ["wtmp1", "nbS"], ["winb"])
                        tiles = [((KC, "KC", kvs, nt, kv, [(identb[:], cmpb[:, nt, :], ["identb", "cmpb"])]),
                                  VCx[:, nt, kv, 0:65], "VCx", VCx[:, nt, kv, 68:196]) for nt in range(NT)]
                        run_branch(tiles, B0, "B0")
                        combine(kv, 0, True, B0, "B0")
                        ts(imp[:], B1[:, 0:128], rz[:, 0:1], None, ALU.mult, None, ["B1", "rz"], ["imp"])
                        for g in range(1, 4):
                            stt(imp[:], B1[:, 128 * g:128 * g + 128], rz[:, g:g + 1], imp[:], ALU.mult, ALU.add,
                                ["B1", "rz", "imp"], ["imp"])
                        tt(imp[:], imp[:], fb[:], ALU.add, ["imp", "fb"], ["imp"])
                        P.dve(lambda e: e.max(out=m8[:, 0:8], in_=imp[:]), ["imp"], ["m8"])
                        P.dve(lambda e: e.match_replace(out=imp2[:], in_to_replace=m8[:, 0:8], in_values=imp[:],
                                                        imm_value=-3e9), ["imp", "m8"], ["imp2"])
                        P.dve(lambda e: e.max(out=m8[:, 8:16], in_=imp2[:]), ["imp2"], ["m8"])
                        ts(imp2[:], imp[:], m8[:, 15:16], None, ALU.is_ge, None, ["imp", "m8"], ["imp2"])
                        stt(imp2[:], imp[:], -1.0, imp2[:], ALU.is_gt, ALU.mult, ["imp", "imp2"], ["imp2"])
                        ts(selb[:], imp2[:], 1.0, -NEG, ALU.subtract, ALU.mult, ["imp2"], ["selb"])
                        kbs = [4 * i - 4 + r8 for r8 in range(8) if 4 * i - 4 + r8 >= 0]
                        tiles = [((kwT, "kwT", kvs, kb, kv, [(identb[:], winb[:, kb - (4 * i - 4), :], ["identb", "winb"])]),
                                  vw[:, kb, kv, 0:65], "vw", None) for kb in kbs]
                        run_branch(tiles, c1, "c1")
                        combine(kv, 2, False, c1, "c1")
                        trp(ptb[:, 0:128], selb[:], identb[:], ["selb", "identb"], ["ptb"])
                        tt(selT4[:], ptb[:, 0:128].unsqueeze(1).to_broadcast([128, 4, 128]),
                           nbS[:, kv, :].rearrange("p (g q) -> p g q", g=4), ALU.add, ["ptb", "nbS"], ["selT4"])
                        sel_rhs = selT4[:].rearrange("p a b -> p (a b)")
                        nkb = 4 * i + 4
                        tiles = []
                        for kb in range(nkb):
                            extra = [(Ebig[:, 128 * kb:128 * kb + 128], sel_rhs, ["Ebig", "selT4"])]
                            if kb >= 4 * i:
                                extra.append((identb[:], causb[:, kb - 4 * i, :], ["identb", "causb"]))
                            tiles.append(((ksT, "ksT", kvs, kb, kv, extra), vs[:, kb, kv, 0:65], "vs", None))
                        run_branch(tiles, c2, "c2")
                        combine(kv, 1, False, c2, "c2")
                    if dbg:
                        P.dma(dbgo["d_yn"][128 * i:128 * i + 128, :], ynsa[:], reads=["ynsa"])
                    actf(ynb[:], ynsa[:], AF.Square, ["ynsa"], ["ynb", "ssq2"], accum=ssq[:, 2:3])
                    calc_rstd(2, 512)
                    cp(ynb[:], ynsa[:], ["ynsa"], ["ynb"])
                    for c in range(4):
                        trp(ptb[:, 128 * c:128 * c + 128], ynb[:, 128 * c:128 * c + 128], identb[:], ["ynb", "identb"], ["ptb"])
                    cp(ynT[:], ptb[:, 0:512].rearrange("p (c t) -> p c t", c=4), ["ptb"], ["ynT"])
                    for hh, (pl, kpl, pn, kpn) in enumerate([(B0, "B0", c0, "c0"), (B1, "B1", c1, "c1")]):
                        cs = slice(512 * hh, 512 * hh + 512)
                        for ch in range(4):
                            mm(pl, ylb[:, ch, :], wout[:, ch, cs], ch == 0, ch == 3, ["ylb", "wout"], [kpl])
                        for ch in range(4):
                            mm(pn, ynT[:, ch, :], wout[:, 4 + ch, cs], ch == 0, ch == 3, ["ynT", "wout"], [kpn])
                        stt(x1[:, cs], pl, rstd[:, 1:2], xo[:, cs], ALU.mult, ALU.add, [kpl, "rstd1", "xo"], ["x1"])
                        stt(x1[:, cs], pn, rstd[:, 2:3], x1[:, cs], ALU.mult, ALU.add, [kpn, "rstd2", "x1"], ["x1"])
                    P.dma(x1d[128 * i:128 * i + 128, :], x1[:], reads=["x1"], writes=["x1d"])
                conv_some(512)
                P.emit()

        with ExitStack() as c3x:
            wmq = sbt(c3x, "wmq", [128, 8, 1024], BF16)
            wmo = sbt(c3x, "wmo", [128, 8, 1024], BF16)
            wpq = sbt(c3x, "wpq", [128, 8, 2048], BF16)
            KmT = sbt(c3x, "KmT", [128, 8, 256], BF16)
            Vm = sbt(c3x, "Vm", [128, 2, 1024], BF16)
            sk = sbt(c3x, "sk", [128, 16, 128], BF16)
            gff = sbt(c3x, "gff", [128, 1024])
            gfi = sbt(c3x, "gfi", [128, 1024])
            memT = sbt(c3x, "memT", [128, 8, 256], BF16)
            kmm = sbt(c3x, "kmm", [128, 4])
            xa = sbt(c3x, "xa", [128, 1024])
            xb2 = sbt(c3x, "xb2", [128, 1024])
            xnf = sbt(c3x, "xnf", [128, 1024])
            xT = sbt(c3x, "xT", [128, 8, 128], BF16)
            qmT = sbt(c3x, "qmT", [128, 8, 128], BF16)
            qmsq = sbt(c3x, "qmsq", [128, 8, 128], BF16)
            negbm = sbt(c3x, "negbm", [1, 512], BF16)
            PTm = [sbt(c3x, "PTm0", [128, 512], BF16), sbt(c3x, "PTm1", [128, 512], BF16)]
            rzb = sbt(c3x, "rzb", [128, 512])
            oT = sbt(c3x, "oT", [128, 8, 128], BF16)
            qpT = sbt(c3x, "qpT", [128, 16, 128], BF16)
            s_sb = sbt(c3x, "s_sb", [128, 16, 128])
            s2 = sbt(c3x, "s2", [128, 128])
            v16 = sbt(c3x, "v16", [128, 16, 16])
            i16 = sbt(c3x, "i16", [128, 16, 16], U32)
            i16f = sbt(c3x, "i16f", [128, 16, 16])
            cand = sbt(c3x, "cand", [128, 8, 256])
            cand2 = sbt(c3x, "cand2", [128, 256])
            cid = sbt(c3x, "cid", [128, 8, 256])
            top = sbt(c3x, "top", [128, 8, 16])
            eid = sbt(c3x, "eid", [128, 128])
            pos = sbt(c3x, "pos", [128, 8, 16], U32)
            posf = sbt(c3x, "posf", [128, 128])
            ai = sbt(c3x, "ai", [128, 128], I32)
            af = sbt(c3x, "af", [128, 128])
            bfl = sbt(c3x, "bfl", [128, 128])
            e1 = sbt(c3x, "e1", [128, 128])
            e2 = sbt(c3x, "e2", [128, 128])
            gw = sbt(c3x, "gw", [128, 8, 16])
            gsum = sbt(c3x, "gsum", [128, 8])
            eidT = sbt(c3x, "eidT", [128, 128], I32)
            gT = sbt(c3x, "gT", [128, 128])
            eidT2 = sbt(c3x, "eidT2", [128, 128], I32)
            gT2 = sbt(c3x, "gT2", [128, 128])
            NB = 8
            UVg = [sbt(c3x, "UVg%d" % k, [128, 2048], BF16) for k in range(NB)]
            gh = sbt(c3x, "gh", [128, 128])
            hTa = sbt(c3x, "hTa", [128, 128])
            hTb = sbt(c3x, "hTb", [128, 128])
            jk = [sbt(c3x, "jk0", [128, 512], BF16), sbt(c3x, "jk1", [128, 512], BF16)]
            selt = [sbt(c3x, "selt0", [128, 128], BF16), sbt(c3x, "selt1", [128, 128], BF16)]
            Dt = [sbt(c3x, "Dt0", [128, 128], BF16), sbt(c3x, "Dt1", [128, 128], BF16)]
            yo = xnf

            if upto >= 3:
                load_w(wmq, "wmq", w_mq, 1024, 0, gain=gv[:, 16:24])
                load_w(wpq, "wpq", w_pq, 2048, 0)
                load_small(sk[:].rearrange("p a b -> p (a b)"), "sk", skT, 2048)
                P.dma(gff[:], gffn.partition_broadcast(128), writes=["gain"])
                P.dma(gfi[:], gfin.partition_broadcast(128), writes=["gfi"])
                for mb in range(2):
                    P.dma(xa[:], memb[128 * mb:128 * mb + 128, :], writes=["xa"])
                    norm_T(xa[:], "xa", memT[:, :, 128 * mb:128 * mb + 128], "memT")
                P.emit()
                load_w(wmo, "wmo", w_mk, 1024, 0, gain=gv[:, 24:32])
                for oc in range(8):
                    for c in range(8):
                        mm(c0[:, 0:256], wmo[:, c, 128 * oc:128 * oc + 128], memT[:, c, :], c == 0, c == 7, ["wmo", "memT"], ["c0"])
                    cp(KmT[:, oc, :], c0[:, 0:256], ["c0"], ["KmT"])
                sqm = sbt(c3x, "sqm", [128, 8, 256], BF16)
                actf(sqm[:].rearrange("p a b -> p (a b)"), KmT[:].rearrange("p a b -> p (a b)"), AF.Square, ["KmT"], ["sqm"])
                for hm in range(4):
                    for dc in range(2):
                        mm(c1[:, 0:256], ones_b[:], sqm[:, 2 * hm + dc, :], dc == 0, dc == 1, ["ones_b", "sqm"], ["c1"])
                    P.dve((lambda hm: lambda e: e.tensor_reduce(out=kmm[:, hm:hm + 1], in_=c1[:, 0:256], axis=AX.X, op=ALU.max))(hm),
                          ["c1"], ["kmm"])
                P.dve(lambda e: e.tensor_reduce(out=kmm[:, 0:1], in_=kmm[:, 0:4], axis=AX.X, op=ALU.max), ["kmm"], ["kmm"])
                ts(kmm[:, 1:2], kmm[:, 0:1], -1.0 / 32, None, ALU.mult, None, ["kmm"], ["expbm"])
                expbm = kmm[:, 1:2]
                P.emit()
                load_w(wmo, "wmo", w_mv, 1024, 0, gain=gv[:, 24:32])
                for mb in range(2):
                    for hh in range(2):
                        for c in range(8):
                            mm(c0, memT[:, c, 128 * mb:128 * mb + 128], wmo[:, c, 512 * hh:512 * hh + 512], c == 0, c == 7,
                               ["wmo", "memT"], ["c0"])
                        cp(Vm[:, mb, 512 * hh:512 * hh + 512], c0, ["c0"], ["Vm"])
                P.emit()
                load_w(wmo, "wmo", w_mo, 1024, 0)

            sqmf = sqm[:].rearrange("p a b -> p (a b)") if upto >= 3 else None
            n3e = n3 if upto >= 3 else 0
            AB2 = [(A0, "A0"), (A1, "A1")]

            def bufs(i):
                p2 = i % 2
                xb2_, kxb2 = (xb2[:], "xb2") if p2 == 0 else (stage[1][:], "stage1")
                xnb_, kxnb = (xnb[:], "xnbP0") if p2 == 0 else (sqmf[:, 0:1024], "xnbP1")
                eidT_, keid = (eidT, "eidT0") if p2 == 0 else (eidT2, "eidT1")
                gT_, kgT = (gT, "gT0") if p2 == 0 else (gT2, "gT1")
                return xb2_, kxb2, xnb_, kxnb, eidT_, keid, gT_, kgT

            def stage_S(i):
                xb2_, kxb2, xnb_, kxnb, eidT_, keid, gT_, kgT = bufs(i)
                P.dma(xa[:], x1d[128 * i:128 * i + 128, :], reads=["x1d"], writes=["xa"])
                norm_T(xa[:], "xa", xT[:], "xT", xbo=(sqmf[:, 1024:2048], "xnbS"))
                for oc in range(8):
                    pt_, kp = AB2[oc // 4]
                    for c in range(8):
                        mm(pt_[:, 128 * (oc % 4):128 * (oc % 4) + 128], wmq[:, c, 128 * oc:128 * oc + 128], xT[:, c, :],
                           oc % 4 == 0 and c == 0, oc % 4 == 3 and c == 7, ["wmq", "xT"], [kp])
                for hh, (pt_, kp) in enumerate(AB2):
                    actf(qmT[:, 4 * hh:4 * hh + 4, :].rearrange("p a b -> p (a b)"), pt_, AF.Copy, [kp], ["qmT"], scale=1.0 / 16)
                    actf(qmsq[:, 4 * hh:4 * hh + 4, :].rearrange("p a b -> p (a b)"), pt_, AF.Square, [kp], ["qmsq"])
                for hm in range(4):
                    for dc in range(2):
                        mm(c2[0:1, 128 * hm:128 * hm + 128], cneg[:, 2:3], qmsq[:, 2 * hm + dc, :], hm == 0 and dc == 0,
                           hm == 3 and dc == 1, ["cneg", "qmsq"], ["c2"])
                cp(negbm[0:1, :], c2[0:1, :], ["c2"], ["negbm"])
                for mc in range(2):
                    sp_, ksp = AB2[mc]
                    for hm in range(4):
                        for dc in range(2):
                            mm(sp_[:, 128 * hm:128 * hm + 128], KmT[:, 2 * hm + dc, 128 * mc:128 * mc + 128],
                               qmT[:, 2 * hm + dc, :], hm == 0 and dc == 0, False, ["KmT", "qmT"], [ksp])
                    mm(sp_, ones_b[0:1, :], negbm[0:1, :], False, True, ["ones_b", "negbm"], [ksp])
                    actf(PTm[mc][:], sp_, AF.Exp, [ksp, "expbm"], ["PTm%d" % mc], bias=expbm)
                for oc in range(8):
                    hm = oc // 2
                    pt_, kp = AB2[oc // 4]
                    for mc in range(2):
                        mm(pt_[:, 128 * (oc % 4):128 * (oc % 4) + 128], Vm[:, mc, 128 * oc:128 * oc + 128],
                           PTm[mc][:, 128 * hm:128 * hm + 128], oc % 4 == 0 and mc == 0, oc % 4 == 3 and mc == 1,
                           ["Vm", "PTm%d" % mc], [kp])
                for hm in range(4):
                    for mc in range(2):
                        mm(c2[:, 128 * hm:128 * hm + 128], ones_b[:], PTm[mc][:, 128 * hm:128 * hm + 128],
                           hm == 0 and mc == 0, hm == 3 and mc == 1, ["ones_b", "PTm%d" % mc], ["c2"])
                P.dve(lambda e: e.reciprocal(out=rzb[:], in_=c2), ["c2"], ["rzb"])
                for oc in range(8):
                    hm = oc // 2
                    pt_, kp = AB2[oc // 4]
                    tt(oT[:, oc, :], pt_[:, 128 * (oc % 4):128 * (oc % 4) + 128], rzb[:, 128 * hm:128 * hm + 128], ALU.mult,
                       [kp, "rzb"], ["oT"])
                for hh, (pt_, kp) in enumerate(AB2):
                    cs = slice(512 * hh, 512 * hh + 512)
                    for oc in range(8):
                        mm(pt_, oT[:, oc, :], wmo[:, oc, cs], oc == 0, oc == 7, ["oT", "wmo"], [kp])
                    tt(xb2_[:, cs], pt_, xa[:, cs], ALU.add, [kp, "xa"], [kxb2])
                if dbg:
                    P.dma(dbgo["d_x2"][128 * i:128 * i + 128, :], xb2_, reads=[kxb2])
                norm_T(xb2_, kxb2, xT[:], "xT", gain=gff[:], xnf=xnf[:], xbo=(xnb_, kxnb))
                for half in range(2):
                    for h8 in range(8):
                        hc = 8 * half + h8
                        pt_, kp = AB2[h8 // 4]
                        for c in range(8):
                            mm(pt_[:, 128 * (hc % 4):128 * (hc % 4) + 128], wpq[:, c, 128 * hc:128 * hc + 128], xT[:, c, :],
                               hc % 4 == 0 and c == 0, hc % 4 == 3 and c == 7, ["wpq", "xT"], [kp])
                    for q2, (pt_, kp) in enumerate(AB2):
                        q4 = 2 * half + q2
                        cp(qpT[:, 4 * q4:4 * q4 + 4, :].rearrange("p a b -> p (a b)"), pt_, [kp], ["qpT"],
                           eng='act' if q4 % 2 else 'dve')
                for half in range(2):
                    for h8 in range(8):
                        hc = 8 * half + h8
                        pt_, kp = AB2[h8 // 4]
                        mm(pt_[:, 128 * (hc % 4):128 * (hc % 4) + 128], qpT[:, hc, :], sk[:, hc, :], hc % 4 == 0, hc % 4 == 3,
                           ["qpT", "sk"], [kp])
                    for q2, (pt_, kp) in enumerate(AB2):
                        q4 = 2 * half + q2
                        cp(s_sb[:, 4 * q4:4 * q4 + 4, :].rearrange("p a b -> p (a b)"), pt_, [kp], ["s_sb"],
                           eng='act' if q4 % 2 else 'dve')
                for hc in range(16):
                    sv = s_sb[:, hc, :]
                    P.dve((lambda hc, sv: lambda e: e.max(out=v16[:, hc, 0:8], in_=sv))(hc, sv), ["s_sb"], ["v16"])
                    P.dve((lambda hc, sv: lambda e: e.max_index(out=i16[:, hc, 0:8], in_max=v16[:, hc, 0:8], in_values=sv))(hc, sv),
                          ["s_sb", "v16"], ["i16"])
                    P.dve((lambda hc, sv: lambda e: e.match_replace(out=s2[:], in_to_replace=v16[:, hc, 0:8], in_values=sv,
                                                                     imm_value=-1e30))(hc, sv), ["s_sb", "v16"], ["s2"])
                    P.dve((lambda hc: lambda e: e.max(out=v16[:, hc, 8:16], in_=s2[:]))(hc), ["s2"], ["v16"])
                    P.dve((lambda hc: lambda e: e.max_index(out=i16[:, hc, 8:16], in_max=v16[:, hc, 8:16], in_values=s2[:]))(hc),
                          ["s2", "v16"], ["i16"])
                cp(i16f[:], i16[:], ["i16"], ["i16f"])
                for h in range(8):
                    tt(cand[:, h, :].rearrange("p (a b) -> p a b", a=16),
                       v16[:, 2 * h, :].unsqueeze(2).to_broadcast([128, 16, 16]),
                       v16[:, 2 * h + 1, :].unsqueeze(1).to_broadcast([128, 16, 16]), ALU.add, ["v16"], ["cand"])
                for h in range(8):
                    cv = cand[:, h, :]
                    P.dve((lambda h, cv: lambda e: e.max(out=top[:, h, 0:8], in_=cv))(h, cv), ["cand"], ["top"])
                    P.dve((lambda h, cv: lambda e: e.max_index(out=pos[:, h, 0:8], in_max=top[:, h, 0:8], in_values=cv))(h, cv),
                          ["cand", "top"], ["pos"])
                    P.dve((lambda h, cv: lambda e: e.match_replace(out=cand2[:], in_to_replace=top[:, h, 0:8], in_values=cv,
                                                                    imm_value=-1e30))(h, cv), ["cand", "top"], ["cand2"])
                    P.dve((lambda h: lambda e: e.max(out=top[:, h, 8:16], in_=cand2[:]))(h), ["cand2"], ["top"])
                    P.dve((lambda h: lambda e: e.max_index(out=pos[:, h, 8:16], in_max=top[:, h, 8:16], in_values=cand2[:]))(h),
                          ["cand2", "top"], ["pos"])
                cp(posf[:], pos[:].rearrange("p a b -> p (a b)"), ["pos"], ["posf"])
                ts(ai[:], posf[:], -7.5, 0.0625, ALU.add, ALU.mult, ["posf"], ["ai"])
                cp(af[:], ai[:], ["ai"], ["af"])
                stt(bfl[:], af[:], -16.0, posf[:], ALU.mult, ALU.add, ["af", "posf"], ["bfl"])
                eq3 = cid[:].rearrange("p h (k a) -> p (h k) a", a=16)
                eq4 = cid[:].rearrange("p h (k a) -> p h k a", a=16)
                iob = ctb[:, 132:148].unsqueeze(1).to_broadcast([128, 128, 16])
                for (src_, par, dst_) in [(af, 0, e1), (bfl, 1, e2)]:
                    tt(eq3, src_[:].unsqueeze(2).to_broadcast([128, 128, 16]), iob, ALU.is_equal, [("af" if par == 0 else "bfl"), "ctb"], ["cid"])
                    tt(eq4, eq4, i16f[:, par:16:2, :].unsqueeze(2).to_broadcast([128, 8, 16, 16]), ALU.mult, ["cid", "i16f"], ["cid"])
                    P.dve((lambda dst_: lambda e: e.tensor_reduce(out=dst_[:], in_=eq3, axis=AX.X, op=ALU.add))(dst_), ["cid"],
                          ["e%d" % par])
                stt(eid[:], e1[:], 128.0, e2[:], ALU.mult, ALU.add, ["e0", "e1"], ["eid"])
                tt(gw[:], top[:], top[:, :, 0:1].to_broadcast([128, 8, 16]), ALU.subtract, ["top"], ["gw"])
                actf(gw[:].rearrange("p a b -> p (a b)"), gw[:].rearrange("p a b -> p (a b)"), AF.Exp, ["gw"], ["gw"])
                P.dve(lambda e: e.tensor_reduce(out=gsum[:], in_=gw[:], axis=AX.X, op=ALU.add), ["gw"], ["gsum"])
                P.dve(lambda e: e.reciprocal(out=gsum[:], in_=gsum[:]), ["gsum"], ["gsum"])
                tt(gw[:], gw[:], gsum[:].unsqueeze(2).to_broadcast([128, 8, 16]), ALU.mult, ["gw", "gsum"], ["gw"])
                ts(eid[:], eid[:], 16383.0, 0.0, ALU.min, ALU.max, ["eid"], ["eid"])
                trp(A0[:, 0:128], eid[:], identf[:], ["eid", "identf"], ["A0"])
                cp(eidT_[:], A0[:, 0:128], ["A0"], [keid])
                trp(A1[:, 0:128], gw[:].rearrange("p a b -> p (a b)"), identf[:], ["gw", "identf"], ["A1"])
                cp(gT_[:], A1[:, 0:128], ["A1"], [kgT])

            LAG = 3

            pref = [0]
            NPRE = 6

            def issue_gather(eT, kE, t):
                u = t % NB
                P.op('pool', (lambda t, u: lambda e: e.indirect_dma_start(
                    out=UVg[u][:], out_offset=None, in_=uv16,
                    in_offset=bass.IndirectOffsetOnAxis(ap=eT[:, t:t + 1], axis=0)))(t, u),
                    [kE], ["UVg%d" % u], dma=True)

            def token_loop(i, q):
                xb2_, kxb2, xnb_, kxnb, eidT_, keid, gT_, kgT = bufs(i)
                npre = pref[0]
                (pa, ka), (pb, kb_) = (c0, "c0"), (c1, "c1")
                nsteps = 128 + LAG
                caps = {'pe': 10, 'dve': 4, 'act': 3, 'pool': 2, 'sp': 2}
                wstep, weng, rstep, load = {}, {}, {}, {}
                lastst = {e_: 0 for e_ in ENGS}
                buckets = [[] for _ in range(nsteps)]
                tail = []
                for item in q:
                    eng_, _fn, reads_, writes_, _dma = item
                    s_ = lastst[eng_]
                    for k_ in tuple(reads_) + tuple(writes_):
                        if k_ in wstep:
                            s_ = max(s_, wstep[k_] + (1 if weng[k_] != eng_ else 0))
                    for k_ in writes_:
                        for e2_, st_ in rstep.get(k_, {}).items():
                            s_ = max(s_, st_ + (1 if e2_ != eng_ else 0))
                    while load.get((s_, eng_), 0) >= caps[eng_]:
                        s_ += 1
                    load[(s_, eng_)] = load.get((s_, eng_), 0) + 1
                    lastst[eng_] = s_
                    for k_ in writes_:
                        wstep[k_] = s_
                        weng[k_] = eng_ if not _dma else 'dma'
                        rstep[k_] = {}
                    for k_ in reads_:
                        if k_ not in writes_:
                            d_ = rstep.setdefault(k_, {})
                            d_[eng_] = max(d_.get(eng_, 0), s_)
                    (buckets[s_] if s_ < nsteps else tail).append(item)

                def u_side(t):
                    u = t % NB
                    if t >= npre:
                        issue_gather(eidT_, keid, t)
                    s2_ = t % 2
                    actf(selt[s2_][:], ones_b[:], AF.Copy, ["ones_b", "identf"], ["selt%d" % s2_], scale=identf[:, t:t + 1])
                    mm(pa, selt[s2_][:], xnb_[:, 0:512], True, True, ["selt%d" % s2_, kxnb], [ka])
                    mm(pb, selt[s2_][:], xnb_[:, 512:1024], True, True, ["selt%d" % s2_, kxnb], [kb_])
                    stt(jk[0][:], UVg[u][:, 0:512], 1.0, pa, ALU.mult, ALU.mult, ["UVg%d" % u, ka], ["ha%d" % t, "jk0"],
                        accum=hTa[:, t:t + 1])
                    stt(jk[1][:], UVg[u][:, 512:1024], 1.0, pb, ALU.mult, ALU.mult, ["UVg%d" % u, kb_], ["hb%d" % t, "jk1"],
                        accum=hTb[:, t:t + 1])

                def v_pre(t):
                    actf(gh[:, t:t + 1], hTa[:, t:t + 1], AF.Gelu_apprx_tanh, ["ha%d" % t, "hb%d" % t], ["gh%d" % t],
                         bias=hTb[:, t:t + 1])
                    d2 = t % 2
                    actf(gh[:, t:t + 1], gh[:, t:t + 1], AF.Copy, ["gh%d" % t, kgT], ["gh%d" % t], scale=gT_[:, t:t + 1])
                    actf(Dt[d2][:], Rsel[:, 127 - t:255 - t], AF.Copy, ["Rsel", "gh%d" % t], ["Dt%d" % d2],
                         scale=gh[:, t:t + 1])

                def v_mm(t):
                    u = t % NB
                    d2 = t % 2
                    mm(B0, Dt[d2][:], UVg[u][:, 1024:1536], t == 0, t == 127, ["Dt%d" % d2, "UVg%d" % u], ["B0"])
                    mm(B1, Dt[d2][:], UVg[u][:, 1536:2048], t == 0, t == 127, ["Dt%d" % d2, "UVg%d" % u], ["B1"])

                for step in range(128 + LAG):
                    P.drain(buckets[step], len(buckets[step]))
                    if step >= LAG:
                        v_pre(step - LAG)
                    if step < 128:
                        u_side(step)
                    if step >= LAG:
                        v_mm(step - LAG)
                P.drain(tail, len(tail))
                pref[0] = 0
                if i + 1 < n3e:
                    nb_ = bufs(i + 1)
                    for t_ in range(NPRE):
                        issue_gather(nb_[4], nb_[5], t_)
                    pref[0] = NPRE
                tt(xa[:], B[:, :], xb2_, ALU.add, ["B0", "B1", kxb2], ["xa"])
                actf(cid[:].rearrange("p a b -> p (a b)")[:, 0:1024], xa[:], AF.Square, ["xa"], ["cid", "ssq3"], accum=ssq[:, 3:4])
                calc_rstd(3, 1024)
                stt(yo[:], xa[:], rstd[:, 3:4], gfi[:], ALU.mult, ALU.mult, ["xa", "rstd3", "gfi"], ["xnf"])
                P.dma(yout[128 * i:128 * i + 128, :], yo[:], reads=["xnf"])

            if n3e > 0:
                stage_S(0)
            for i in range(n3e):
                q = []
                if i + 1 < n3e:
                    P.capture_begin()
                    stage_S(i + 1)
                    q = P.capture_end()
                token_loop(i, q)
            P.emit()
    return nc


_NC_CACHE = {}


def _prep_shared(inp):
    f = np.float32
    w_in = np.asarray(inp['w_in'][0], f)
    offs = np.cumsum([0, 512, 512, 512, 128, 128, 128, 128, 128, 128, 24])
    x_lru, gate, q, k_c, v_c, k_s, v_s, k_w, v_w, g_raw = [w_in[:, offs[j]:offs[j + 1]] for j in range(10)]
    qp = q.reshape(1024, 2, 4, 64).transpose(0, 2, 1, 3).reshape(1024, 512)
    wcat = np.ascontiguousarray(np.concatenate([x_lru, k_c, v_c, k_s, k_w, v_s, v_w, gate, qp, g_raw], axis=1))

    def pc(v):
        return np.asarray(v, f).reshape(8, 128).T

    gout = np.concatenate([np.asarray(inp['g_out_lru'][0], f), np.asarray(inp['g_out_nsa'][0], f)])
    gvec = np.zeros((128, 48), f)
    gvec[:, 0:8] = pc(inp['g_mix'][0])
    gvec[:, 8:16] = pc(gout)
    gvec[:, 16:24] = pc(inp['g_mem_q'][0])
    gvec[:, 24:32] = pc(inp['g_mem_kv'][0])
    bgate = np.tile(np.asarray(inp['b_gate'][0], f)[None, :], (128, 1))
    lrup = np.zeros((128, 36), f)
    cwv = np.asarray(inp['conv_w'][0], f)
    lrup[:, 0:16] = cwv.reshape(4, 4, 128).transpose(2, 1, 0).reshape(128, 16)
    lrup[:, 16:20] = np.asarray(inp['conv_b'][0], f).reshape(4, 128).T
    lrup[:, 20:24] = np.asarray(inp['b_rg_a'][0], f).reshape(4, 128).T
    lrup[:, 24:28] = np.asarray(inp['b_rg_i'][0], f).reshape(4, 128).T
    lrup[:, 28:32] = np.asarray(inp['lam'][0], f).reshape(4, 128).T

    def bd(w):
        o = np.zeros((128, 4, 128), f)
        for ch in range(4):
            o[0:64, ch, 0:64] = w[2 * ch]
            o[64:128, ch, 64:128] = w[2 * ch + 1]
        return o.reshape(128, 512)

    wabd = bd(np.asarray(inp['w_rg_a'][0], f))
    wibd = bd(np.asarray(inp['w_rg_i'][0], f))

    def w1bd(w1):
        w = np.asarray(w1, f).reshape(32, 64, 64)
        o = np.zeros((128, 32, 128), f)
        o[0:64, :, 0:64] = w.transpose(1, 0, 2)
        o[64:128, :, 64:128] = w.transpose(1, 0, 2)
        return o.reshape(128, 4096)

    def w2bd(w2):
        o = np.zeros((128, 128), f)
        o[0:64, 0:64] = w2
        o[64:128, 64:128] = w2
        return o

    def posT(p):
        return np.ascontiguousarray(np.tile(np.asarray(p, f).T, (2, 1)))

    npr = np.arange(512)
    ci = (npr - 1) * 16
    bj = np.arange(128) * 64
    ovl = ((ci[:, None] < bj[None, :] + 64) & (ci[:, None] + 32 > bj[None, :])).astype(f)
    ovl[0, :] = 0.0
    ctab = np.zeros((128, 148), f)
    ctab[:, 132:148] = np.arange(16, dtype=f)[None, :]
    p = np.arange(128)
    for nt in range(4):
        ctab[:, nt] = 16.0 * (128 * nt + p) + 15.0
    ctab[0, 0] = 1e9
    for kb in range(64):
        ctab[:, 4 + kb] = 128.0 * kb + p
        ctab[:, 68 + kb] = 128.0 * kb + p + 512.0
    skT = np.ascontiguousarray(np.asarray(inp['sub_keys'][0], f).reshape(16, 128, 128).transpose(2, 0, 1).reshape(128, 2048))
    sh = dict(
        wcat=wcat, gvec=gvec, bgate=bgate, wabd=wabd, wibd=wibd,
        w1k=w1bd(inp['cmp_w1_k'][0]), w1v=w1bd(inp['cmp_w1_v'][0]),
        w2k=w2bd(np.asarray(inp['cmp_w2_k'][0], f)), w2v=w2bd(np.asarray(inp['cmp_w2_v'][0], f)),
        posk=posT(inp['cmp_pos_k'][0]), posv=posT(inp['cmp_pos_v'][0]), ovl=ovl, ctab=ctab,
        w_out=np.ascontiguousarray(np.asarray(inp['w_out'][0], f)),
        w_mq=np.ascontiguousarray(np.asarray(inp['w_mq'][0], f)),
        w_mk=np.ascontiguousarray(np.asarray(inp['w_mk'][0], f)),
        w_mv=np.ascontiguousarray(np.asarray(inp['w_mv'][0], f)),
        w_mo=np.ascontiguousarray(np.asarray(inp['w_mo'][0], f)),
        w_pq=np.ascontiguousarray(np.asarray(inp['w_pq'][0], f)),
        skT=skT,
        gffn=np.asarray(inp['g_ffn'][0], f).reshape(1, 1024),
        gfin=np.asarray(inp['g_final'], f).reshape(1, 1024),
        peer_u=np.ascontiguousarray(np.asarray(inp['peer_u'][0], f)),
        peer_v=np.ascontiguousarray(np.asarray(inp['peer_v'][0], f)),
    )
    return sh, lrup


def _own_idx(c):
    return np.concatenate([128 * (4 * i + c) + np.arange(128) for i in range(16)])


def _prep_core(inp, sh, lrup, core):
    f = np.float32
    b, c = core // 4, core % 4
    own = _own_idx(c)
    x = np.asarray(inp['x'], f)
    m = dict(sh)
    m['xfull'] = np.ascontiguousarray(x[b])
    m['xown'] = np.ascontiguousarray(x[b][own])
    m['memb'] = np.ascontiguousarray(np.asarray(inp['mem'], f)[b])
    lp = lrup.copy()
    lp[:, 32 + c] = 1.0
    m['lrup'] = lp
    tq = own.reshape(16, 128).astype(f)
    m['tq4'] = np.ascontiguousarray(np.broadcast_to(np.tile(tq, (1, 4))[:, None, :], (16, 128, 512)))
    cur = (own // 64).reshape(16, 128)
    j = np.arange(128)[None, None, :]
    cu = cur[:, :, None]
    fbt = np.where(j > cu, -1e9, 0.0).astype(f)
    forced = ((j == 0) | (j == cu) | (j == cu - 1)) & (j <= cu)
    fbt = np.where(forced, 1e6 * (1.0 + j), fbt).astype(f)
    m['fbt'] = np.ascontiguousarray(fbt)
    return m


def kernel(**inputs):
    if 'nc' not in _NC_CACHE:
        _NC_CACHE['nc'] = build(False)
    nc = _NC_CACHE['nc']
    sh, lrup = _prep_shared(inputs)
    in_maps = [_prep_core(inputs, sh, lrup, core) for core in range(8)]
    res = run_bass_kernel_spmd(nc, in_maps, core_ids=list(range(8)))
    out = np.zeros((2, 8192, 1024), np.float32)
    for core in range(8):
        b, c = core // 4, core % 4
        out[b, _own_idx(c)] = res.results[core]["yout"]
    return out
```
